# Optimizing a Trainium2 kernel written in Bass

```python
import jax, jax.numpy as jnp
from jax import lax
import numpy as np

D_MODEL = 1024
BATCH = 4
SEQ = 8192
DEPTH = 1

MOBA_HEADS = 8
HEAD_DIM = 64
MOBA_WIDTH = MOBA_HEADS * HEAD_DIM
MOBA_BLOCK = 256
MOBA_TOPK = 3
MOBA_Q_CHUNK = 32
ROPE_THETA = 500000.0
ROPE_DIM = HEAD_DIM // 4
RWKV_HEADS = 8
RWKV_WIDTH = RWKV_HEADS * HEAD_DIM
RWKV_DECAY_LORA = 64
RWKV_AAA_LORA = 64
RWKV_GATE_LORA = 128
RWKV_SHIFT_WIDTH = 3 * RWKV_WIDTH + RWKV_DECAY_LORA + RWKV_AAA_LORA + RWKV_GATE_LORA
GN_EPS = 64e-5
IN_SIZES = (MOBA_WIDTH, MOBA_WIDTH, MOBA_WIDTH, RWKV_SHIFT_WIDTH, 2 * D_MODEL)
IN_WIDTH = sum(IN_SIZES)
PEER_HEADS = 8
PEER_NKEYS = 128
PEER_EXPERTS = PEER_NKEYS * PEER_NKEYS
PEER_QDIM = 256
PEER_HALF = PEER_QDIM // 2
PEER_TOPK = 16
PEER_CHUNK = 128
RMS_EPS = 1e-6
NEG_INF = -1e30

kernel_name = 'hybrid_moba_rwkv7_peer_adaln'


def rms_norm(x, g, eps=RMS_EPS):
    x32 = x.astype(jnp.float32)
    y = x32 * lax.rsqrt(jnp.mean(x32 * x32, axis=-1, keepdims=True) + eps)
    return (y * g.astype(jnp.float32)).astype(x.dtype)


def rope_partial(x, pos):
    half = ROPE_DIM // 2
    inv = ROPE_THETA ** (-(jnp.arange(half, dtype=jnp.float32) * 2.0) / ROPE_DIM)
    ang = pos.astype(jnp.float32)[..., None] * inv
    cos = jnp.cos(ang)[:, :, None, :]
    sin = jnp.sin(ang)[:, :, None, :]
    x32 = x.astype(jnp.float32)
    x1 = x32[..., :half]
    x2 = x32[..., half:ROPE_DIM]
    out = jnp.concatenate([x1 * cos - x2 * sin, x2 * cos + x1 * sin, x32[..., ROPE_DIM:]], axis=-1)
    return out.astype(x.dtype)


def moba_attention(q, k, v):
    B, S, H, Dh = q.shape
    nb = -(-S // MOBA_BLOCK)
    sp = nb * MOBA_BLOCK
    pad = ((0, 0), (0, 0), (0, sp - S), (0, 0))
    q, k, v = (jnp.pad(t.transpose(0, 2, 1, 3), pad) for t in (q, k, v))
    kb = k.reshape(B, H, nb, MOBA_BLOCK, Dh)
    vb = v.reshape(B, H, nb, MOBA_BLOCK, Dh)
    scale = Dh ** -0.5
    qblk = jnp.arange(sp) // MOBA_BLOCK
    n_sel = min(MOBA_TOPK, nb - 1)
    if n_sel > 0:
        kmean = jnp.mean(kb.astype(jnp.float32), axis=3)
        gscore = jnp.einsum('bhsd,bhnd->bhsn', q.astype(jnp.float32), kmean)
        past = jnp.arange(nb)[None, :] < qblk[:, None]
        gscore = jnp.where(past, gscore, NEG_INF)
        _, sel = lax.top_k(gscore, n_sel)
        sel_ok = jnp.arange(n_sel)[None, :] < qblk[:, None]
    bi = jnp.arange(B)[:, None, None, None]
    hi = jnp.arange(H)[None, :, None, None]

    def attend_chunk(i):
        s0 = i * MOBA_Q_CHUNK
        qc = lax.dynamic_slice_in_dim(q, s0, MOBA_Q_CHUNK, axis=2)
        qpos = s0 + jnp.arange(MOBA_Q_CHUNK)
        own = s0 // MOBA_BLOCK
        k_own = lax.dynamic_index_in_dim(kb, own, axis=2, keepdims=False)
        v_own = lax.dynamic_index_in_dim(vb, own, axis=2, keepdims=False)
        kpos = own * MOBA_BLOCK + jnp.arange(MOBA_BLOCK)
        s_own = jnp.einsum('bhcd,bhkd->bhck', qc, k_own).astype(jnp.float32) * scale
        s_own = jnp.where(kpos[None, :] <= qpos[:, None], s_own, NEG_INF)
        if n_sel == 0:
            p = jax.nn.softmax(s_own, axis=-1).astype(v.dtype)
            return jnp.einsum('bhck,bhkd->bhcd', p, v_own)
        selc = lax.dynamic_slice_in_dim(sel, s0, MOBA_Q_CHUNK, axis=2)
        ok = lax.dynamic_slice_in_dim(sel_ok, s0, MOBA_Q_CHUNK, axis=0)
        k_sel = kb[bi, hi, selc]
        v_sel = vb[bi, hi, selc]
        s_sel = jnp.einsum('bhcd,bhcnkd->bhcnk', qc, k_sel).astype(jnp.float32) * scale
        s_sel = jnp.where(ok[None, None, :, :, None], s_sel, NEG_INF)
        s_all = jnp.concatenate([s_sel.reshape(B, H, MOBA_Q_CHUNK, n_sel * MOBA_BLOCK), s_own], axis=-1)
        p = jax.nn.softmax(s_all, axis=-1).astype(v.dtype)
        p_sel = p[..., :n_sel * MOBA_BLOCK].reshape(B, H, MOBA_Q_CHUNK, n_sel, MOBA_BLOCK)
        p_own = p[..., n_sel * MOBA_BLOCK:]
        return (jnp.einsum('bhcnk,bhcnkd->bhcd', p_sel, v_sel)
                + jnp.einsum('bhck,bhkd->bhcd', p_own, v_own))

    out = lax.map(attend_chunk, jnp.arange(sp // MOBA_Q_CHUNK))
    out = out.transpose(1, 0, 3, 2, 4).reshape(B, sp, H, Dh)
    return out[:, :S]


def token_shift(z, mu):
    prev = jnp.pad(z, ((0, 0), (1, 0), (0, 0)))[:, :-1]
    return z + (prev - z) * mu


def rwkv7_time_mix(zr, zk, zv, zw, za, zg, w0, w2, a0, a2, g2, k_k, k_a, r_k, ln_g, ln_b):
    B, S, _ = zr.shape
    H, N = RWKV_HEADS, HEAD_DIM
    f32 = jnp.float32
    w = -jax.nn.softplus(-(w0 + jnp.tanh(zw) @ w2)) - 0.5
    decay = jnp.exp(-jnp.exp(w.astype(f32)))
    a = jax.nn.sigmoid(a0 + za @ a2)
    g = jax.nn.sigmoid(zg) @ g2
    heads = lambda t: t.reshape(B, S, H, N).astype(f32)
    kk = heads(zk * k_k)
    kk = kk / jnp.maximum(jnp.sqrt(jnp.sum(kk * kk, axis=-1, keepdims=True)), 1e-12)
    k = zk * (1.0 + (a - 1.0) * k_a)
    r_h, k_h, v_h, a_h, d_h = heads(zr), heads(k), heads(zv), heads(a), heads(decay)

    def step(state, inp):
        r_t, d_t, k_t, v_t, kk_t, a_t = inp
        sa = jnp.einsum('bhij,bhj->bhi', state, -kk_t)
        state = (state * d_t[:, :, None, :] + sa[..., None] * (kk_t * a_t)[:, :, None, :]
                 + v_t[..., None] * k_t[:, :, None, :])
        return state, jnp.einsum('bhij,bhj->bhi', state, r_t)

    xs = tuple(t.transpose(1, 0, 2, 3) for t in (r_h, d_h, k_h, v_h, kk, a_h))
    _, y = lax.scan(step, jnp.zeros((B, H, N, N), f32), xs)
    y = y.transpose(1, 0, 2, 3)
    mean = jnp.mean(y, axis=-1, keepdims=True)
    var = jnp.mean(jnp.square(y - mean), axis=-1, keepdims=True)
    y = ((y - mean) * lax.rsqrt(var + GN_EPS)).reshape(B, S, H * N)
    y = y * ln_g.astype(f32) + ln_b.astype(f32)
    bonus = jnp.sum(r_h * k_h * r_k.astype(f32), axis=-1, keepdims=True) * v_h
    y = y + bonus.reshape(B, S, H * N)
    return (y * g.astype(f32)).astype(zr.dtype)


def peer_ffn(xn, wq, subkeys, u_tab, v_tab):
    B, S, D = xn.shape
    T = B * S
    xt = xn.reshape(T, D)
    q = (xt @ wq).reshape(T, PEER_HEADS, 2, PEER_HALF)
    s = jnp.einsum('thpd,hpnd->thpn', q, subkeys).astype(jnp.float32)
    top_s, top_i = lax.top_k(s, PEER_TOPK)
    cand_s = (top_s[:, :, 0, :, None] + top_s[:, :, 1, None, :]).reshape(T, PEER_HEADS, PEER_TOPK * PEER_TOPK)
    cand_i = (top_i[:, :, 0, :, None] * PEER_NKEYS + top_i[:, :, 1, None, :]).reshape(T, PEER_HEADS, PEER_TOPK * PEER_TOPK)
    best_s, best_j = lax.top_k(cand_s, PEER_TOPK)
    idx = jnp.take_along_axis(cand_i, best_j, axis=-1)
    gates = jax.nn.softmax(best_s, axis=-1)
    nc = T // PEER_CHUNK

    def retrieve(args):
        xc, ic, gc = args
        uc = u_tab[ic]
        vc = v_tab[ic]
        hdn = jax.nn.gelu(jnp.einsum('cd,chkd->chk', xc, uc).astype(jnp.float32), approximate=False) * gc
        return jnp.einsum('chk,chkd->cd', hdn.astype(vc.dtype), vc)

    out = lax.map(retrieve, (xt.reshape(nc, PEER_CHUNK, D),
                             idx.reshape(nc, PEER_CHUNK, PEER_HEADS, PEER_TOPK),
                             gates.reshape(nc, PEER_CHUNK, PEER_HEADS, PEER_TOPK)))
    return out.reshape(B, S, D).astype(xn.dtype)


def setup_inputs(seed: int = 0) -> dict:
    key = jax.random.key(seed)
    ks = iter(jax.random.split(key, 40))
    L = DEPTH
    D = D_MODEL
    nrm = lambda shape, sc: jax.random.normal(next(ks), shape, jnp.float32) * sc
    unif = lambda shape, lo, hi: jax.random.uniform(next(ks), shape, jnp.float32, lo, hi)
    x = nrm((BATCH, SEQ, D), 1.0)
    c = nrm((BATCH, D), 1.0)
    positions = jnp.tile(jnp.arange(SEQ, dtype=jnp.int32)[None, :], (BATCH, 1))
    return {
        'x': x, 'c': c, 'positions': positions,
        'w_ada': nrm((L, D, 6 * D), 0.5 * D ** -0.5),
        'b_ada': nrm((L, 6 * D), 0.02),
        'norm1_g': 1.0 + nrm((L, D), 0.05),
        'w_in': nrm((L, D, IN_WIDTH), D ** -0.5),
        'q_norm_g': 1.0 + nrm((L, HEAD_DIM), 0.05),
        'k_norm_g': 1.0 + nrm((L, HEAD_DIM), 0.05),
        'rwkv_mu': unif((L, RWKV_SHIFT_WIDTH), 0.0, 1.0),
        'rwkv_w0': nrm((L, RWKV_WIDTH), 1.0) - 2.0,
        'rwkv_w2': nrm((L, RWKV_DECAY_LORA, RWKV_WIDTH), 0.5 * RWKV_DECAY_LORA ** -0.5),
        'rwkv_a0': nrm((L, RWKV_WIDTH), 0.5),
        'rwkv_a2': nrm((L, RWKV_AAA_LORA, RWKV_WIDTH), 0.5 * RWKV_AAA_LORA ** -0.5),
        'rwkv_g2': nrm((L, RWKV_GATE_LORA, RWKV_WIDTH), RWKV_GATE_LORA ** -0.5),
        'rwkv_k_k': 0.85 + nrm((L, RWKV_WIDTH), 0.1),
        'rwkv_k_a': 1.0 + nrm((L, RWKV_WIDTH), 0.1),
        'rwkv_r_k': nrm((L, RWKV_HEADS, HEAD_DIM), 0.1),
        'rwkv_ln_g': 1.0 + nrm((L, RWKV_WIDTH), 0.05),
        'rwkv_ln_b': nrm((L, RWKV_WIDTH), 0.02),
        'w_proj_moba': nrm((L, MOBA_WIDTH, D), MOBA_WIDTH ** -0.5),
        'w_proj_rwkv': nrm((L, RWKV_WIDTH, D), RWKV_WIDTH ** -0.5),
        'w_out': nrm((L, D, D), D ** -0.5),
        'norm2_g': 1.0 + nrm((L, D), 0.05),
        'peer_wq': nrm((L, D, PEER_HEADS * PEER_QDIM), D ** -0.5),
        'peer_subkeys': nrm((L, PEER_HEADS, 2, PEER_NKEYS, PEER_HALF), PEER_HALF ** -0.5),
        'peer_u': nrm((L, PEER_EXPERTS, D), D ** -0.5),
        'peer_v': nrm((L, PEER_EXPERTS, D), PEER_HEADS ** -0.5),
    }


def reference(x, c, positions, w_ada, b_ada, norm1_g, w_in, q_norm_g, k_norm_g, rwkv_mu,
              rwkv_w0, rwkv_w2, rwkv_a0, rwkv_a2, rwkv_g2, rwkv_k_k, rwkv_k_a, rwkv_r_k,
              rwkv_ln_g, rwkv_ln_b, w_proj_moba, w_proj_rwkv, w_out, norm2_g, peer_wq,
              peer_subkeys, peer_u, peer_v):
    B, S, D = x.shape
    split_at = [int(v) for v in np.cumsum(IN_SIZES)[:-1]]
    rwkv_split = [int(v) for v in np.cumsum((RWKV_WIDTH, RWKV_WIDTH, RWKV_WIDTH, RWKV_DECAY_LORA, RWKV_AAA_LORA))]
    h = x
    for l in range(DEPTH):
        mod = (jax.nn.silu(c) @ w_ada[l] + b_ada[l])[:, None, :]
        sh1, sc1, gt1, sh2, sc2, gt2 = jnp.split(mod, 6, axis=-1)
        xn = rms_norm(h, norm1_g[l]) * (1.0 + sc1) + sh1
        proj = xn @ w_in[l]
        qm, km, vm, zrw, gate_logits = jnp.split(proj, split_at, axis=-1)
        qm = rope_partial(rms_norm(qm.reshape(B, S, MOBA_HEADS, HEAD_DIM), q_norm_g[l]), positions)
        km = rope_partial(rms_norm(km.reshape(B, S, MOBA_HEADS, HEAD_DIM), k_norm_g[l]), positions)
        vm = vm.reshape(B, S, MOBA_HEADS, HEAD_DIM)
        o_moba = moba_attention(qm, km, vm).reshape(B, S, MOBA_WIDTH)
        zrw = token_shift(zrw, rwkv_mu[l])
        zr, zk, zv, zw, za, zg = jnp.split(zrw, rwkv_split, axis=-1)
        o_rwkv = rwkv7_time_mix(zr, zk, zv, zw, za, zg, rwkv_w0[l], rwkv_w2[l], rwkv_a0[l],
                                rwkv_a2[l], rwkv_g2[l], rwkv_k_k[l], rwkv_k_a[l], rwkv_r_k[l],
                                rwkv_ln_g[l], rwkv_ln_b[l])
        g_moba, g_rwkv = jnp.split(jax.nn.sigmoid(gate_logits), 2, axis=-1)
        mixed = g_moba * (o_moba @ w_proj_moba[l]) + g_rwkv * (o_rwkv @ w_proj_rwkv[l])
        h = h + gt1 * (mixed @ w_out[l])
        xn2 = rms_norm(h, norm2_g[l]) * (1.0 + sc2) + sh2
        h = h + gt2 * peer_ffn(xn2, peer_wq[l], peer_subkeys[l], peer_u[l], peer_v[l])
    return h
```

```python
import numpy as np
import concourse.bass as bass
import concourse.mybir as mybir
from concourse.bass_utils import run_bass_kernel_spmd
from contextlib import ExitStack

F32 = mybir.dt.float32
BF16 = mybir.dt.bfloat16
I32 = mybir.dt.int32
AF = mybir.ActivationFunctionType
ALU = mybir.AluOpType
AX = mybir.AxisListType
EPOCH = 24000
P = 128


class Buf:
    def __init__(self, t, name):
        self.t = t
        self.name = name
        self.ws = {}
        self.rs = {}
        self.dkey = None
        self.dcnt = 0

    def __getitem__(self, k):
        return self.t[k]


class KB:
    ENG = ("pe", "act", "dve", "pool", "sp")

    def __init__(self, nc, stack):
        self.nc = nc
        self.stack = stack
        self.phs = []
        self.scope_bufs = []
        self.dpool = []
        self.ops = {e: [] for e in self.ENG}
        self.cnt = {e: 0 for e in self.ENG}
        self.known = {e: {} for e in self.ENG}
        self.sems = {}
        self.nsem = 0
        self.nins = 0

    def sem(self, key):
        if key not in self.sems:
            self.sems[key] = self.stack.enter_context(self.nc.semaphore(f"s{self.nsem}"))
            self.nsem += 1
        return self.sems[key]

    def gsbuf(self, name, shape, dt):
        return Buf(self.stack.enter_context(self.nc.sbuf_tensor(name, list(shape), dt)), name)

    def sbuf(self, name, shape, dt):
        self.nsem += 0
        self.uid = getattr(self, "uid", 0) + 1
        name = f"{name}_u{self.uid}"
        b = Buf(self.phs[-1].enter_context(self.nc.sbuf_tensor(name, list(shape), dt)), name)
        self.scope_bufs[-1].append(b)
        return b

    def psum(self, name, shape, dt):
        self.uid = getattr(self, "uid", 0) + 1
        name = f"{name}_u{self.uid}"
        return Buf(self.phs[-1].enter_context(self.nc.psum_tensor(name, list(shape), dt)), name)

    def dram(self, name, shape, dt, kind="Internal"):
        return Buf(self.nc.dram_tensor(name, list(shape), dt, kind=kind).ap(), name)

    def _deps(self, eng, reads, writes):
        d = {}
        for b in reads:
            for k, v in b.ws.items():
                if d.get(k, 0) < v:
                    d[k] = v
        for b in writes:
            for k, v in b.ws.items():
                if d.get(k, 0) < v:
                    d[k] = v
            for k, v in b.rs.items():
                if d.get(k, 0) < v:
                    d[k] = v
        kn = self.known[eng]
        for k, v in d.items():
            if eng == "pe" and k[0] == "pe":
                continue
            if kn.get(k, 0) >= v:
                continue
            kn[k] = v
            self.ops[eng].append(("wait", k, v))

    def op(self, eng, fn, reads=(), writes=()):
        self._deps(eng, reads, writes)
        self.cnt[eng] += 1
        n = self.cnt[eng]
        key = (eng, (n - 1) // EPOCH)
        val = (n - 1) % EPOCH + 1
        self.sem(key)
        self.ops[eng].append(("op", fn, key))
        for b in reads:
            if b.rs.get(key, 0) < val:
                b.rs[key] = val
        for b in writes:
            if b.ws.get(key, 0) < val:
                b.ws[key] = val

    def dma(self, q, out_ap, in_ap, reads, writes, sb, **kw):
        self._deps(q, reads, writes)
        if sb.dkey is None:
            if self.dpool:
                sb.dkey, sb.dcnt = self.dpool.pop()
            else:
                sb.dkey = ("d", sb.name)
                self.sem(sb.dkey)
        sb.dcnt += 16
        key, val = sb.dkey, sb.dcnt
        self.ops[q].append(("dma", out_ap, in_ap, key, kw))
        for b in reads:
            if b.rs.get(key, 0) < val:
                b.rs[key] = val
        for b in writes:
            if b.ws.get(key, 0) < val:
                b.ws[key] = val

    def wait_all(self, eng, bufs):
        self._deps(eng, bufs, ())

    def begin(self):
        st = ExitStack()
        st.__enter__()
        self.phs.append(st)
        self.scope_bufs.append([])

    def end(self):
        nc = self.nc
        mine = self.scope_bufs.pop()
        for b in mine:
            if b.dkey is not None:
                if self.known["sp"].get(b.dkey, 0) < b.dcnt:
                    self.known["sp"][b.dkey] = b.dcnt
                    self.ops["sp"].append(("wait", b.dkey, b.dcnt))
                self.dpool.append((b.dkey, b.dcnt))
        with nc.Block() as blk:
            def run(e, name):
                pend = None
                for o in self.ops[name]:
                    if o[0] == "wait":
                        if pend is not None:
                            self.nins += 1
                            e.wait_ge(self.sems[pend[1]], pend[2])
                        pend = o
                        continue
                    self.nins += 1
                    if o[0] == "op":
                        ins = o[1](e)
                        if pend is not None:
                            ins._wait_ge(self.sems[pend[1]], pend[2])
                        ins.then_inc(self.sems[o[2]], 1)
                    else:
                        ins = e.dma_start(out=o[1], in_=o[2], **o[4])
                        if pend is not None:
                            ins._wait_ge(self.sems[pend[1]], pend[2])
                        ins.then_inc(self.sems[o[3]], 16)
                    pend = None
                if pend is not None:
                    self.nins += 1
                    e.wait_ge(self.sems[pend[1]], pend[2])
                self.ops[name] = []

            @blk.tensor
            def _(e):
                run(e, "pe")

            @blk.scalar
            def _(e):
                run(e, "act")

            @blk.vector
            def _(e):
                run(e, "dve")

            @blk.gpsimd
            def _(e):
                run(e, "pool")

            @blk.sync
            def _(e):
                run(e, "sp")
        self.phs.pop().__exit__(None, None, None)


NT = 8192
NOWN = 4096
NTILE = NT // P
OWN0 = (NT - NOWN) // P


def bc(ap, shape):
    return ap.to_broadcast(list(shape))


class Ctx:
    pass


def declare(kb, dbg):
    g = Ctx()
    g.dbg = dbg
    sk = "ExternalOutput" if dbg else "Internal"
    EI = "ExternalInput"
    g.xs = kb.dram("xs", [NT, 1024], F32, EI)
    g.pos = kb.dram("pos", [NT, 1], I32, EI)
    g.cc = kb.dram("cc", [8, 128], F32, EI)
    g.pvd = kb.dram("pv", [128, 1], F32, EI)
    g.invf = kb.dram("invf", [128, 8], F32, EI)
    g.w_ada = kb.dram("w_ada", [1024, 6144], F32, EI)
    g.b_ada = kb.dram("b_ada", [1, 6144], F32, EI)
    g.norm1_g = kb.dram("norm1_g", [1, 1024], F32, EI)
    g.w_in = kb.dram("w_in", [1024, 5376], F32, EI)
    g.q_norm_g = kb.dram("q_norm_g", [1, 64], F32, EI)
    g.k_norm_g = kb.dram("k_norm_g", [1, 64], F32, EI)
    g.rwkv_mu = kb.dram("rwkv_mu", [1, 1792], F32, EI)
    g.rwkv_w0 = kb.dram("rwkv_w0", [1, 512], F32, EI)
    g.rwkv_w2 = kb.dram("rwkv_w2", [64, 512], F32, EI)
    g.rwkv_a0 = kb.dram("rwkv_a0", [1, 512], F32, EI)
    g.rwkv_a2 = kb.dram("rwkv_a2", [64, 512], F32, EI)
    g.rwkv_g2 = kb.dram("rwkv_g2", [128, 512], F32, EI)
    g.rwkv_k_k = kb.dram("rwkv_k_k", [1, 512], F32, EI)
    g.rwkv_k_a = kb.dram("rwkv_k_a", [1, 512], F32, EI)
    g.rwkv_r_k = kb.dram("rwkv_r_k", [1, 512], F32, EI)
    g.rwkv_ln_g = kb.dram("rwkv_ln_g", [1, 512], F32, EI)
    g.rwkv_ln_b = kb.dram("rwkv_ln_b", [1, 512], F32, EI)
    g.w_proj_moba = kb.dram("w_proj_moba", [512, 1024], F32, EI)
    g.w_proj_rwkv = kb.dram("w_proj_rwkv", [512, 1024], F32, EI)
    g.w_out = kb.dram("w_out", [1024, 1024], F32, EI)
    g.norm2_g = kb.dram("norm2_g", [1, 1024], F32, EI)
    g.peer_wq = kb.dram("peer_wq", [1024, 2048], F32, EI)
    g.peer_sk = kb.dram("peer_sk", [16, 128, 128], F32, EI)
    g.peer_u = kb.dram("peer_u", [16384, 1024], F32, EI)
    g.peer_v = kb.dram("peer_v", [16384, 1024], F32, EI)
    g.out = kb.dram("out", [NOWN, 1024], F32, "ExternalOutput")
    g.QT = kb.dram("QT", [4, 128, NOWN], BF16, sk)
    g.KT = kb.dram("KT", [4, 128, NT], BF16, sk)
    g.VM = kb.dram("VM", [NT, 512], BF16, sk)
    g.RS = kb.dram("RS", [6, 8, NT, 64], F32, sk)
    g.GR = kb.dram("GR", [NOWN, 512], F32, sk)
    g.GATES = kb.dram("GATES", [NOWN, 2048], BF16, sk)
    g.OM = kb.dram("OM", [NOWN, 512], BF16, sk)
    g.ORW = kb.dram("ORW", [NOWN, 512], BF16, sk)
    g.H1 = kb.dram("H1", [NOWN, 1024], F32, sk)
    g.XN2T = kb.dram("XN2T", [8, 128, NOWN], BF16, sk)
    g.SC = kb.dram("SC", [NOWN, 16, 128], F32, sk)
    g.TH = kb.dram("TH", [NOWN, 4, 8], F32, sk)
    g.UT = kb.dram("UT", [8, 128, 16384], BF16, "Internal")
    g.VB = kb.dram("VB", [16384, 1024], BF16, "Internal")
    g.ident_f = kb.gsbuf("ident_f", [128, 128], F32)
    g.ident_b = kb.gsbuf("ident_b", [128, 128], BF16)
    g.ones_f = kb.gsbuf("ones_f", [128, 128], F32)
    g.modc = kb.gsbuf("modc", [128, 48], F32)
    g.sc1p = kb.gsbuf("sc1p", [128, 8], F32)
    g.sc2p = kb.gsbuf("sc2p", [128, 8], F32)
    g.gt_bc = kb.gsbuf("gt_bc", [128, 2, 1024], F32)
    g.pv = kb.gsbuf("pvt", [128, 1], F32)
    g.kmT = kb.gsbuf("kmT", [128, 4, NT // 256], F32)
    g.npi = kb.gsbuf("npi", [128, 1], F32)
    return g


def phase0(kb, g):
    kb.begin()
    V = lambda fn, r, w: kb.op("dve", fn, r, w)
    A = lambda fn, r, w: kb.op("act", fn, r, w)
    PE = lambda fn, r, w: kb.op("pe", fn, r, w)
    PL = lambda fn, r, w: kb.op("pool", fn, r, w)
    PL(lambda e: e.memset(g.ones_f[:], 1.0), [], [g.ones_f])
    PL(lambda e: e.memset(g.npi[:], -float(np.pi)), [], [g.npi])
    PL(lambda e: e.memset(g.ident_f[:], 1.0), [], [g.ident_f])
    PL(lambda e: e.affine_select(out=g.ident_f[:], in_=g.ident_f[:], pattern=[[-1, 128]],
                                 compare_op=ALU.is_equal, fill=0.0, base=0, channel_multiplier=1),
       [g.ident_f], [g.ident_f])
    V(lambda e: e.tensor_copy(g.ident_b[:], g.ident_f[:]), [g.ident_f], [g.ident_b])
    kb.dma("sp", g.pv[:], g.pvd[:], [g.pvd], [g.pv], g.pv)
    c8 = kb.sbuf("c8", [8, 128], F32)
    scT = kb.sbuf("scT", [128, 8], F32)
    modrow = kb.sbuf("modrow", [1, 6144], F32)
    brow = kb.sbuf("brow", [1, 6144], F32)
    grow = kb.sbuf("grow", [1, 2, 1024], F32)
    gcol = kb.sbuf("gcol", [128, 16], F32)
    wst = [kb.sbuf(f"wst{i}", [128, 8, 512], F32) for i in range(2)]
    ps = kb.psum("p0ps", [128, 512], F32)
    ps2 = kb.psum("p0ps2", [128, 512], F32)
    kb.dma("sp", c8[:], g.cc[:], [g.cc], [c8], c8)
    kb.dma("sp", brow[:], g.b_ada[:], [g.b_ada], [brow], brow)
    kb.dma("sp", grow[:, 0, :], g.norm1_g[:], [g.norm1_g], [grow], grow)
    kb.dma("sp", grow[:, 1, :], g.norm2_g[:], [g.norm2_g], [grow], grow)
    PE(lambda e: e.transpose(ps[:, 0:8], c8[:], g.ident_f[0:8, 0:8]), [c8, g.ident_f], [ps])
    A(lambda e: e.activation(scT[:], ps[:, 0:8], AF.Silu), [ps], [scT])
    wv = g.w_ada.t.rearrange("(k p) n -> p k n", p=128)
    for jg in range(12):
        wb_ = wst[jg % 2]
        kb.dma("sp", wb_[:], wv[:, :, jg * 512:(jg + 1) * 512], [g.w_ada], [wb_], wb_)
        for k in range(8):
            PE(lambda e, k=k, wb_=wb_: e.matmul(ps2[0:1, :], scT[:, k:k + 1], wb_[:, k, :],
                                                  start=(k == 0), stop=(k == 7)), [scT, wb_], [ps2])
        V(lambda e, jg=jg: e.tensor_tensor(modrow[:, jg * 512:(jg + 1) * 512], ps2[0:1, :],
                                           brow[:, jg * 512:(jg + 1) * 512], ALU.add), [ps2, brow], [modrow])
    for j in range(48):
        PE(lambda e, j=j: e.matmul(ps[:, 16 + j:17 + j], modrow[0:1, j * 128:(j + 1) * 128], g.ones_f[0:1, 0:1],
                                   start=True, stop=True), [modrow, g.ones_f], [ps])
    V(lambda e: e.tensor_copy(g.modc[:], ps[:, 16:64]), [ps], [g.modc])
    for j in range(16):
        PE(lambda e, j=j: e.matmul(ps[:, 64 + j:65 + j], grow[0:1, j // 8, (j % 8) * 128:(j % 8 + 1) * 128],
                                   g.ones_f[0:1, 0:1], start=True, stop=True), [grow, g.ones_f], [ps])
    V(lambda e: e.tensor_copy(gcol[:], ps[:, 64:80]), [ps], [gcol])
    V(lambda e: e.scalar_tensor_tensor(g.sc1p[:], g.modc[:, 8:16], 1.0, gcol[:, 0:8], ALU.add, ALU.mult),
      [g.modc, gcol], [g.sc1p])
    V(lambda e: e.scalar_tensor_tensor(g.sc2p[:], g.modc[:, 32:40], 1.0, gcol[:, 8:16], ALU.add, ALU.mult),
      [g.modc, gcol], [g.sc2p])
    for i, c0 in enumerate((16 * 128, 40 * 128)):
        for hh in range(2):
            PE(lambda e, c0=c0, hh=hh: e.matmul(ps2[:, :], g.ones_f[0:1, :], modrow[0:1, c0 + hh * 512:c0 + (hh + 1) * 512],
                                                start=True, stop=True), [g.ones_f, modrow], [ps2])
            A(lambda e, i=i, hh=hh: e.copy(g.gt_bc[:, i, hh * 512:(hh + 1) * 512], ps2[:, :]), [ps2], [g.gt_bc])
    kb.end()


def phase_a(kb, g, ntiles=NTILE):
    kb.begin()
    V = lambda fn, r, w: kb.op("dve", fn, r, w)
    A = lambda fn, r, w: kb.op("act", fn, r, w)
    PE = lambda fn, r, w: kb.op("pe", fn, r, w)
    PL = lambda fn, r, w: kb.op("pool", fn, r, w)
    Wb = kb.sbuf("Wb", [128, 8, 5376], BF16)
    Wm = kb.sbuf("Wm", [128, 8, 1792], BF16)
    kb.begin()
    mu_bc = kb.sbuf("mu_bc", [128, 1792], F32)
    omu_bc = kb.sbuf("omu_bc", [128, 1792], F32)
    wst = [kb.sbuf(f"awst{i}", [128, 8, 256], F32) for i in range(2)]
    kb.dma("sp", mu_bc[:], g.rwkv_mu.t.partition_broadcast(128), [g.rwkv_mu], [mu_bc], mu_bc)
    V(lambda e: e.tensor_scalar(omu_bc[:], mu_bc[:], -1.0, 1.0, ALU.mult, ALU.add), [mu_bc], [omu_bc])
    wv = g.w_in.t.rearrange("(k p) n -> p k n", p=128)
    engs = ["dve", "act", "pool"]
    for pc in range(21):
        c0 = pc * 256
        st = wst[pc % 2]
        kb.dma("sp", st[:], wv[:, :, c0:c0 + 256], [g.w_in], [st], st)
        if 1536 <= c0 < 3328:
            m0 = c0 - 1536
            V(lambda e, st=st, c0=c0, m0=m0: e.tensor_tensor(Wb[:, :, c0:c0 + 256], st[:],
              bc(omu_bc[:, m0:m0 + 256].unsqueeze(1), [128, 8, 256]), ALU.mult), [st, omu_bc], [Wb])
            PL(lambda e, st=st, m0=m0: e.tensor_tensor(Wm[:, :, m0:m0 + 256], st[:],
               bc(mu_bc[:, m0:m0 + 256].unsqueeze(1), [128, 8, 256]), ALU.mult), [st, mu_bc], [Wm])
        else:
            en = engs[pc % 2]
            if en == "act":
                A(lambda e, st=st, c0=c0: e.copy(Wb[:, :, c0:c0 + 256], st[:]), [st], [Wb])
            else:
                V(lambda e, st=st, c0=c0: e.tensor_copy(Wb[:, :, c0:c0 + 256], st[:]), [st], [Wb])
    kb.end()
    def rowbc(name, src, n):
        t = kb.sbuf(name, [128, n], F32)
        kb.dma("sp", t[:], src.t.partition_broadcast(128), [src], [t], t)
        return t
    qg = rowbc("qg", g.q_norm_g, 64)
    kg = rowbc("kg", g.k_norm_g, 64)
    w0b = rowbc("w0b", g.rwkv_w0, 512)
    a0b = rowbc("a0b", g.rwkv_a0, 512)
    kkb = rowbc("kkb", g.rwkv_k_k, 512)
    kab = rowbc("kab", g.rwkv_k_a, 512)
    invf = kb.sbuf("invf_t", [128, 8], F32)
    kb.dma("sp", invf[:], g.invf[:], [g.invf], [invf], invf)
    w2a2 = kb.sbuf("w2a2", [128, 512], F32)
    g2t = kb.sbuf("g2t", [128, 512], F32)
    kb.dma("sp", w2a2[0:64, :], g.rwkv_w2[:], [g.rwkv_w2], [w2a2], w2a2)
    kb.dma("sp", w2a2[64:128, :], g.rwkv_a2[:], [g.rwkv_a2], [w2a2], w2a2)
    kb.dma("sp", g2t[:], g.rwkv_g2[:], [g.rwkv_g2], [g2t], g2t)
    onesc = kb.sbuf("onesc", [128, 1], F32)
    PL(lambda e: e.memset(onesc[:], 1.0 / 256.0), [], [onesc])

    xt = [kb.sbuf(f"xt{i}", [128, 1024], F32) for i in range(2)]
    junk = kb.sbuf("junk", [128, 1024], BF16)
    xnT = [kb.sbuf(f"xnT{i}", [128, 8, 129], BF16) for i in range(2)]
    stt = [kb.sbuf(f"stt{i}", [128, 4], F32) for i in range(2)]
    posi = [kb.sbuf(f"posi{i}", [128, 1], I32) for i in range(2)]
    cs = [kb.sbuf(f"cs{i}", [128, 5, 16], F32) for i in range(2)]
    csi = [kb.sbuf(f"csi{i}", [128, 16], I32) for i in range(2)]
    psT = [kb.psum(f"psT{i}", [128, 512], F32) for i in range(2)]
    psM = [kb.psum(f"psM{i}", [128, 512], F32) for i in range(4)]
    psB = kb.psum("psB", [128, 1024], BF16)
    psX = kb.psum("psX", [128, 512], F32)
    NB = 3
    t1 = [kb.sbuf(f"t1_{i}", [128, 512], F32) for i in range(NB)]
    t2 = [kb.sbuf(f"t2_{i}", [128, 512], F32) for i in range(NB)]
    ssq = [kb.sbuf(f"ssq{i}", [128, 16], F32) for i in range(NB)]
    rp = [kb.sbuf(f"rp{i}", [128, 4, 8, 8], F32) for i in range(NB)]
    qf = [kb.sbuf(f"qf{i}", [128, 512], BF16) for i in range(NB)]
    qTs = [kb.sbuf(f"qTs{i}", [128, 4, 128], BF16) for i in range(NB)]
    kmp = [kb.sbuf(f"kmp{i}", [128, 4], F32) for i in range(2)]
    la = [kb.sbuf(f"la{i}", [128, 256], F32) for i in range(2)]
    laT = [kb.sbuf(f"laT{i}", [128, 256], F32) for i in range(2)]
    av = [kb.sbuf(f"av{i}", [128, 512], F32) for i in range(2)]
    sto = [kb.sbuf(f"sto{i}", [128, 512], F32) for i in range(8)]
    gsb = [kb.sbuf(f"gsb{i}", [128, 512], BF16) for i in range(3)]
    cnt = {"m": 0, "b": 0, "s": 0, "g": 0}

    def nextM():
        cnt["m"] += 1
        return psM[cnt["m"] % 4]

    def nextS():
        cnt["s"] += 1
        return sto[cnt["s"] % 8]

    def mm(ps_, ncols, col0, xn, prev_c0=None):
        for k in range(8):
            PE(lambda e, k=k: e.matmul(ps_[:, 0:ncols], xn[:, k, 1:129], Wb[:, k, col0:col0 + ncols],
                                       start=(k == 0), stop=(k == 7 and prev_c0 is None)), [xn, Wb], [ps_])
        if prev_c0 is not None:
            for k in range(8):
                PE(lambda e, k=k: e.matmul(ps_[:, 0:ncols], xn[:, k, 0:128], Wm[:, k, prev_c0:prev_c0 + ncols],
                                           start=False, stop=(k == 7)), [xn, Wm], [ps_])

    def stream_store(si, src, t0):
        dst = g.RS.t[si, :, t0:t0 + 128, :].rearrange("h t n -> t h n")
        kb.dma("sp", dst, src[:].rearrange("p (h n) -> p h n", h=8), [src], [g.RS], src)

    def tile_body(it):
        own = it >= OWN0
        t0 = it * 128
        x_ = xt[it % 2]
        xn = xnT[it % 2]
        xnp = xnT[(it + 1) % 2]
        st_ = stt[it % 2]
        kb.dma("sp", x_[:], g.xs[t0:t0 + 128, :], [g.xs], [x_], x_)
        pi = posi[it % 2]
        kb.dma("sp", pi[:], g.pos[t0:t0 + 128, :], [g.pos], [pi], pi)
        A(lambda e, x_=x_, st_=st_: e.activation(junk[:], x_[:], AF.Square, accum_out=st_[:, 0:1]), [x_], [junk, st_])
        V(lambda e, st_=st_: e.tensor_scalar(st_[:, 1:2], st_[:, 0:1], 1.0 / 1024.0, 1e-6, ALU.mult, ALU.add), [st_], [st_])
        A(lambda e, st_=st_: e.activation(st_[:, 2:3], st_[:, 1:2], AF.Sqrt), [st_], [st_])
        V(lambda e, st_=st_: e.reciprocal(st_[:, 2:3], st_[:, 2:3]), [st_], [st_])
        V(lambda e, x_=x_, st_=st_: e.tensor_scalar_mul(x_[:], x_[:], st_[:, 2:3]), [x_, st_], [x_])
        for k in range(8):
            pt = psT[k // 4]
            PE(lambda e, k=k, pt=pt, x_=x_: e.transpose(pt[:, (k % 4) * 128:(k % 4 + 1) * 128], x_[:, k * 128:(k + 1) * 128],
                                                        g.ident_f[:]), [x_, g.ident_f], [pt])
            A(lambda e, k=k, pt=pt, xn=xn: e.activation(xn[:, k, 1:129], pt[:, (k % 4) * 128:(k % 4 + 1) * 128], AF.Identity,
                                                       bias=g.modc[:, k:k + 1], scale=g.sc1p[:, k:k + 1]),
              [pt, g.modc, g.sc1p], [xn])
        if it == 0:
            V(lambda e, xn=xn: e.memset(xn[:, :, 0:1], 0.0), [], [xn])
        elif it == OWN0:
            V(lambda e, xn=xn, xnp=xnp: e.tensor_scalar_mul(xn[:, :, 0:1], xnp[:, :, 128:129], g.pv[:, 0:1]), [xnp, g.pv], [xn])
        else:
            V(lambda e, xn=xn, xnp=xnp: e.tensor_copy(xn[:, :, 0:1], xnp[:, :, 128:129]), [xnp], [xn])
        c_ = cs[it % 2]
        ci_ = csi[it % 2]
        PI = float(np.pi)
        V(lambda e: e.tensor_copy(c_[:, 1, 0:1], pi[:]), [pi], [c_])
        V(lambda e: e.tensor_scalar_mul(c_[:, 0, 0:8], invf[:], c_[:, 1, 0:1]), [invf, c_], [c_])
        V(lambda e: e.tensor_scalar_add(c_[:, 0, 8:16], c_[:, 0, 0:8], 0.5 * PI), [c_], [c_])
        V(lambda e: e.tensor_scalar_mul(c_[:, 1, :], c_[:, 0, :], 1.0 / (2 * PI)), [c_], [c_])
        V(lambda e: e.tensor_copy(ci_[:], c_[:, 1, :]), [c_], [ci_])
        V(lambda e: e.tensor_copy(c_[:, 1, :], ci_[:]), [ci_], [c_])
        V(lambda e: e.scalar_tensor_tensor(c_[:, 2, :], c_[:, 1, :], -2 * PI, c_[:, 0, :], ALU.mult, ALU.add), [c_], [c_])
        V(lambda e: e.tensor_single_scalar(c_[:, 1, :], c_[:, 2, :], PI, ALU.is_gt), [c_], [c_])
        V(lambda e: e.scalar_tensor_tensor(c_[:, 3, :], c_[:, 1, :], -2 * PI, c_[:, 2, :], ALU.mult, ALU.add), [c_], [c_])
        V(lambda e: e.tensor_single_scalar(c_[:, 1, :], c_[:, 3, :], -PI, ALU.is_lt), [c_], [c_])
        V(lambda e: e.scalar_tensor_tensor(c_[:, 2, :], c_[:, 1, :], 2 * PI, c_[:, 3, :], ALU.mult, ALU.add), [c_], [c_])
        A(lambda e: e.activation(c_[:, 4, :], c_[:, 2, :], AF.Sin), [c_], [c_])

        def qk_post(ps_, gb, is_q):
            i = cnt["b"] % NB
            cnt["b"] += 1
            a1, a2, sq_, rp_, qf_, qT_ = t1[i], t2[i], ssq[i], rp[i], qf[i], qTs[i]
            A(lambda e: e.activation(a1[:], ps_[:], AF.Square), [ps_], [a1])
            V(lambda e: e.tensor_reduce(sq_[:, 0:8], a1[:].rearrange("p (h d) -> p h d", h=8), AX.X, ALU.add), [a1], [sq_])
            V(lambda e: e.tensor_scalar(sq_[:, 0:8], sq_[:, 0:8], 1.0 / 64.0, 1e-6, ALU.mult, ALU.add), [sq_], [sq_])
            A(lambda e: e.activation(sq_[:, 8:16], sq_[:, 0:8], AF.Sqrt), [sq_], [sq_])
            V(lambda e: e.reciprocal(sq_[:, 8:16], sq_[:, 8:16]), [sq_], [sq_])
            V(lambda e: e.tensor_tensor(a2[:].rearrange("p (h d) -> p h d", h=8), ps_[:].rearrange("p (h d) -> p h d", h=8),
                                        bc(sq_[:, 8:16].unsqueeze(2), [128, 8, 64]), ALU.mult), [ps_, sq_], [a2])
            V(lambda e: e.tensor_tensor(a2[:].rearrange("p (h d) -> p h d", h=8), a2[:].rearrange("p (h d) -> p h d", h=8),
                                        bc(gb[:].unsqueeze(1), [128, 8, 64]), ALU.mult), [a2, gb], [a2])
            v3 = a2[:].rearrange("p (h d) -> p h d", h=8)
            x1, x2 = v3[:, :, 0:8], v3[:, :, 8:16]
            sinb = bc(c_[:, 4, 0:8].unsqueeze(1), [128, 8, 8])
            cosb = bc(c_[:, 4, 8:16].unsqueeze(1), [128, 8, 8])
            V(lambda e: e.tensor_tensor(rp_[:, 0], x1, cosb, ALU.mult), [a2, c_], [rp_])
            V(lambda e: e.tensor_tensor(rp_[:, 1], x2, sinb, ALU.mult), [a2, c_], [rp_])
            V(lambda e: e.tensor_tensor(rp_[:, 2], x2, cosb, ALU.mult), [a2, c_], [rp_])
            V(lambda e: e.tensor_tensor(rp_[:, 3], x1, sinb, ALU.mult), [a2, c_], [rp_])
            V(lambda e: e.tensor_tensor(x1, rp_[:, 0], rp_[:, 1], ALU.subtract), [rp_], [a2])
            V(lambda e: e.tensor_tensor(x2, rp_[:, 2], rp_[:, 3], ALU.add), [rp_], [a2])
            A(lambda e: e.copy(qf_[:], a2[:]), [a2], [qf_])
            for pr in range(4):
                PE(lambda e, pr=pr: e.transpose(psB[:, pr * 128:(pr + 1) * 128], qf_[:, pr * 128:(pr + 1) * 128], g.ident_b[:]),
                   [qf_, g.ident_b], [psB])
            V(lambda e: e.tensor_copy(qT_[:].rearrange("p c t -> p (c t)"), psB[:, 0:512]), [psB], [qT_])
            if is_q:
                to = t0 - OWN0 * 128
                kb.dma("sp", g.QT.t[:, :, to:to + 128].rearrange("c p t -> p c t"), qT_[:], [qT_], [g.QT], qT_)
            else:
                kb.dma("sp", g.KT.t[:, :, t0:t0 + 128].rearrange("c p t -> p c t"), qT_[:], [qT_], [g.KT], qT_)
                km_ = kmp[it % 2]
                for pr in range(4):
                    PE(lambda e, pr=pr: e.matmul(psX[:, 256 + pr:257 + pr], a2[:, pr * 128:(pr + 1) * 128], onesc[:],
                                                 start=True, stop=True), [a2, onesc], [psX])
                V(lambda e: e.tensor_copy(km_[:], psX[:, 256:260]), [psX], [km_])
                if it % 2 == 1:
                    V(lambda e: e.tensor_tensor(g.kmT[:, :, it // 2], kmp[0][:], kmp[1][:], ALU.add), [kmp[0], kmp[1]], [g.kmT])

        psl = nextM()
        mm(psl, 256, 3072, xn, prev_c0=1536)
        la_ = la[it % 2]
        laT_ = laT[it % 2]
        A(lambda e: e.activation(la_[:, 0:64], psl[:, 0:64], AF.Tanh), [psl], [la_])
        V(lambda e: e.tensor_copy(la_[:, 64:128], psl[:, 64:128]), [psl], [la_])
        A(lambda e: e.activation(la_[:, 128:256], psl[:, 128:256], AF.Sigmoid), [psl], [la_])
        PE(lambda e: e.transpose(psX[:, 0:128], la_[:, 0:128], g.ident_f[:]), [la_, g.ident_f], [psX])
        PE(lambda e: e.transpose(psX[:, 128:256], la_[:, 128:256], g.ident_f[:]), [la_, g.ident_f], [psX])
        V(lambda e: e.tensor_copy(laT_[:], psX[:, 0:256]), [psX], [laT_])
        pw = nextM()
        PE(lambda e: e.matmul(pw[:], laT_[0:64, 0:128], w2a2[0:64, :], start=True, stop=True), [laT_, w2a2], [pw])
        ld_ = nextS()
        V(lambda e: e.tensor_tensor(ld_[:], pw[:], w0b[:], ALU.add), [pw, w0b], [ld_])
        A(lambda e: e.activation(ld_[:], ld_[:], AF.Sigmoid), [ld_], [ld_])
        V(lambda e: e.tensor_scalar_mul(ld_[:], ld_[:], -0.6065306597126334), [ld_], [ld_])
        stream_store(1, ld_, t0)
        pa = nextM()
        PE(lambda e: e.matmul(pa[:], laT_[64:128, 0:128], w2a2[64:128, :], start=True, stop=True), [laT_, w2a2], [pa])
        a_ = av[it % 2]
        V(lambda e: e.tensor_tensor(a_[:], pa[:], a0b[:], ALU.add), [pa, a0b], [a_])
        A(lambda e: e.activation(a_[:], a_[:], AF.Sigmoid), [a_], [a_])
        if own:
            pg = nextM()
            PE(lambda e: e.matmul(pg[:], laT_[:, 128:256], g2t[:], start=True, stop=True), [laT_, g2t], [pg])
            gg = nextS()
            A(lambda e: e.copy(gg[:], pg[:]), [pg], [gg])
            to = t0 - OWN0 * 128
            kb.dma("sp", g.GR[to:to + 128, :], gg[:], [gg], [g.GR], gg)
        pr_ = nextM()
        mm(pr_, 512, 1536, xn, prev_c0=0)
        r_ = nextS()
        A(lambda e: e.copy(r_[:], pr_[:]), [pr_], [r_])
        stream_store(0, r_, t0)
        pk = nextM()
        mm(pk, 512, 2048, xn, prev_c0=512)
        i = cnt["b"] % NB
        cnt["b"] += 1
        a1, sq_ = t1[i], ssq[i]
        kkn = nextS()
        V(lambda e: e.tensor_tensor(kkn[:], pk[:], kkb[:], ALU.mult), [pk, kkb], [kkn])
        V(lambda e: e.tensor_tensor(a1[:], kkn[:], kkn[:], ALU.mult), [kkn], [a1])
        V(lambda e: e.tensor_reduce(sq_[:, 0:8], a1[:].rearrange("p (h d) -> p h d", h=8), AX.X, ALU.add), [a1], [sq_])
        V(lambda e: e.tensor_scalar_add(sq_[:, 0:8], sq_[:, 0:8], 1e-24), [sq_], [sq_])
        A(lambda e: e.activation(sq_[:, 8:16], sq_[:, 0:8], AF.Sqrt), [sq_], [sq_])
        V(lambda e: e.reciprocal(sq_[:, 8:16], sq_[:, 8:16]), [sq_], [sq_])
        V(lambda e: e.scalar_tensor_tensor(kkn[:].rearrange("p (h d) -> p h d", h=8), kkn[:].rearrange("p (h d) -> p h d", h=8), -1.0,
                                           bc(sq_[:, 8:16].unsqueeze(2), [128, 8, 64]), ALU.mult, ALU.mult), [kkn, sq_], [kkn])
        stream_store(4, kkn, t0)
        b_ = nextS()
        V(lambda e: e.scalar_tensor_tensor(b_[:], kkn[:], -1.0, a_[:], ALU.mult, ALU.mult), [kkn, a_], [b_])
        stream_store(5, b_, t0)
        k_ = nextS()
        V(lambda e: e.scalar_tensor_tensor(a1[:], a_[:], -1.0, kab[:], ALU.add, ALU.mult), [a_, kab], [a1])
        V(lambda e: e.scalar_tensor_tensor(k_[:], a1[:], 1.0, pk[:], ALU.add, ALU.mult), [a1, pk], [k_])
        stream_store(2, k_, t0)
        pv_ = nextM()
        mm(pv_, 512, 2560, xn, prev_c0=1024)
        v_ = nextS()
        if own:
            A(lambda e: e.copy(v_[:], pv_[:]), [pv_], [v_])
        else:
            V(lambda e: e.tensor_scalar_mul(v_[:], pv_[:], g.pv[:, 0:1]), [pv_, g.pv], [v_])
        stream_store(3, v_, t0)
        pmk = nextM()
        mm(pmk, 512, 512, xn)
        qk_post(pmk, kg, False)
        pmv = nextM()
        mm(pmv, 512, 1024, xn)
        vb = gsb[cnt["g"] % 3]
        cnt["g"] += 1
        A(lambda e: e.copy(vb[:], pmv[:]), [pmv], [vb])
        kb.dma("sp", g.VM[t0:t0 + 128, :], vb[:], [vb], [g.VM], vb)
        if own:
            pmq = nextM()
            mm(pmq, 512, 0, xn)
            qk_post(pmq, qg, True)
            to = t0 - OWN0 * 128
            for gi in range(4):
                pg_ = nextM()
                mm(pg_, 512, 3328 + gi * 512, xn)
                gb_ = gsb[cnt["g"] % 3]
                cnt["g"] += 1
                A(lambda e, gb_=gb_, pg_=pg_: e.activation(gb_[:], pg_[:], AF.Sigmoid), [pg_], [gb_])
                kb.dma("sp", g.GATES[to:to + 128, gi * 512:(gi + 1) * 512], gb_[:], [gb_], [g.GATES], gb_)
    for it in range(ntiles):
        tile_body(it)
    kb.end()


def phase_b(kb, g, nqb=16):
    kb.begin()
    V = lambda fn, r, w: kb.op("dve", fn, r, w)
    A = lambda fn, r, w: kb.op("act", fn, r, w)
    PE = lambda fn, r, w: kb.op("pe", fn, r, w)
    PL = lambda fn, r, w: kb.op("pool", fn, r, w)
    NKB = NT // 256
    QB0 = OWN0 // 2
    kmTb = kb.sbuf("kmTb", [128, 4, NKB], BF16)
    V(lambda e: e.tensor_copy(kmTb[:], g.kmT[:]), [g.kmT], [kmTb])
    pastm = kb.sbuf("pastm", [128, 16, NKB], F32)
    pfx = kb.sbuf("pfx", [128, 1], F32)
    PL(lambda e: e.memset(pastm[:], 0.0), [], [pastm])
    for qbl in range(16):
        PL(lambda e, qbl=qbl: e.memset(pastm[:, qbl, QB0 + qbl:NKB], -1e30), [], [pastm])
    V(lambda e: e.tensor_scalar(pfx[:], g.pv[:], -1.0, 1e30, ALU.add, ALU.mult), [g.pv], [pfx])
    V(lambda e: e.tensor_scalar(pastm[:, :, 0:QB0], pastm[:, :, 0:QB0], pfx[:, 0:1], None, ALU.add), [pastm, pfx], [pastm])
    tri = kb.sbuf("tri", [128, 2, 256], BF16)
    PL(lambda e: e.memset(tri[:], 1.0), [], [tri])
    for kc in range(2):
        PL(lambda e, kc=kc: e.affine_select(out=tri[:, kc, :], in_=tri[:, kc, :], pattern=[[1, 256]], compare_op=ALU.is_ge,
                                            fill=0.0, base=-kc * 128, channel_multiplier=-1), [tri], [tri])
    KTs = [kb.sbuf(f"KTs{i}", [128, NT], BF16) for i in range(2)]
    QTs = [kb.sbuf(f"QTs{i}", [128, NOWN], BF16) for i in range(2)]
    Vs = [kb.sbuf(f"Vs{i}", [128, NT // 128, 2, 65], BF16) for i in range(2)]
    for i in range(2):
        PL(lambda e, i=i: e.memset(Vs[i][:, :, :, 64:65], 1.0), [], [Vs[i]])
    sel = [kb.sbuf(f"sel{i}", [128, 32, 2, NKB], F32) for i in range(2)]
    gsm = [kb.sbuf(f"gsm{i}", [128, NKB], F32) for i in range(2)]
    g8 = [kb.sbuf(f"g8{i}", [128, 8], F32) for i in range(2)]
    m1 = [kb.sbuf(f"m1{i}", [128, NKB], F32) for i in range(2)]
    psS = [kb.psum(f"psS{i}", [128, 512], F32) for i in range(3)]
    psO = [kb.psum(f"psO{i}", [128, 2, 65], F32) for i in range(3)]
    psG = [kb.psum(f"psG{i}", [128, 64], F32) for i in range(2)]
    pts = [kb.sbuf(f"pts{i}", [128, 2, 256], BF16) for i in range(4)]
    acc = [kb.sbuf(f"acc{i}", [128, 2, 65], F32) for i in range(2)]
    rc = [kb.sbuf(f"rc{i}", [128, 2], F32) for i in range(2)]
    ob = [kb.sbuf(f"ob{i}", [128, 2, 64], BF16) for i in range(4)]
    cn = {"s": 0, "o": 0, "p": 0, "a": 0, "b": 0, "g": 0}
    vview = g.VM.t.rearrange("(c p) (h d) -> p c h d", p=128, d=64)

    def pair_body(pr):
        KT_, QT_, V_, sel_ = KTs[pr % 2], QTs[pr % 2], Vs[pr % 2], sel[pr % 2]
        kb.dma("sp", KT_[:], g.KT.t[pr], [g.KT], [KT_], KT_)
        kb.dma("sp", QT_[:], g.QT.t[pr], [g.QT], [QT_], QT_)
        for cq in range(4):
            c0 = cq * (NT // 512)
            c1 = c0 + NT // 512
            for h2 in range(2):
                kb.dma("sp", V_[:, c0:c1, h2, 0:64], vview[:, c0:c1, 2 * pr + h2, :], [g.VM], [V_], V_)
        for qt in range(2 * nqb):
            qbl = qt // 2
            for h2 in range(2):
                def selbody(qt=qt, qbl=qbl, h2=h2):
                    i = cn["g"] % 2
                    cn["g"] += 1
                    pg, gs_, g8_, m1_ = psG[i], gsm[i], g8[i], m1[i]
                    rows = slice(h2 * 64, (h2 + 1) * 64)
                    PE(lambda e: e.matmul(pg[:, 0:NKB], QT_[rows, qt * 128:(qt + 1) * 128], kmTb[rows, pr, :], start=True, stop=True),
                       [QT_, kmTb], [pg])
                    V(lambda e: e.tensor_tensor(gs_[:], pg[:, 0:NKB], pastm[:, qbl, :], ALU.add), [pg, pastm], [gs_])
                    V(lambda e: e.max(out=g8_[:], in_=gs_[:]), [gs_], [g8_])
                    V(lambda e: e.tensor_scalar(m1_[:], gs_[:], g8_[:, 2:3], None, ALU.is_ge), [gs_, g8_], [m1_])
                    V(lambda e: e.scalar_tensor_tensor(sel_[:, qt, h2, :], gs_[:], -1e29, m1_[:], ALU.is_gt, ALU.mult), [gs_, m1_], [sel_])
                    V(lambda e: e.memset(sel_[:, qt, h2, QB0 + qbl:QB0 + qbl + 1], 1.0), [], [sel_])
                selbody()
        for h2 in range(2):
            rows = slice(h2 * 64, (h2 + 1) * 64)
            for qbl in range(nqb):
                def qb_body(h2=h2, rows=rows, qbl=qbl):
                    qb = QB0 + qbl
                    acc_ = acc[cn["a"] % 2]
                    rc_ = rc[cn["a"] % 2]
                    cn["a"] += 1
                    for kblk in range(qb + 1):
                        def kb_body(kblk=kblk):
                            pS = psS[cn["s"] % 3]
                            cn["s"] += 1
                            pO = psO[cn["o"] % 3]
                            cn["o"] += 1
                            pt = pts[cn["p"] % 4]
                            cn["p"] += 1
                            for kc in range(2):
                                c = kblk * 2 + kc
                                PE(lambda e, kc=kc, c=c: e.matmul(pS[:, kc * 256:(kc + 1) * 256], KT_[rows, c * 128:(c + 1) * 128],
                                                                  QT_[rows, qbl * 256:(qbl + 1) * 256], start=True, stop=True),
                                   [KT_, QT_], [pS])
                            A(lambda e: e.activation(pt[:].rearrange("p a b -> p (a b)"), pS[:], AF.Exp, scale=0.125), [pS], [pt])
                            if kblk == qb:
                                V(lambda e: e.tensor_tensor(pt[:], pt[:], tri[:], ALU.mult), [pt, tri], [pt])
                            for qt in range(2):
                                for kc in range(2):
                                    c = kblk * 2 + kc
                                    PE(lambda e, qt=qt, kc=kc, c=c: e.matmul(pO[:, qt, :], pt[:, kc, qt * 128:(qt + 1) * 128], V_[:, c, h2, :],
                                                                             start=(kc == 0), stop=(kc == 1)), [pt, V_], [pO])
                            for qt in range(2):
                                sc = sel_[:, qbl * 2 + qt, h2, kblk:kblk + 1]
                                if kblk == 0:
                                    V(lambda e, qt=qt, sc=sc: e.tensor_scalar_mul(acc_[:, qt, :], pO[:, qt, :], sc), [pO, sel_], [acc_])
                                else:
                                    V(lambda e, qt=qt, sc=sc: e.scalar_tensor_tensor(acc_[:, qt, :], pO[:, qt, :], sc, acc_[:, qt, :],
                                                                                     ALU.mult, ALU.add), [pO, sel_, acc_], [acc_])
                        kb_body()
                    ob_ = ob[cn["b"] % 4]
                    cn["b"] += 1
                    V(lambda e: e.reciprocal(rc_[:], acc_[:, :, 64]), [acc_], [rc_])
                    for qt in range(2):
                        V(lambda e, qt=qt: e.tensor_scalar_mul(ob_[:, qt, :], acc_[:, qt, 0:64], rc_[:, qt:qt + 1]), [acc_, rc_], [ob_])
                    hh = pr * 2 + h2
                    dst = g.OM.t[qbl * 256:(qbl + 1) * 256, hh * 64:(hh + 1) * 64].rearrange("(a p) d -> p a d", p=128)
                    kb.dma("sp", dst, ob_[:], [ob_], [g.OM], ob_)
                qb_body()

    for pr in range(4):
        pair_body(pr)
    kb.end()


def phase_c(kb, g, nchunks=NT // 64):
    kb.begin()
    V = lambda fn, r, w: kb.op("dve", fn, r, w)
    A = lambda fn, r, w: kb.op("act", fn, r, w)
    PE = lambda fn, r, w: kb.op("pe", fn, r, w)
    PL = lambda fn, r, w: kb.op("pool", fn, r, w)
    I_ = g.ident_f
    Lbd = kb.sbuf("Lbd", [128, 128], F32)
    Msu = kb.sbuf("Msu", [128, 128], F32)
    Msl = kb.sbuf("Msl", [128, 128], F32)
    Obd = kb.sbuf("Obd", [128, 128], F32)
    ind2 = kb.sbuf("ind2", [128, 2], F32)
    for t_, op_ in ((Lbd, ALU.is_ge), (Msu, ALU.is_gt)):
        PL(lambda e, t_=t_: e.memset(t_[:], 1.0), [], [t_])
        PL(lambda e, t_=t_, op_=op_: e.affine_select(out=t_[:], in_=t_[:], pattern=[[1, 128]], compare_op=op_, fill=0.0,
                                                     base=0, channel_multiplier=-1), [t_], [t_])
        PL(lambda e, t_=t_: e.memset(t_[0:64, 64:128], 0.0), [], [t_])
    PL(lambda e: e.memset(Msl[:], 1.0), [], [Msl])
    PL(lambda e: e.affine_select(out=Msl[:], in_=Msl[:], pattern=[[-1, 128]], compare_op=ALU.is_gt, fill=0.0,
                                 base=0, channel_multiplier=1), [Msl], [Msl])
    PL(lambda e: e.memset(Msl[64:128, 0:64], 0.0), [], [Msl])
    PL(lambda e: e.memset(Obd[:], 0.0), [], [Obd])
    PL(lambda e: e.memset(Obd[0:64, 0:64], 1.0), [], [Obd])
    PL(lambda e: e.memset(Obd[64:128, 64:128], 1.0), [], [Obd])
    PL(lambda e: e.memset(ind2[:], 0.0), [], [ind2])
    PL(lambda e: e.memset(ind2[0:64, 0:1], 1.0), [], [ind2])
    PL(lambda e: e.memset(ind2[64:128, 1:2], 1.0), [], [ind2])
    cst = kb.sbuf("cst", [128, 3, 4, 64], F32)
    for hp in range(4):
        for h2 in range(2):
            hh = hp * 2 + h2
            for j, src in enumerate((g.rwkv_ln_g, g.rwkv_ln_b, g.rwkv_r_k)):
                kb.dma("sp", cst[h2 * 64:(h2 + 1) * 64, j, hp, :], src.t[:, hh * 64:(hh + 1) * 64].partition_broadcast(64),
                       [src], [cst], cst)
    ldb = [kb.sbuf(f"cld{i}", [128, 4, 6, 64], F32) for i in range(2)]
    gtb = [kb.sbuf(f"cgt{i}", [128, 4, 64], F32) for i in range(2)]
    E = kb.sbuf("cE", [128, 4, 4, 64], F32)
    X = kb.sbuf("cX", [128, 4, 4, 64], F32)
    TA = kb.sbuf("cTA", [128, 4, 4, 64], F32)
    BK = kb.sbuf("cBK", [128, 4, 2, 64], F32)
    Vbd = kb.sbuf("cVbd", [128, 4, 128], F32)
    Ubd = kb.sbuf("cUbd", [128, 4, 128], F32)
    TT = kb.sbuf("cTT", [64, 4, 4, 128], F32)
    AA = [kb.sbuf(f"cAA{i}", [128, 4, 2, 128], F32) for i in range(2)]
    AXm = kb.sbuf("cAX", [128, 4, 3, 128], F32)
    Y = [kb.sbuf(f"cY{i}", [128, 4, 128], F32) for i in range(2)]
    WT = kb.sbuf("cWT", [64, 4, 128], F32)
    PC = kb.sbuf("cPC", [64, 4, 2], F32)
    hs = [kb.sbuf(f"ch{i}", [64, 4, 128], F32) for i in range(2)]
    htmp = kb.sbuf("chtmp", [64, 4, 128], F32)
    O = kb.sbuf("cO", [128, 4, 64], F32)
    stt = kb.sbuf("cst2", [128, 8, 4], F32)
    t1 = kb.sbuf("ct1", [128, 4, 64], F32)
    t2 = kb.sbuf("ct2", [128, 4, 64], F32)
    obb = [kb.sbuf(f"cob{i}", [128, 4, 64], BF16) for i in range(2)]
    psA = kb.psum("cps", [128, 4, 2, 512], F32)

    class PB:
        def __init__(self, b):
            self.buf = Buf(None, f"cpsbank{b}")
            self.b = b

        def s(self, hp, sl, rows=slice(0, 128)):
            return psA.t[rows, hp, self.b, sl]

        def all(self, sl, rows=slice(0, 128)):
            return psA.t[rows, :, self.b, sl]
    P0, P1 = PB(0), PB(1)
    PL(lambda e: e.memset(Vbd[:], 0.0), [], [Vbd])
    PL(lambda e: e.memset(Ubd[:], 0.0), [], [Ubd])
    PL(lambda e: e.memset(hs[0][:], 0.0), [], [hs[0]])
    b4 = lambda m: bc(m[:].unsqueeze(1), [128, 4, 128])
    orw4 = g.ORW.t.rearrange("t (hp h2 n) -> t hp h2 n", hp=4, h2=2)
    gr4 = g.GR.t.rearrange("t (hp h2 n) -> t hp h2 n", hp=4, h2=2)

    def chunk(c):
        own = c >= (NT - NOWN) // 64
        ld = ldb[c % 2]
        gt = gtb[c % 2]
        hcur, hnew = hs[c % 2], hs[(c + 1) % 2]
        to = c * 64 - (NT - NOWN)
        for hp in range(4):
            for h2 in range(2):
                src = g.RS.t[:, hp * 2 + h2, c * 64:(c + 1) * 64, :].rearrange("s t n -> t s n")
                kb.dma("sp", ld[h2 * 64:(h2 + 1) * 64, hp, :, :], src, [g.RS], [ld], ld)
        if own:
            for h2 in range(2):
                kb.dma("sp", gt[h2 * 64:(h2 + 1) * 64, :, :], gr4[to:to + 64, :, h2, :], [g.GR], [gt], gt)
        for hp in range(4):
            PE(lambda e, hp=hp: e.matmul(P0.s(hp, slice(0, 64)), Lbd[:], ld[:, hp, 1, :], start=True, stop=True), [Lbd, ld], [P0.buf])
            PE(lambda e, hp=hp: e.matmul(P0.s(hp, slice(64, 128)), Obd[:], ld[:, hp, 1, :], start=True, stop=True), [Obd, ld], [P0.buf])
            PE(lambda e, hp=hp: e.matmul(P0.s(hp, slice(128, 130), slice(0, 64)), ld[:, hp, 1, :], ind2[:], start=True, stop=True),
               [ld, ind2], [P0.buf])
        V(lambda e: e.tensor_copy(E[:, :, 0, :], P0.all(slice(0, 64))), [P0.buf], [E])
        V(lambda e: e.tensor_scalar_mul(E[:, :, 1, :], P0.all(slice(0, 64)), -1.0), [P0.buf], [E])
        V(lambda e: e.tensor_tensor(E[:, :, 2, :], P0.all(slice(0, 64)), ld[:, :, 1, :], ALU.subtract), [P0.buf, ld], [E])
        V(lambda e: e.tensor_tensor(E[:, :, 3, :], P0.all(slice(64, 128)), E[:, :, 0, :], ALU.subtract), [P0.buf, E], [E])
        A(lambda e: e.activation(X[:], E[:], AF.Exp), [E], [X])
        A(lambda e: e.activation(PC[:], P0.all(slice(128, 130), slice(0, 64)), AF.Exp), [P0.buf], [PC])
        V(lambda e: e.tensor_tensor(TA[:, :, 0, :], ld[:, :, 4, :], X[:, :, 2, :], ALU.mult), [ld, X], [TA])
        V(lambda e: e.tensor_tensor(TA[:, :, 1, :], ld[:, :, 0, :], X[:, :, 0, :], ALU.mult), [ld, X], [TA])
        V(lambda e: e.tensor_tensor(TA[:, :, 2, :], ld[:, :, 5, :], X[:, :, 1, :], ALU.mult), [ld, X], [TA])
        V(lambda e: e.tensor_tensor(TA[:, :, 3, :], ld[:, :, 2, :], X[:, :, 1, :], ALU.mult), [ld, X], [TA])
        PL(lambda e: e.tensor_tensor(BK[:, :, 0, :], ld[:, :, 5, :], X[:, :, 3, :], ALU.mult), [ld, X], [BK])
        PL(lambda e: e.tensor_tensor(BK[:, :, 1, :], ld[:, :, 2, :], X[:, :, 3, :], ALU.mult), [ld, X], [BK])
        PL(lambda e: e.tensor_copy(Vbd[0:64, :, 0:64], ld[0:64, :, 3, :]), [ld], [Vbd])
        PL(lambda e: e.tensor_copy(Vbd[64:128, :, 64:128], ld[64:128, :, 3, :]), [ld], [Vbd])
        for hp in range(4):
            for j in range(4):
                PE(lambda e, hp=hp, j=j: e.transpose(P1.s(hp, slice(j * 128, (j + 1) * 128), slice(0, 64)), TA[:, hp, j, :], I_[:]),
                   [TA, I_], [P1.buf])
        A(lambda e: e.copy(TT[:].rearrange("p a j t -> p a (j t)"), P1.all(slice(0, 512), slice(0, 64))), [P1.buf], [TT])
        for hp in range(4):
            AtT, RtT, BtT, KtT = TT[:, hp, 0, :], TT[:, hp, 1, :], TT[:, hp, 2, :], TT[:, hp, 3, :]
            PE(lambda e, hp=hp, a=AtT, b=BtT: e.matmul(P0.s(hp, slice(0, 128)), a, b, start=True, stop=True), [TT], [P0.buf])
            PE(lambda e, hp=hp, a=BtT, b=AtT: e.matmul(P0.s(hp, slice(128, 256)), a, b, start=True, stop=True), [TT], [P0.buf])
            PE(lambda e, hp=hp, a=KtT, b=AtT: e.matmul(P0.s(hp, slice(256, 384)), a, b, start=True, stop=True), [TT], [P0.buf])
            PE(lambda e, hp=hp, a=BtT, b=RtT: e.matmul(P0.s(hp, slice(384, 512)), a, b, start=True, stop=True), [TT], [P0.buf])
            PE(lambda e, hp=hp, a=KtT, b=RtT: e.matmul(P1.s(hp, slice(0, 128)), a, b, start=True, stop=True), [TT], [P1.buf])
        V(lambda e: e.tensor_tensor(AA[0][:, :, 0, :], P0.all(slice(0, 128)), b4(Msl), ALU.mult), [P0.buf, Msl], [AA[0]])
        V(lambda e: e.tensor_tensor(AA[0][:, :, 1, :], P0.all(slice(128, 256)), b4(Msu), ALU.mult), [P0.buf, Msu], [AA[0]])
        V(lambda e: e.tensor_tensor(AXm[:, :, 0, :], P0.all(slice(256, 384)), b4(Msu), ALU.mult), [P0.buf, Msu], [AXm])
        V(lambda e: e.tensor_tensor(AXm[:, :, 1, :], P0.all(slice(384, 512)), b4(Lbd), ALU.mult), [P0.buf, Lbd], [AXm])
        V(lambda e: e.tensor_tensor(AXm[:, :, 2, :], P1.all(slice(0, 128)), b4(Lbd), ALU.mult), [P1.buf, Lbd], [AXm])
        for hp in range(4):
            PE(lambda e, hp=hp: e.matmul(P1.s(hp, slice(128, 192)), AXm[:, hp, 0, :], ld[:, hp, 3, :], start=True, stop=True), [AXm, ld], [P1.buf])
        A(lambda e: e.copy(Y[0][:, :, 0:64], TA[:, :, 0, :]), [TA], [Y[0]])
        A(lambda e: e.copy(Y[0][:, :, 64:128], P1.all(slice(128, 192))), [P1.buf], [Y[0]])
        for lev in range(6):
            a, b = lev % 2, (lev + 1) % 2
            pp = P0 if lev % 2 == 0 else P1
            for hp in range(4):
                PE(lambda e, hp=hp, a=a, pp=pp: e.matmul(pp.s(hp, slice(0, 128)), AA[a][:, hp, 1, :], Y[a][:, hp, :], start=True, stop=True),
                   [AA[a], Y[a]], [pp.buf])
            V(lambda e, a=a, b=b, pp=pp: e.tensor_tensor(Y[b][:], pp.all(slice(0, 128)), Y[a][:], ALU.add), [pp.buf, Y[a]], [Y[b]])
            if lev < 5:
                for hp in range(4):
                    PE(lambda e, hp=hp, a=a, pp=pp: e.matmul(pp.s(hp, slice(128, 256)), AA[a][:, hp, 1, :], AA[a][:, hp, 0, :], start=True, stop=True),
                       [AA[a]], [pp.buf])
                    PE(lambda e, hp=hp, a=a, pp=pp: e.matmul(pp.s(hp, slice(256, 384)), AA[a][:, hp, 0, :], AA[a][:, hp, 1, :], start=True, stop=True),
                       [AA[a]], [pp.buf])
                A(lambda e, b=b, pp=pp: e.copy(AA[b][:].rearrange("p h a t -> p h (a t)"), pp.all(slice(128, 384))), [pp.buf], [AA[b]])
        Xf = Y[0]
        for hp in range(4):
            PE(lambda e, hp=hp: e.transpose(P0.s(hp, slice(0, 128), slice(0, 64)), Xf[:, hp, 0:64], I_[:]), [Xf, I_], [P0.buf])
        A(lambda e: e.copy(WT[:], P0.all(slice(0, 128), slice(0, 64))), [P0.buf], [WT])
        for hp in range(4):
            PE(lambda e, hp=hp: e.matmul(P0.s(hp, slice(128, 256)), WT[:, hp, :], hcur[:, hp, :], start=True, stop=True), [WT, hcur], [P0.buf])
        V(lambda e: e.tensor_tensor(Ubd[0:64, :, 0:64], P0.all(slice(128, 192), slice(0, 64)), Xf[0:64, :, 64:128], ALU.add), [P0.buf, Xf], [Ubd])
        V(lambda e: e.tensor_tensor(Ubd[64:128, :, 64:128], P0.all(slice(192, 256), slice(64, 128)), Xf[64:128, :, 64:128], ALU.add),
          [P0.buf, Xf], [Ubd])
        if own:
            for hp in range(4):
                PE(lambda e, hp=hp: e.matmul(P1.s(hp, slice(0, 128)), TT[:, hp, 1, :], hcur[:, hp, :], start=True, stop=False), [TT, hcur], [P1.buf])
                PE(lambda e, hp=hp: e.matmul(P1.s(hp, slice(0, 128)), AXm[:, hp, 1, :], Ubd[:, hp, :], start=False, stop=False), [AXm, Ubd], [P1.buf])
                PE(lambda e, hp=hp: e.matmul(P1.s(hp, slice(0, 128)), AXm[:, hp, 2, :], Vbd[:, hp, :], start=False, stop=True), [AXm, Vbd], [P1.buf])
            A(lambda e: e.copy(O[0:64, :, :], P1.all(slice(0, 64), slice(0, 64))), [P1.buf], [O])
            A(lambda e: e.copy(O[64:128, :, :], P1.all(slice(64, 128), slice(64, 128))), [P1.buf], [O])
        for hp in range(4):
            PE(lambda e, hp=hp: e.matmul(P0.s(hp, slice(256, 384), slice(0, 64)), BK[:, hp, 0, :], Ubd[:, hp, :], start=True, stop=False),
               [BK, Ubd], [P0.buf])
            PE(lambda e, hp=hp: e.matmul(P0.s(hp, slice(256, 384), slice(0, 64)), BK[:, hp, 1, :], Vbd[:, hp, :], start=False, stop=True),
               [BK, Vbd], [P0.buf])
        V(lambda e: e.tensor_tensor(htmp[:].rearrange("p h (a v) -> p h a v", a=2), hcur[:].rearrange("p h (a v) -> p h a v", a=2),
                                    bc(PC[:].unsqueeze(3), [64, 4, 2, 64]), ALU.mult), [hcur, PC], [htmp])
        V(lambda e: e.tensor_tensor(hnew[:], htmp[:], P0.all(slice(256, 384), slice(0, 64)), ALU.add), [htmp, P0.buf], [hnew])
        if not own:
            return
        ob = obb[c % 2]
        b64 = lambda ap: bc(ap.unsqueeze(2), [128, 4, 64])
        V(lambda e: e.tensor_reduce(stt[:, 0, :], O[:], AX.X, ALU.add), [O], [stt])
        V(lambda e: e.tensor_scalar_mul(stt[:, 1, :], stt[:, 0, :], 1.0 / 64.0), [stt], [stt])
        V(lambda e: e.tensor_tensor(t1[:], O[:], b64(stt[:, 1, :]), ALU.subtract), [O, stt], [t1])
        A(lambda e: e.activation(t2[:], t1[:], AF.Square), [t1], [t2])
        V(lambda e: e.tensor_reduce(stt[:, 2, :], t2[:], AX.X, ALU.add), [t2], [stt])
        V(lambda e: e.tensor_scalar(stt[:, 3, :], stt[:, 2, :], 1.0 / 64.0, GN_EPS_, ALU.mult, ALU.add), [stt], [stt])
        A(lambda e: e.activation(stt[:, 4, :], stt[:, 3, :], AF.Sqrt), [stt], [stt])
        V(lambda e: e.reciprocal(stt[:, 4, :], stt[:, 4, :]), [stt], [stt])
        V(lambda e: e.tensor_tensor(t1[:], t1[:], b64(stt[:, 4, :]), ALU.mult), [t1, stt], [t1])
        V(lambda e: e.tensor_tensor(t1[:], t1[:], cst[:, 0], ALU.mult), [t1, cst], [t1])
        V(lambda e: e.tensor_tensor(t1[:], t1[:], cst[:, 1], ALU.add), [t1, cst], [t1])
        PL(lambda e: e.tensor_tensor(t2[:], ld[:, :, 0, :], ld[:, :, 2, :], ALU.mult), [ld], [t2])
        PL(lambda e: e.tensor_tensor(t2[:], t2[:], cst[:, 2], ALU.mult), [t2, cst], [t2])
        V(lambda e: e.tensor_reduce(stt[:, 5, :], t2[:], AX.X, ALU.add), [t2], [stt])
        V(lambda e: e.tensor_tensor(t2[:], ld[:, :, 3, :], b64(stt[:, 5, :]), ALU.mult), [ld, stt], [t2])
        V(lambda e: e.tensor_tensor(t1[:], t1[:], t2[:], ALU.add), [t1, t2], [t1])
        V(lambda e: e.tensor_tensor(ob[:], t1[:], gt[:], ALU.mult), [t1, gt], [ob])
        for h2 in range(2):
            kb.dma("sp", orw4[to:to + 64, :, h2, :], ob[h2 * 64:(h2 + 1) * 64, :, :], [ob], [g.ORW], ob)

    for c in range(nchunks):
        chunk(c)
    kb.end()


GN_EPS_ = 64e-5


def phase_p(kb, g, nblk=128):
    kb.begin()
    V = lambda fn, r, w: kb.op("dve", fn, r, w)
    A = lambda fn, r, w: kb.op("act", fn, r, w)
    PE = lambda fn, r, w: kb.op("pe", fn, r, w)
    PL = lambda fn, r, w: kb.op("pool", fn, r, w)
    uf = [kb.sbuf(f"uf{i}", [128, 1024], F32) for i in range(3)]
    ub = [kb.sbuf(f"ub{i}", [128, 1024], BF16) for i in range(3)]
    ut = [kb.sbuf(f"ut{i}", [128, 8, 128], BF16) for i in range(3)]
    vf = [kb.sbuf(f"vf{i}", [128, 1024], F32) for i in range(3)]
    vb = [kb.sbuf(f"vb{i}", [128, 1024], BF16) for i in range(3)]
    pb = [kb.psum(f"ppb{i}", [128, 1024], BF16) for i in range(3)]

    def body(b):
        i = b % 3
        kb.dma("sp", uf[i][:], g.peer_u[b * 128:(b + 1) * 128, :], [g.peer_u], [uf[i]], uf[i])
        kb.dma("sp", vf[i][:], g.peer_v[b * 128:(b + 1) * 128, :], [g.peer_v], [vf[i]], vf[i])
        V(lambda e: e.tensor_copy(ub[i][:], uf[i][:]), [uf[i]], [ub[i]])
        PL(lambda e: e.tensor_copy(vb[i][:], vf[i][:]), [vf[i]], [vb[i]])
        kb.dma("sp", g.VB[b * 128:(b + 1) * 128, :], vb[i][:], [vb[i]], [g.VB], vb[i])
        for k in range(8):
            PE(lambda e, k=k: e.transpose(pb[i][:, k * 128:(k + 1) * 128], ub[i][:, k * 128:(k + 1) * 128], g.ident_b[:]),
               [ub[i], g.ident_b], [pb[i]])
        A(lambda e: e.copy(ut[i][:].rearrange("p k e -> p (k e)"), pb[i][:]), [pb[i]], [ut[i]])
        kb.dma("sp", g.UT.t[:, :, b * 128:(b + 1) * 128].rearrange("k p e -> p k e"), ut[i][:], [ut[i]], [g.UT], ut[i])
    for b in range(nblk):
        body(b)
    kb.end()


def phase_d(kb, g, ntl=NOWN // 128):
    kb.begin()
    V = lambda fn, r, w: kb.op("dve", fn, r, w)
    A = lambda fn, r, w: kb.op("act", fn, r, w)
    PE = lambda fn, r, w: kb.op("pe", fn, r, w)
    PL = lambda fn, r, w: kb.op("pool", fn, r, w)
    Wpm = kb.sbuf("Wpm", [128, 4, 1024], BF16)
    Wpr = kb.sbuf("Wpr", [128, 4, 1024], BF16)
    Wo = kb.sbuf("Wo", [128, 8, 1024], BF16)
    Wq = kb.sbuf("Wq", [128, 8, 2048], BF16)
    skT = kb.sbuf("skT", [128, 16, 128], BF16)
    kb.begin()
    stg = [kb.sbuf(f"dstg{i}", [128, 4, 1024], F32) for i in range(2)]
    psk = kb.psum("psk", [128, 512], F32)
    n = [0]

    def ldw(dst, src, k0, nk, c0, nc_):
        s_ = stg[n[0] % 2]
        n[0] += 1
        kb.dma("sp", s_[:, 0:nk, 0:nc_], src.t.rearrange("(k p) n -> p k n", p=128)[:, k0:k0 + nk, c0:c0 + nc_], [src], [s_], s_)
        if n[0] % 2:
            V(lambda e: e.tensor_copy(dst[:, k0:k0 + nk, c0:c0 + nc_], s_[:, 0:nk, 0:nc_]), [s_], [dst])
        else:
            A(lambda e: e.copy(dst[:, k0:k0 + nk, c0:c0 + nc_], s_[:, 0:nk, 0:nc_]), [s_], [dst])
    ldw(Wpm, g.w_proj_moba, 0, 4, 0, 1024)
    ldw(Wpr, g.w_proj_rwkv, 0, 4, 0, 1024)
    ldw(Wo, g.w_out, 0, 4, 0, 1024)
    ldw(Wo, g.w_out, 4, 4, 0, 1024)
    for k0 in (0, 4):
        for c0 in (0, 1024):
            ldw(Wq, g.peer_wq, k0, 4, c0, 1024)
    for hp in range(16):
        s_ = stg[n[0] % 2]
        n[0] += 1
        kb.dma("sp", s_[:, 0, 0:128], g.peer_sk[hp], [g.peer_sk], [s_], s_)
        PE(lambda e, s_=s_: e.transpose(psk[:, 0:128], s_[:, 0, 0:128], g.ident_f[:]), [s_, g.ident_f], [psk])
        V(lambda e, hp=hp: e.tensor_copy(skT[:, hp, :], psk[:, 0:128]), [psk], [skT])
    kb.end()

    NB = 2
    om = [kb.sbuf(f"om{i}", [128, 2, 512], BF16) for i in range(NB)]
    gts = [kb.sbuf(f"gts{i}", [128, 2048], BF16) for i in range(NB)]
    xo = [kb.sbuf(f"xo{i}", [128, 1024], F32) for i in range(NB)]
    oT = [kb.sbuf(f"oT{i}", [128, 8, 128], BF16) for i in range(NB)]
    m1 = [kb.sbuf(f"dm1{i}", [128, 1024], F32) for i in range(NB)]
    mix = [kb.sbuf(f"mix{i}", [128, 1024], BF16) for i in range(NB)]
    mixT = [kb.sbuf(f"mixT{i}", [128, 8, 128], BF16) for i in range(NB)]
    h1 = [kb.sbuf(f"h1{i}", [128, 1024], F32) for i in range(NB)]
    junk = kb.sbuf("djunk", [128, 1024], BF16)
    stt = [kb.sbuf(f"dstt{i}", [128, 4], F32) for i in range(NB)]
    xn2T = [kb.sbuf(f"xn2T{i}", [128, 8, 128], BF16) for i in range(NB)]
    qT = [kb.sbuf(f"qT{i}", [128, 16, 128], BF16) for i in range(NB)]
    S = [kb.sbuf(f"S{i}", [128, 16, 128], F32) for i in range(NB)]
    S2 = [kb.sbuf(f"S2{i}", [128, 128], F32) for i in range(NB)]
    top = [kb.sbuf(f"top{i}", [128, 16, 16], F32) for i in range(NB)]
    cand = [kb.sbuf(f"cand{i}", [128, 8, 256], F32) for i in range(NB)]
    c2 = [kb.sbuf(f"c2{i}", [128, 256], F32) for i in range(NB)]
    t16 = [kb.sbuf(f"t16{i}", [128, 8, 16], F32) for i in range(NB)]
    thr = [kb.sbuf(f"thr{i}", [128, 4, 8], F32) for i in range(NB)]
    pB = [kb.psum(f"dpB{i}", [128, 1024], BF16) for i in range(2)]
    pM = [kb.psum(f"dpM{i}", [128, 512], F32) for i in range(4)]
    pT = [kb.psum(f"dpT{i}", [128, 512], F32) for i in range(2)]
    cn = {"m": 0}

    def nM():
        cn["m"] += 1
        return pM[cn["m"] % 4]

    def body(it):
        i = it % NB
        t0 = it * 128
        om_, g_, x_, oT_, m1_, mix_, mixT_, h1_, st_, xn_, qT_, S_, S2_, top_, cand_, c2_, t16_, thr_ = (
            om[i], gts[i], xo[i], oT[i], m1[i], mix[i], mixT[i], h1[i], stt[i], xn2T[i], qT[i], S[i], S2[i], top[i], cand[i],
            c2[i], t16[i], thr[i])
        kb.dma("sp", om_[:, 0, :], g.OM[t0:t0 + 128, :], [g.OM], [om_], om_)
        kb.dma("sp", om_[:, 1, :], g.ORW[t0:t0 + 128, :], [g.ORW], [om_], om_)
        kb.dma("sp", g_[:], g.GATES[t0:t0 + 128, :], [g.GATES], [g_], g_)
        kb.dma("sp", x_[:], g.xs[NT - NOWN + t0:NT - NOWN + t0 + 128, :], [g.xs], [x_], x_)
        pb = pB[it % 2]
        for j in range(8):
            PE(lambda e, j=j: e.transpose(pb[:, j * 128:(j + 1) * 128], om_[:, j // 4, (j % 4) * 128:(j % 4 + 1) * 128], g.ident_b[:]),
               [om_, g.ident_b], [pb])
        A(lambda e: e.copy(oT_[:].rearrange("p k t -> p (k t)"), pb[:]), [pb], [oT_])
        for br, W_ in ((0, Wpm), (1, Wpr)):
            for hf in range(2):
                p_ = nM()
                for k in range(4):
                    PE(lambda e, k=k, p_=p_, W_=W_, br=br, hf=hf: e.matmul(p_[:], oT_[:, br * 4 + k, :], W_[:, k, hf * 512:(hf + 1) * 512],
                                                                           start=(k == 0), stop=(k == 3)), [oT_, W_], [p_])
                cs_ = slice(hf * 512, (hf + 1) * 512)
                gs_ = slice(br * 1024 + hf * 512, br * 1024 + (hf + 1) * 512)
                if br == 0:
                    V(lambda e, p_=p_, cs_=cs_, gs_=gs_: e.tensor_tensor(m1_[:, cs_], p_[:], g_[:, gs_], ALU.mult), [p_, g_], [m1_])
                else:
                    V(lambda e, p_=p_, cs_=cs_, gs_=gs_: e.tensor_tensor(h1_[:, cs_], p_[:], g_[:, gs_], ALU.mult), [p_, g_], [h1_])
                    PL(lambda e, cs_=cs_: e.tensor_tensor(mix_[:, cs_], m1_[:, cs_], h1_[:, cs_], ALU.add), [m1_, h1_], [mix_])
        pb2 = pB[(it + 1) % 2]
        for j in range(8):
            PE(lambda e, j=j: e.transpose(pb2[:, j * 128:(j + 1) * 128], mix_[:, j * 128:(j + 1) * 128], g.ident_b[:]),
               [mix_, g.ident_b], [pb2])
        A(lambda e: e.copy(mixT_[:].rearrange("p k t -> p (k t)"), pb2[:]), [pb2], [mixT_])
        for hf in range(2):
            p_ = nM()
            for k in range(8):
                PE(lambda e, k=k, p_=p_, hf=hf: e.matmul(p_[:], mixT_[:, k, :], Wo[:, k, hf * 512:(hf + 1) * 512],
                                                         start=(k == 0), stop=(k == 7)), [mixT_, Wo], [p_])
            cs_ = slice(hf * 512, (hf + 1) * 512)
            V(lambda e, p_=p_, cs_=cs_: e.tensor_tensor(h1_[:, cs_], p_[:], g.gt_bc[:, 0, cs_], ALU.mult), [p_, g.gt_bc], [h1_])
            V(lambda e, cs_=cs_: e.tensor_tensor(h1_[:, cs_], h1_[:, cs_], x_[:, cs_], ALU.add), [h1_, x_], [h1_])
        kb.dma("sp", g.H1[t0:t0 + 128, :], h1_[:], [h1_], [g.H1], h1_)
        A(lambda e: e.activation(junk[:], h1_[:], AF.Square, accum_out=st_[:, 0:1]), [h1_], [junk, st_])
        V(lambda e: e.tensor_scalar(st_[:, 1:2], st_[:, 0:1], 1.0 / 1024.0, 1e-6, ALU.mult, ALU.add), [st_], [st_])
        A(lambda e: e.activation(st_[:, 2:3], st_[:, 1:2], AF.Sqrt), [st_], [st_])
        V(lambda e: e.reciprocal(st_[:, 2:3], st_[:, 2:3]), [st_], [st_])
        V(lambda e: e.tensor_scalar_mul(m1_[:], h1_[:], st_[:, 2:3]), [h1_, st_], [m1_])
        for k in range(8):
            pt = pT[k // 4]
            PE(lambda e, k=k, pt=pt: e.transpose(pt[:, (k % 4) * 128:(k % 4 + 1) * 128], m1_[:, k * 128:(k + 1) * 128], g.ident_f[:]),
               [m1_, g.ident_f], [pt])
            A(lambda e, k=k, pt=pt: e.activation(xn_[:, k, :], pt[:, (k % 4) * 128:(k % 4 + 1) * 128], AF.Identity,
                                                 bias=g.modc[:, 24 + k:25 + k], scale=g.sc2p[:, k:k + 1]), [pt, g.modc, g.sc2p], [xn_])
        kb.dma("sp", g.XN2T.t[:, :, t0:t0 + 128].rearrange("k p t -> p k t"), xn_[:], [xn_], [g.XN2T], xn_)
        for q4 in range(4):
            p_ = nM()
            for jj in range(4):
                hp = q4 * 4 + jj
                for k in range(8):
                    PE(lambda e, k=k, p_=p_, hp=hp, jj=jj: e.matmul(p_[:, jj * 128:(jj + 1) * 128], Wq[:, k, hp * 128:(hp + 1) * 128], xn_[:, k, :],
                                                                  start=(k == 0), stop=(k == 7)), [Wq, xn_], [p_])
            A(lambda e, p_=p_, q4=q4: e.copy(qT_[:, q4 * 4:(q4 + 1) * 4, :].rearrange("p a t -> p (a t)"), p_[:]), [p_], [qT_])
        for q4 in range(4):
            p_ = nM()
            for jj in range(4):
                hp = q4 * 4 + jj
                PE(lambda e, p_=p_, hp=hp, jj=jj: e.matmul(p_[:, jj * 128:(jj + 1) * 128], qT_[:, hp, :], skT[:, hp, :], start=True, stop=True),
                   [qT_, skT], [p_])
            V(lambda e, p_=p_, q4=q4: e.tensor_copy(S_[:, q4 * 4:(q4 + 1) * 4, :].rearrange("p a n -> p (a n)"), p_[:]), [p_], [S_])
        kb.dma("sp", g.SC[t0:t0 + 128, :, :], S_[:], [S_], [g.SC], S_)
        for hp in range(16):
            V(lambda e, hp=hp: e.max(out=top_[:, hp, 0:8], in_=S_[:, hp, :]), [S_], [top_])
            V(lambda e, hp=hp: e.match_replace(out=S2_[:], in_to_replace=top_[:, hp, 0:8], in_values=S_[:, hp, :], imm_value=-1e30),
              [S_, top_], [S2_])
            V(lambda e, hp=hp: e.max(out=top_[:, hp, 8:16], in_=S2_[:]), [S2_], [top_])
        t4 = top_[:].rearrange("p (h two) a -> p h two a", two=2)
        V(lambda e: e.tensor_tensor(cand_[:].rearrange("p h (a b) -> p h a b", b=16), bc(t4[:, :, 0, :].unsqueeze(3), [128, 8, 16, 16]),
                                    bc(t4[:, :, 1, :].unsqueeze(2), [128, 8, 16, 16]), ALU.add), [top_], [cand_])
        for h in range(8):
            V(lambda e, h=h: e.max(out=t16_[:, h, 0:8], in_=cand_[:, h, :]), [cand_], [t16_])
            V(lambda e, h=h: e.match_replace(out=c2_[:], in_to_replace=t16_[:, h, 0:8], in_values=cand_[:, h, :], imm_value=-1e30),
              [cand_, t16_], [c2_])
            V(lambda e, h=h: e.max(out=t16_[:, h, 8:16], in_=c2_[:]), [c2_], [t16_])
        V(lambda e: e.tensor_copy(thr_[:, 0, :], t16_[:, :, 15]), [t16_], [thr_])
        V(lambda e: e.tensor_scalar_mul(thr_[:, 1, :], t16_[:, :, 0], -1.0), [t16_], [thr_])
        V(lambda e: e.tensor_tensor(t16_[:], t16_[:], bc(thr_[:, 1, :].unsqueeze(2), [128, 8, 16]), ALU.add), [t16_, thr_], [t16_])
        A(lambda e: e.activation(t16_[:], t16_[:], AF.Exp), [t16_], [t16_])
        V(lambda e: e.tensor_reduce(thr_[:, 3, :], t16_[:], AX.X, ALU.add), [t16_], [thr_])
        V(lambda e: e.reciprocal(thr_[:, 2, :], thr_[:, 3, :]), [thr_], [thr_])
        kb.dma("sp", g.TH[t0:t0 + 128, :, :], thr_[:], [thr_], [g.TH], thr_)
    for it in range(ntl):
        body(it)
    kb.end()


def phase_e(kb, g, ngroups=NOWN // 256, nec=16):
    kb.begin()
    V = lambda fn, r, w: kb.op("dve", fn, r, w)
    A = lambda fn, r, w: kb.op("act", fn, r, w)
    PE = lambda fn, r, w: kb.op("pe", fn, r, w)
    PL = lambda fn, r, w: kb.op("pool", fn, r, w)
    UTs = [kb.sbuf(f"UTs{i}", [128, 8, 1024], BF16) for i in range(2)]
    VBs = [kb.sbuf(f"VBs{i}", [128, 8, 1024], BF16) for i in range(2)]
    xn = [kb.sbuf(f"exn{i}", [128, 8, 256], BF16) for i in range(2)]
    Ssb = [kb.sbuf(f"eS{i}", [128, 2, 16, 128], F32) for i in range(1)]
    th = [kb.sbuf(f"eth{i}", [128, 2, 4, 8], F32) for i in range(2)]
    Dg = [kb.sbuf(f"eDg{i}", [128, 2, 8, 128], BF16) for i in range(2)]
    Zt = [kb.sbuf(f"eZ{i}", [128, 1024], F32) for i in range(3)]
    Et = [kb.sbuf(f"eE{i}", [128, 1024], BF16) for i in range(3)]
    Mt = [[[kb.sbuf(f"eM{b}_{tt}_{h}", [128, 1024], BF16) for h in range(8)] for tt in range(2)] for b in range(2)]
    gl = [kb.sbuf(f"egl{i}", [128, 256], BF16) for i in range(3)]
    hd = [kb.sbuf(f"ehd{i}", [128, 256], BF16) for i in range(3)]
    h1t = [kb.sbuf(f"eh1{i}", [128, 2, 1024], F32) for i in range(1)]
    fo = [kb.sbuf(f"efo{i}", [128, 1024], F32) for i in range(2)]
    pO = [kb.psum(f"epO{i}", [128, 512], F32) for i in range(4)]
    pS = [kb.psum(f"epS{i}", [128, 512], F32) for i in range(2)]
    pG = [kb.psum(f"epG{i}", [128, 512], F32) for i in range(2)]
    cn = {"z": 0, "w": 0, "s": 0, "g": 0, "h": 0, "m": 0}
    utv = g.UT.t.rearrange("k p e -> p k e")
    vbv = g.VB.t.rearrange("(b p) d -> p b d", p=128)

    def group(tg):
        i = tg % 2
        t0 = tg * 256
        xn_, S_, th_, Dg_, h1_ = xn[i], Ssb[0], th[i], Dg[i], h1t[0]
        kb.dma("sp", xn_[:], g.XN2T.t[:, :, t0:t0 + 256].rearrange("k p t -> p k t"), [g.XN2T], [xn_], xn_)
        for tt in range(2):
            kb.dma("sp", S_[:, tt], g.SC[t0 + tt * 128:t0 + (tt + 1) * 128, :, :], [g.SC], [S_], S_)
            kb.dma("sp", th_[:, tt], g.TH[t0 + tt * 128:t0 + (tt + 1) * 128, :, :], [g.TH], [th_], th_)
            kb.dma("sp", h1_[:, tt, :], g.H1[t0 + tt * 128:t0 + (tt + 1) * 128, :], [g.H1], [h1_], h1_)
            for h in range(8):
                V(lambda e, tt=tt, h=h: e.tensor_scalar_mul(Dg_[:, tt, h, :], g.ident_f[:], th_[:, tt, 2, h:h + 1]), [g.ident_f, th_], [Dg_])
        for ec in range(nec):
            def chunk(ec=ec):
                w = cn["w"] % 2
                cn["w"] += 1
                U_, Vb_ = UTs[w], VBs[w]
                kb.dma("sp", U_[:], utv[:, :, ec * 1024:(ec + 1) * 1024], [g.UT], [U_], U_)
                kb.dma("sp", Vb_[:], vbv[:, ec * 8:(ec + 1) * 8, :], [g.VB], [Vb_], Vb_)
                mb = cn["m"] % 2
                cn["m"] += 1
                for tt in range(2):
                    for h in range(8):
                        def gate(tt=tt, h=h):
                            z = cn["z"] % 3
                            cn["z"] += 1
                            Z_, E_, M_ = Zt[z], Et[z], Mt[mb][tt][h]
                            PL(lambda e: e.tensor_tensor(Z_[:].rearrange("p (a b) -> p a b", b=128),
                                                        bc(S_[:, tt, 2 * h, ec * 8:(ec + 1) * 8].unsqueeze(2), [128, 8, 128]),
                                                        bc(S_[:, tt, 2 * h + 1, :].unsqueeze(1), [128, 8, 128]), ALU.add), [S_], [Z_])
                            A(lambda e: e.activation(E_[:], Z_[:], AF.Exp, bias=th_[:, tt, 1, h:h + 1]), [Z_, th_], [E_])
                            V(lambda e: e.scalar_tensor_tensor(M_[:], Z_[:], th_[:, tt, 0, h:h + 1], E_[:], ALU.is_ge, ALU.mult),
                               [Z_, th_, E_], [M_])
                        gate()
                for ib in range(8):
                    def blk(ib=ib):
                        eb = ec * 8 + ib
                        ps_ = pS[cn["s"] % 2]
                        pg_ = pG[cn["g"] % 2]
                        cn["s"] += 1
                        cn["g"] += 1
                        for k in range(8):
                            PE(lambda e, k=k: e.matmul(ps_[:, 0:256], U_[:, k, ib * 128:(ib + 1) * 128], xn_[:, k, :], start=(k == 0), stop=(k == 7)),
                               [U_, xn_], [ps_])
                        for tt in range(2):
                            for h in range(8):
                                M_ = Mt[mb][tt][h]
                                PE(lambda e, tt=tt, h=h, M_=M_: e.matmul(pg_[:, tt * 128:(tt + 1) * 128], M_[:, ib * 128:(ib + 1) * 128], Dg_[:, tt, h, :],
                                                                       start=(h == 0), stop=(h == 7)), [M_, Dg_], [pg_])
                        j = cn["h"] % 3
                        cn["h"] += 1
                        A(lambda e: e.activation(gl[j][:], ps_[:, 0:256], AF.Gelu), [ps_], [gl[j]])
                        V(lambda e: e.tensor_tensor(hd[j][:], gl[j][:], pg_[:, 0:256], ALU.mult), [gl[j], pg_], [hd[j]])
                        for tt in range(2):
                            for hf in range(2):
                                PE(lambda e, tt=tt, hf=hf: e.matmul(pO[tt * 2 + hf][:], hd[j][:, tt * 128:(tt + 1) * 128], Vb_[:, ib, hf * 512:(hf + 1) * 512],
                                                                  start=(eb == 0), stop=(eb == nec * 8 - 1)), [hd[j], Vb_], [pO[tt * 2 + hf]])
                    blk()
            chunk()
        for tt in range(2):
            f_ = fo[tt]
            for hf in range(2):
                cs_ = slice(hf * 512, (hf + 1) * 512)
                V(lambda e, tt=tt, hf=hf, cs_=cs_, f_=f_: e.tensor_tensor(f_[:, cs_], pO[tt * 2 + hf][:], g.gt_bc[:, 1, cs_], ALU.mult),
                  [pO[tt * 2 + hf], g.gt_bc], [f_])
                V(lambda e, tt=tt, cs_=cs_, f_=f_: e.tensor_tensor(f_[:, cs_], f_[:, cs_], h1_[:, tt, cs_], ALU.add), [f_, h1_], [f_])
            kb.dma("sp", g.out[t0 + tt * 128:t0 + (tt + 1) * 128, :], f_[:], [f_], [g.out], f_)
    for tg in range(ngroups):
        group(tg)
    kb.end()


def build(dbg=False):
    nc = bass.Bass("TRN2", target_bir_lowering=False)
    gst = ExitStack()
    with gst:
        kb = KB(nc, gst)
        g = declare(kb, dbg)
        phase0(kb, g)
        phase_a(kb, g)
        phase_p(kb, g)
        phase_b(kb, g)
        phase_c(kb, g)
        phase_d(kb, g)
        phase_e(kb, g)
        kb.begin()
        kb.wait_all("sp", [g.out])
        kb.end()
    return nc


def host_inputs(inputs, core, shared):
    b, half = core // 2, core % 2
    x = np.asarray(inputs["x"], dtype=np.float32)
    pos = np.asarray(inputs["positions"]).astype(np.int32)
    xs = np.zeros((NT, 1024), np.float32)
    ps = np.zeros((NT, 1), np.int32)
    if half == 1:
        xs[:] = x[b]
        ps[:, 0] = pos[b]
    else:
        xs[NOWN:] = x[b, :NOWN]
        ps[NOWN:, 0] = pos[b, :NOWN]
    m = dict(shared)
    m["xs"] = xs
    m["pos"] = ps
    m["cc"] = np.ascontiguousarray(np.asarray(inputs["c"], np.float32)[b].reshape(8, 128))
    m["pv"] = np.full((128, 1), float(half), np.float32)
    return m


def shared_inputs(inputs):
    m = {}
    invf = (500000.0 ** (-(np.arange(8, dtype=np.float32) * 2.0) / 16.0)).astype(np.float32)
    m["invf"] = np.ascontiguousarray(np.broadcast_to(invf[None, :], (128, 8)))

    def w(name, shape, key=None):
        m[name] = np.ascontiguousarray(np.asarray(inputs[key or name], np.float32).reshape(shape))
    w("w_ada", (1024, 6144)); w("b_ada", (1, 6144)); w("norm1_g", (1, 1024)); w("w_in", (1024, 5376))
    w("q_norm_g", (1, 64)); w("k_norm_g", (1, 64)); w("rwkv_mu", (1, 1792)); w("rwkv_w0", (1, 512))
    w("rwkv_w2", (64, 512)); w("rwkv_a0", (1, 512)); w("rwkv_a2", (64, 512)); w("rwkv_g2", (128, 512))
    w("rwkv_k_k", (1, 512)); w("rwkv_k_a", (1, 512)); w("rwkv_r_k", (1, 512)); w("rwkv_ln_g", (1, 512))
    w("rwkv_ln_b", (1, 512)); w("w_proj_moba", (512, 1024)); w("w_proj_rwkv", (512, 1024)); w("w_out", (1024, 1024))
    w("norm2_g", (1, 1024)); w("peer_wq", (1024, 2048)); w("peer_sk", (16, 128, 128), "peer_subkeys")
    w("peer_u", (16384, 1024)); w("peer_v", (16384, 1024))
    return m


def kernel(**inputs):
    nc = build(False)
    shared = shared_inputs(inputs)
    in_maps = [host_inputs(inputs, c, shared) for c in range(8)]
    res = run_bass_kernel_spmd(nc, in_maps, core_ids=list(range(8)))
    out = np.zeros((4, 8192, 1024), np.float32)
    for c in range(8):
        b, half = c // 2, c % 2
        out[b, half * NOWN:(half + 1) * NOWN] = np.asarray(res.results[c]["out"], np.float32)
    return out
```

```python
import numpy as np
import concourse.bass as bass
import concourse.mybir as mybir
from concourse.bass_utils import run_bass_kernel_spmd
from contextlib import ExitStack

F32 = mybir.dt.float32
BF16 = mybir.dt.bfloat16
I32 = mybir.dt.int32
AF = mybir.ActivationFunctionType
ALU = mybir.AluOpType
AX = mybir.AxisListType
EPOCH = 24000
P = 128


class Buf:
    def __init__(self, t, name):
        self.t = t
        self.name = name
        self.ws = {}
        self.rs = {}
        self.dkey = None
        self.dcnt = 0

    def __getitem__(self, k):
        return self.t[k]


class KB:
    ENG = ("pe", "act", "dve", "pool", "sp")

    def __init__(self, nc, stack):
        self.nc = nc
        self.stack = stack
        self.phs = []
        self.scope_bufs = []
        self.dpool = []
        self.ops = {e: [] for e in self.ENG}
        self.cnt = {e: 0 for e in self.ENG}
        self.known = {e: {} for e in self.ENG}
        self.sems = {}
        self.nsem = 0
        self.nins = 0

    def sem(self, key):
        if key not in self.sems:
            self.sems[key] = self.stack.enter_context(self.nc.semaphore(f"s{self.nsem}"))
            self.nsem += 1
        return self.sems[key]

    def gsbuf(self, name, shape, dt):
        return Buf(self.stack.enter_context(self.nc.sbuf_tensor(name, list(shape), dt)), name)

    def sbuf(self, name, shape, dt):
        self.nsem += 0
        self.uid = getattr(self, "uid", 0) + 1
        name = f"{name}_u{self.uid}"
        b = Buf(self.phs[-1].enter_context(self.nc.sbuf_tensor(name, list(shape), dt)), name)
        self.scope_bufs[-1].append(b)
        return b

    def psum(self, name, shape, dt):
        self.uid = getattr(self, "uid", 0) + 1
        name = f"{name}_u{self.uid}"
        return Buf(self.phs[-1].enter_context(self.nc.psum_tensor(name, list(shape), dt)), name)

    def dram(self, name, shape, dt, kind="Internal"):
        return Buf(self.nc.dram_tensor(name, list(shape), dt, kind=kind).ap(), name)

    def _deps(self, eng, reads, writes):
        d = {}
        for b in reads:
            for k, v in b.ws.items():
                if d.get(k, 0) < v:
                    d[k] = v
        for b in writes:
            for k, v in b.ws.items():
                if d.get(k, 0) < v:
                    d[k] = v
            for k, v in b.rs.items():
                if d.get(k, 0) < v:
                    d[k] = v
        kn = self.known[eng]
        for k, v in d.items():
            if eng == "pe" and k[0] == "pe":
                continue
            if kn.get(k, 0) >= v:
                continue
            kn[k] = v
            self.ops[eng].append(("wait", k, v))

    def op(self, eng, fn, reads=(), writes=()):
        self._deps(eng, reads, writes)
        self.cnt[eng] += 1
        n = self.cnt[eng]
        key = (eng, (n - 1) // EPOCH)
        val = (n - 1) % EPOCH + 1
        self.sem(key)
        self.ops[eng].append(("op", fn, key))
        for b in reads:
            if b.rs.get(key, 0) < val:
                b.rs[key] = val
        for b in writes:
            if b.ws.get(key, 0) < val:
                b.ws[key] = val

    def dma(self, q, out_ap, in_ap, reads, writes, sb, **kw):
        self._deps(q, reads, writes)
        if sb.dkey is None:
            if self.dpool:
                sb.dkey, sb.dcnt = self.dpool.pop()
            else:
                sb.dkey = ("d", sb.name)
                self.sem(sb.dkey)
        sb.dcnt += 16
        key, val = sb.dkey, sb.dcnt
        self.ops[q].append(("dma", out_ap, in_ap, key, kw))
        for b in reads:
            if b.rs.get(key, 0) < val:
                b.rs[key] = val
        for b in writes:
            if b.ws.get(key, 0) < val:
                b.ws[key] = val

    def wait_all(self, eng, bufs):
        self._deps(eng, bufs, ())

    def begin(self):
        st = ExitStack()
        st.__enter__()
        self.phs.append(st)
        self.scope_bufs.append([])

    def end(self):
        nc = self.nc
        mine = self.scope_bufs.pop()
        for b in mine:
            if b.dkey is not None:
                if self.known["sp"].get(b.dkey, 0) < b.dcnt:
                    self.known["sp"][b.dkey] = b.dcnt
                    self.ops["sp"].append(("wait", b.dkey, b.dcnt))
                self.dpool.append((b.dkey, b.dcnt))
        with nc.Block() as blk:
            def run(e, name):
                pend = None
                for o in self.ops[name]:
                    if o[0] == "wait":
                        if pend is not None:
                            self.nins += 1
                            e.wait_ge(self.sems[pend[1]], pend[2])
                        pend = o
                        continue
                    self.nins += 1
                    if o[0] == "op":
                        ins = o[1](e)
                        if pend is not None:
                            ins._wait_ge(self.sems[pend[1]], pend[2])
                        ins.then_inc(self.sems[o[2]], 1)
                    else:
                        ins = e.dma_start(out=o[1], in_=o[2], **o[4])
                        if pend is not None:
                            ins._wait_ge(self.sems[pend[1]], pend[2])
                        ins.then_inc(self.sems[o[3]], 16)
                    pend = None
                if pend is not None:
                    self.nins += 1
                    e.wait_ge(self.sems[pend[1]], pend[2])
                self.ops[name] = []

            @blk.tensor
            def _(e):
                run(e, "pe")

            @blk.scalar
            def _(e):
                run(e, "act")

            @blk.vector
            def _(e):
                run(e, "dve")

            @blk.gpsimd
            def _(e):
                run(e, "pool")

            @blk.sync
            def _(e):
                run(e, "sp")
        self.phs.pop().__exit__(None, None, None)


NT = 8192
NOWN = 4096
NTILE = NT // P
OWN0 = (NT - NOWN) // P


def bc(ap, shape):
    return ap.to_broadcast(list(shape))


class Ctx:
    pass


def declare(kb, dbg):
    g = Ctx()
    g.dbg = dbg
    sk = "ExternalOutput" if dbg else "Internal"
    EI = "ExternalInput"
    g.xs = kb.dram("xs", [NT, 1024], F32, EI)
    g.pos = kb.dram("pos", [NT, 1], I32, EI)
    g.cc = kb.dram("cc", [8, 128], F32, EI)
    g.pvd = kb.dram("pv", [128, 1], F32, EI)
    g.invf = kb.dram("invf", [128, 8], F32, EI)
    g.w_ada = kb.dram("w_ada", [1024, 6144], F32, EI)
    g.b_ada = kb.dram("b_ada", [1, 6144], F32, EI)
    g.norm1_g = kb.dram("norm1_g", [1, 1024], F32, EI)
    g.w_in = kb.dram("w_in", [1024, 5376], F32, EI)
    g.q_norm_g = kb.dram("q_norm_g", [1, 64], F32, EI)
    g.k_norm_g = kb.dram("k_norm_g", [1, 64], F32, EI)
    g.rwkv_mu = kb.dram("rwkv_mu", [1, 1792], F32, EI)
    g.rwkv_w0 = kb.dram("rwkv_w0", [1, 512], F32, EI)
    g.rwkv_w2 = kb.dram("rwkv_w2", [64, 512], F32, EI)
    g.rwkv_a0 = kb.dram("rwkv_a0", [1, 512], F32, EI)
    g.rwkv_a2 = kb.dram("rwkv_a2", [64, 512], F32, EI)
    g.rwkv_g2 = kb.dram("rwkv_g2", [128, 512], F32, EI)
    g.rwkv_k_k = kb.dram("rwkv_k_k", [1, 512], F32, EI)
    g.rwkv_k_a = kb.dram("rwkv_k_a", [1, 512], F32, EI)
    g.rwkv_r_k = kb.dram("rwkv_r_k", [1, 512], F32, EI)
    g.rwkv_ln_g = kb.dram("rwkv_ln_g", [1, 512], F32, EI)
    g.rwkv_ln_b = kb.dram("rwkv_ln_b", [1, 512], F32, EI)
    g.w_proj_moba = kb.dram("w_proj_moba", [512, 1024], F32, EI)
    g.w_proj_rwkv = kb.dram("w_proj_rwkv", [512, 1024], F32, EI)
    g.w_out = kb.dram("w_out", [1024, 1024], F32, EI)
    g.norm2_g = kb.dram("norm2_g", [1, 1024], F32, EI)
    g.peer_wq = kb.dram("peer_wq", [1024, 2048], F32, EI)
    g.peer_sk = kb.dram("peer_sk", [16, 128, 128], F32, EI)
    g.peer_u = kb.dram("peer_u", [16384, 1024], F32, EI)
    g.peer_v = kb.dram("peer_v", [16384, 1024], F32, EI)
    g.out = kb.dram("out", [NOWN, 1024], F32, "ExternalOutput")
    g.QT = kb.dram("QT", [4, 128, NOWN], BF16, sk)
    g.KT = kb.dram("KT", [4, 128, NT], BF16, sk)
    g.VM = kb.dram("VM", [NT, 512], BF16, sk)
    g.RS = kb.dram("RS", [6, 8, NT, 64], F32, sk)
    g.GR = kb.dram("GR", [NOWN, 512], F32, sk)
    g.GATES = kb.dram("GATES", [NOWN, 2048], BF16, sk)
    g.OM = kb.dram("OM", [NOWN, 512], BF16, sk)
    g.ORW = kb.dram("ORW", [NOWN, 512], BF16, sk)
    g.H1 = kb.dram("H1", [NOWN, 1024], F32, sk)
    g.XN2T = kb.dram("XN2T", [8, 128, NOWN], BF16, sk)
    g.SC = kb.dram("SC", [NOWN, 16, 128], F32, sk)
    g.TH = kb.dram("TH", [NOWN, 4, 8], F32, sk)
    g.UT = kb.dram("UT", [8, 128, 16384], BF16, "Internal")
    g.VB = kb.dram("VB", [16384, 1024], BF16, "Internal")
    g.ident_f = kb.gsbuf("ident_f", [128, 128], F32)
    g.ident_b = kb.gsbuf("ident_b", [128, 128], BF16)
    g.ones_f = kb.gsbuf("ones_f", [128, 128], F32)
    g.modc = kb.gsbuf("modc", [128, 48], F32)
    g.sc1p = kb.gsbuf("sc1p", [128, 8], F32)
    g.sc2p = kb.gsbuf("sc2p", [128, 8], F32)
    g.gt_bc = kb.gsbuf("gt_bc", [128, 2, 1024], F32)
    g.pv = kb.gsbuf("pvt", [128, 1], F32)
    g.kmT = kb.gsbuf("kmT", [128, 4, NT // 256], F32)
    g.npi = kb.gsbuf("npi", [128, 1], F32)
    return g


def phase0(kb, g):
    kb.begin()
    V = lambda fn, r, w: kb.op("dve", fn, r, w)
    A = lambda fn, r, w: kb.op("act", fn, r, w)
    PE = lambda fn, r, w: kb.op("pe", fn, r, w)
    PL = lambda fn, r, w: kb.op("pool", fn, r, w)
    PL(lambda e: e.memset(g.ones_f[:], 1.0), [], [g.ones_f])
    PL(lambda e: e.memset(g.npi[:], -float(np.pi)), [], [g.npi])
    PL(lambda e: e.memset(g.ident_f[:], 1.0), [], [g.ident_f])
    PL(lambda e: e.affine_select(out=g.ident_f[:], in_=g.ident_f[:], pattern=[[-1, 128]],
                                 compare_op=ALU.is_equal, fill=0.0, base=0, channel_multiplier=1),
       [g.ident_f], [g.ident_f])
    V(lambda e: e.tensor_copy(g.ident_b[:], g.ident_f[:]), [g.ident_f], [g.ident_b])
    kb.dma("sp", g.pv[:], g.pvd[:], [g.pvd], [g.pv], g.pv)
    c8 = kb.sbuf("c8", [8, 128], F32)
    scT = kb.sbuf("scT", [128, 8], F32)
    modrow = kb.sbuf("modrow", [1, 6144], F32)
    brow = kb.sbuf("brow", [1, 6144], F32)
    grow = kb.sbuf("grow", [1, 2, 1024], F32)
    gcol = kb.sbuf("gcol", [128, 16], F32)
    wst = [kb.sbuf(f"wst{i}", [128, 8, 512], F32) for i in range(2)]
    ps = kb.psum("p0ps", [128, 512], F32)
    ps2 = kb.psum("p0ps2", [128, 512], F32)
    kb.dma("sp", c8[:], g.cc[:], [g.cc], [c8], c8)
    kb.dma("sp", brow[:], g.b_ada[:], [g.b_ada], [brow], brow)
    kb.dma("sp", grow[:, 0, :], g.norm1_g[:], [g.norm1_g], [grow], grow)
    kb.dma("sp", grow[:, 1, :], g.norm2_g[:], [g.norm2_g], [grow], grow)
    PE(lambda e: e.transpose(ps[:, 0:8], c8[:], g.ident_f[0:8, 0:8]), [c8, g.ident_f], [ps])
    A(lambda e: e.activation(scT[:], ps[:, 0:8], AF.Silu), [ps], [scT])
    wv = g.w_ada.t.rearrange("(k p) n -> p k n", p=128)
    for jg in range(12):
        wb_ = wst[jg % 2]
        kb.dma("sp", wb_[:], wv[:, :, jg * 512:(jg + 1) * 512], [g.w_ada], [wb_], wb_)
        for k in range(8):
            PE(lambda e, k=k, wb_=wb_: e.matmul(ps2[0:1, :], scT[:, k:k + 1], wb_[:, k, :],
                                                  start=(k == 0), stop=(k == 7)), [scT, wb_], [ps2])
        V(lambda e, jg=jg: e.tensor_tensor(modrow[:, jg * 512:(jg + 1) * 512], ps2[0:1, :],
                                           brow[:, jg * 512:(jg + 1) * 512], ALU.add), [ps2, brow], [modrow])
    for j in range(48):
        PE(lambda e, j=j: e.matmul(ps[:, 16 + j:17 + j], modrow[0:1, j * 128:(j + 1) * 128], g.ones_f[0:1, 0:1],
                                   start=True, stop=True), [modrow, g.ones_f], [ps])
    V(lambda e: e.tensor_copy(g.modc[:], ps[:, 16:64]), [ps], [g.modc])
    for j in range(16):
        PE(lambda e, j=j: e.matmul(ps[:, 64 + j:65 + j], grow[0:1, j // 8, (j % 8) * 128:(j % 8 + 1) * 128],
                                   g.ones_f[0:1, 0:1], start=True, stop=True), [grow, g.ones_f], [ps])
    V(lambda e: e.tensor_copy(gcol[:], ps[:, 64:80]), [ps], [gcol])
    V(lambda e: e.scalar_tensor_tensor(g.sc1p[:], g.modc[:, 8:16], 1.0, gcol[:, 0:8], ALU.add, ALU.mult),
      [g.modc, gcol], [g.sc1p])
    V(lambda e: e.scalar_tensor_tensor(g.sc2p[:], g.modc[:, 32:40], 1.0, gcol[:, 8:16], ALU.add, ALU.mult),
      [g.modc, gcol], [g.sc2p])
    for i, c0 in enumerate((16 * 128, 40 * 128)):
        for hh in range(2):
            PE(lambda e, c0=c0, hh=hh: e.matmul(ps2[:, :], g.ones_f[0:1, :], modrow[0:1, c0 + hh * 512:c0 + (hh + 1) * 512],
                                                start=True, stop=True), [g.ones_f, modrow], [ps2])
            A(lambda e, i=i, hh=hh: e.copy(g.gt_bc[:, i, hh * 512:(hh + 1) * 512], ps2[:, :]), [ps2], [g.gt_bc])
    kb.end()


def phase_a(kb, g, ntiles=NTILE):
    kb.begin()
    V = lambda fn, r, w: kb.op("dve", fn, r, w)
    A = lambda fn, r, w: kb.op("act", fn, r, w)
    PE = lambda fn, r, w: kb.op("pe", fn, r, w)
    PL = lambda fn, r, w: kb.op("pool", fn, r, w)
    Wb = kb.sbuf("Wb", [128, 8, 5376], BF16)
    Wm = kb.sbuf("Wm", [128, 8, 1792], BF16)
    kb.begin()
    mu_bc = kb.sbuf("mu_bc", [128, 1792], F32)
    omu_bc = kb.sbuf("omu_bc", [128, 1792], F32)
    wst = [kb.sbuf(f"awst{i}", [128, 8, 256], F32) for i in range(2)]
    kb.dma("sp", mu_bc[:], g.rwkv_mu.t.partition_broadcast(128), [g.rwkv_mu], [mu_bc], mu_bc)
    V(lambda e: e.tensor_scalar(omu_bc[:], mu_bc[:], -1.0, 1.0, ALU.mult, ALU.add), [mu_bc], [omu_bc])
    wv = g.w_in.t.rearrange("(k p) n -> p k n", p=128)
    engs = ["dve", "act", "pool"]
    for pc in range(21):
        c0 = pc * 256
        st = wst[pc % 2]
        kb.dma("sp", st[:], wv[:, :, c0:c0 + 256], [g.w_in], [st], st)
        if 1536 <= c0 < 3328:
            m0 = c0 - 1536
            V(lambda e, st=st, c0=c0, m0=m0: e.tensor_tensor(Wb[:, :, c0:c0 + 256], st[:],
              bc(omu_bc[:, m0:m0 + 256].unsqueeze(1), [128, 8, 256]), ALU.mult), [st, omu_bc], [Wb])
            PL(lambda e, st=st, m0=m0: e.tensor_tensor(Wm[:, :, m0:m0 + 256], st[:],
               bc(mu_bc[:, m0:m0 + 256].unsqueeze(1), [128, 8, 256]), ALU.mult), [st, mu_bc], [Wm])
        else:
            en = engs[pc % 2]
            if en == "act":
                A(lambda e, st=st, c0=c0: e.copy(Wb[:, :, c0:c0 + 256], st[:]), [st], [Wb])
            else:
                V(lambda e, st=st, c0=c0: e.tensor_copy(Wb[:, :, c0:c0 + 256], st[:]), [st], [Wb])
    kb.end()
    def rowbc(name, src, n):
        t = kb.sbuf(name, [128, n], F32)
        kb.dma("sp", t[:], src.t.partition_broadcast(128), [src], [t], t)
        return t
    qg = rowbc("qg", g.q_norm_g, 64)
    kg = rowbc("kg", g.k_norm_g, 64)
    w0b = rowbc("w0b", g.rwkv_w0, 512)
    a0b = rowbc("a0b", g.rwkv_a0, 512)
    kkb = rowbc("kkb", g.rwkv_k_k, 512)
    kab = rowbc("kab", g.rwkv_k_a, 512)
    invf = kb.sbuf("invf_t", [128, 8], F32)
    kb.dma("sp", invf[:], g.invf[:], [g.invf], [invf], invf)
    w2a2 = kb.sbuf("w2a2", [128, 512], F32)
    g2t = kb.sbuf("g2t", [128, 512], F32)
    kb.dma("sp", w2a2[0:64, :], g.rwkv_w2[:], [g.rwkv_w2], [w2a2], w2a2)
    kb.dma("sp", w2a2[64:128, :], g.rwkv_a2[:], [g.rwkv_a2], [w2a2], w2a2)
    kb.dma("sp", g2t[:], g.rwkv_g2[:], [g.rwkv_g2], [g2t], g2t)
    onesc = kb.sbuf("onesc", [128, 1], F32)
    PL(lambda e: e.memset(onesc[:], 1.0 / 256.0), [], [onesc])

    xt = [kb.sbuf(f"xt{i}", [128, 1024], F32) for i in range(2)]
    junk = kb.sbuf("junk", [128, 1024], BF16)
    xnT = [kb.sbuf(f"xnT{i}", [128, 8, 129], BF16) for i in range(2)]
    stt = [kb.sbuf(f"stt{i}", [128, 4], F32) for i in range(2)]
    posi = [kb.sbuf(f"posi{i}", [128, 1], I32) for i in range(2)]
    cs = [kb.sbuf(f"cs{i}", [128, 5, 16], F32) for i in range(2)]
    csi = [kb.sbuf(f"csi{i}", [128, 16], I32) for i in range(2)]
    psT = [kb.psum(f"psT{i}", [128, 512], F32) for i in range(2)]
    psM = [kb.psum(f"psM{i}", [128, 512], F32) for i in range(4)]
    psB = kb.psum("psB", [128, 1024], BF16)
    psX = kb.psum("psX", [128, 512], F32)
    NB = 3
    t1 = [kb.sbuf(f"t1_{i}", [128, 512], F32) for i in range(NB)]
    t2 = [kb.sbuf(f"t2_{i}", [128, 512], F32) for i in range(NB)]
    ssq = [kb.sbuf(f"ssq{i}", [128, 16], F32) for i in range(NB)]
    rp = [kb.sbuf(f"rp{i}", [128, 4, 8, 8], F32) for i in range(NB)]
    qf = [kb.sbuf(f"qf{i}", [128, 512], BF16) for i in range(NB)]
    qTs = [kb.sbuf(f"qTs{i}", [128, 4, 128], BF16) for i in range(NB)]
    kmp = [kb.sbuf(f"kmp{i}", [128, 4], F32) for i in range(2)]
    la = [kb.sbuf(f"la{i}", [128, 256], F32) for i in range(2)]
    laT = [kb.sbuf(f"laT{i}", [128, 256], F32) for i in range(2)]
    av = [kb.sbuf(f"av{i}", [128, 512], F32) for i in range(2)]
    sto = [kb.sbuf(f"sto{i}", [128, 512], F32) for i in range(8)]
    gsb = [kb.sbuf(f"gsb{i}", [128, 512], BF16) for i in range(3)]
    cnt = {"m": 0, "b": 0, "s": 0, "g": 0}

    def nextM():
        cnt["m"] += 1
        return psM[cnt["m"] % 4]

    def nextS():
        cnt["s"] += 1
        return sto[cnt["s"] % 8]

    def mm(ps_, ncols, col0, xn, prev_c0=None):
        for k in range(8):
            PE(lambda e, k=k: e.matmul(ps_[:, 0:ncols], xn[:, k, 1:129], Wb[:, k, col0:col0 + ncols],
                                       start=(k == 0), stop=(k == 7 and prev_c0 is None)), [xn, Wb], [ps_])
        if prev_c0 is not None:
            for k in range(8):
                PE(lambda e, k=k: e.matmul(ps_[:, 0:ncols], xn[:, k, 0:128], Wm[:, k, prev_c0:prev_c0 + ncols],
                                           start=False, stop=(k == 7)), [xn, Wm], [ps_])

    def stream_store(si, src, t0):
        dst = g.RS.t[si, :, t0:t0 + 128, :].rearrange("h t n -> t h n")
        kb.dma("sp", dst, src[:].rearrange("p (h n) -> p h n", h=8), [src], [g.RS], src)

    def tile_body(it):
        own = it >= OWN0
        t0 = it * 128
        x_ = xt[it % 2]
        xn = xnT[it % 2]
        xnp = xnT[(it + 1) % 2]
        st_ = stt[it % 2]
        kb.dma("sp", x_[:], g.xs[t0:t0 + 128, :], [g.xs], [x_], x_)
        pi = posi[it % 2]
        kb.dma("sp", pi[:], g.pos[t0:t0 + 128, :], [g.pos], [pi], pi)
        A(lambda e, x_=x_, st_=st_: e.activation(junk[:], x_[:], AF.Square, accum_out=st_[:, 0:1]), [x_], [junk, st_])
        V(lambda e, st_=st_: e.tensor_scalar(st_[:, 1:2], st_[:, 0:1], 1.0 / 1024.0, 1e-6, ALU.mult, ALU.add), [st_], [st_])
        A(lambda e, st_=st_: e.activation(st_[:, 2:3], st_[:, 1:2], AF.Sqrt), [st_], [st_])
        V(lambda e, st_=st_: e.reciprocal(st_[:, 2:3], st_[:, 2:3]), [st_], [st_])
        V(lambda e, x_=x_, st_=st_: e.tensor_scalar_mul(x_[:], x_[:], st_[:, 2:3]), [x_, st_], [x_])
        for k in range(8):
            pt = psT[k // 4]
            PE(lambda e, k=k, pt=pt, x_=x_: e.transpose(pt[:, (k % 4) * 128:(k % 4 + 1) * 128], x_[:, k * 128:(k + 1) * 128],
                                                        g.ident_f[:]), [x_, g.ident_f], [pt])
            A(lambda e, k=k, pt=pt, xn=xn: e.activation(xn[:, k, 1:129], pt[:, (k % 4) * 128:(k % 4 + 1) * 128], AF.Identity,
                                                       bias=g.modc[:, k:k + 1], scale=g.sc1p[:, k:k + 1]),
              [pt, g.modc, g.sc1p], [xn])
        if it == 0:
            V(lambda e, xn=xn: e.memset(xn[:, :, 0:1], 0.0), [], [xn])
        elif it == OWN0:
            V(lambda e, xn=xn, xnp=xnp: e.tensor_scalar_mul(xn[:, :, 0:1], xnp[:, :, 128:129], g.pv[:, 0:1]), [xnp, g.pv], [xn])
        else:
            V(lambda e, xn=xn, xnp=xnp: e.tensor_copy(xn[:, :, 0:1], xnp[:, :, 128:129]), [xnp], [xn])
        c_ = cs[it % 2]
        ci_ = csi[it % 2]
        PI = float(np.pi)
        V(lambda e: e.tensor_copy(c_[:, 1, 0:1], pi[:]), [pi], [c_])
        V(lambda e: e.tensor_scalar_mul(c_[:, 0, 0:8], invf[:], c_[:, 1, 0:1]), [invf, c_], [c_])
        V(lambda e: e.tensor_scalar_add(c_[:, 0, 8:16], c_[:, 0, 0:8], 0.5 * PI), [c_], [c_])
        V(lambda e: e.tensor_scalar_mul(c_[:, 1, :], c_[:, 0, :], 1.0 / (2 * PI)), [c_], [c_])
        V(lambda e: e.tensor_copy(ci_[:], c_[:, 1, :]), [c_], [ci_])
        V(lambda e: e.tensor_copy(c_[:, 1, :], ci_[:]), [ci_], [c_])
        V(lambda e: e.scalar_tensor_tensor(c_[:, 2, :], c_[:, 1, :], -2 * PI, c_[:, 0, :], ALU.mult, ALU.add), [c_], [c_])
        V(lambda e: e.tensor_single_scalar(c_[:, 1, :], c_[:, 2, :], PI, ALU.is_gt), [c_], [c_])
        V(lambda e: e.scalar_tensor_tensor(c_[:, 3, :], c_[:, 1, :], -2 * PI, c_[:, 2, :], ALU.mult, ALU.add), [c_], [c_])
        V(lambda e: e.tensor_single_scalar(c_[:, 1, :], c_[:, 3, :], -PI, ALU.is_lt), [c_], [c_])
        V(lambda e: e.scalar_tensor_tensor(c_[:, 2, :], c_[:, 1, :], 2 * PI, c_[:, 3, :], ALU.mult, ALU.add), [c_], [c_])
        A(lambda e: e.activation(c_[:, 4, :], c_[:, 2, :], AF.Sin), [c_], [c_])

        def qk_post(ps_, gb, is_q):
            i = cnt["b"] % NB
            cnt["b"] += 1
            a1, a2, sq_, rp_, qf_, qT_ = t1[i], t2[i], ssq[i], rp[i], qf[i], qTs[i]
            A(lambda e: e.activation(a1[:], ps_[:], AF.Square), [ps_], [a1])
            V(lambda e: e.tensor_reduce(sq_[:, 0:8], a1[:].rearrange("p (h d) -> p h d", h=8), AX.X, ALU.add), [a1], [sq_])
            V(lambda e: e.tensor_scalar(sq_[:, 0:8], sq_[:, 0:8], 1.0 / 64.0, 1e-6, ALU.mult, ALU.add), [sq_], [sq_])
            A(lambda e: e.activation(sq_[:, 8:16], sq_[:, 0:8], AF.Sqrt), [sq_], [sq_])
            V(lambda e: e.reciprocal(sq_[:, 8:16], sq_[:, 8:16]), [sq_], [sq_])
            V(lambda e: e.tensor_tensor(a2[:].rearrange("p (h d) -> p h d", h=8), ps_[:].rearrange("p (h d) -> p h d", h=8),
                                        bc(sq_[:, 8:16].unsqueeze(2), [128, 8, 64]), ALU.mult), [ps_, sq_], [a2])
            V(lambda e: e.tensor_tensor(a2[:].rearrange("p (h d) -> p h d", h=8), a2[:].rearrange("p (h d) -> p h d", h=8),
                                        bc(gb[:].unsqueeze(1), [128, 8, 64]), ALU.mult), [a2, gb], [a2])
            v3 = a2[:].rearrange("p (h d) -> p h d", h=8)
            x1, x2 = v3[:, :, 0:8], v3[:, :, 8:16]
            sinb = bc(c_[:, 4, 0:8].unsqueeze(1), [128, 8, 8])
            cosb = bc(c_[:, 4, 8:16].unsqueeze(1), [128, 8, 8])
            V(lambda e: e.tensor_tensor(rp_[:, 0], x1, cosb, ALU.mult), [a2, c_], [rp_])
            V(lambda e: e.tensor_tensor(rp_[:, 1], x2, sinb, ALU.mult), [a2, c_], [rp_])
            V(lambda e: e.tensor_tensor(rp_[:, 2], x2, cosb, ALU.mult), [a2, c_], [rp_])
            V(lambda e: e.tensor_tensor(rp_[:, 3], x1, sinb, ALU.mult), [a2, c_], [rp_])
            V(lambda e: e.tensor_tensor(x1, rp_[:, 0], rp_[:, 1], ALU.subtract), [rp_], [a2])
            V(lambda e: e.tensor_tensor(x2, rp_[:, 2], rp_[:, 3], ALU.add), [rp_], [a2])
            A(lambda e: e.copy(qf_[:], a2[:]), [a2], [qf_])
            for pr in range(4):
                PE(lambda e, pr=pr: e.transpose(psB[:, pr * 128:(pr + 1) * 128], qf_[:, pr * 128:(pr + 1) * 128], g.ident_b[:]),
                   [qf_, g.ident_b], [psB])
            V(lambda e: e.tensor_copy(qT_[:].rearrange("p c t -> p (c t)"), psB[:, 0:512]), [psB], [qT_])
            if is_q:
                to = t0 - OWN0 * 128
                kb.dma("sp", g.QT.t[:, :, to:to + 128].rearrange("c p t -> p c t"), qT_[:], [qT_], [g.QT], qT_)
            else:
                kb.dma("sp", g.KT.t[:, :, t0:t0 + 128].rearrange("c p t -> p c t"), qT_[:], [qT_], [g.KT], qT_)
                km_ = kmp[it % 2]
                for pr in range(4):
                    PE(lambda e, pr=pr: e.matmul(psX[:, 256 + pr:257 + pr], a2[:, pr * 128:(pr + 1) * 128], onesc[:],
                                                 start=True, stop=True), [a2, onesc], [psX])
                V(lambda e: e.tensor_copy(km_[:], psX[:, 256:260]), [psX], [km_])
                if it % 2 == 1:
                    V(lambda e: e.tensor_tensor(g.kmT[:, :, it // 2], kmp[0][:], kmp[1][:], ALU.add), [kmp[0], kmp[1]], [g.kmT])

        psl = nextM()
        mm(psl, 256, 3072, xn, prev_c0=1536)
        la_ = la[it % 2]
        laT_ = laT[it % 2]
        A(lambda e: e.activation(la_[:, 0:64], psl[:, 0:64], AF.Tanh), [psl], [la_])
        V(lambda e: e.tensor_copy(la_[:, 64:128], psl[:, 64:128]), [psl], [la_])
        A(lambda e: e.activation(la_[:, 128:256], psl[:, 128:256], AF.Sigmoid), [psl], [la_])
        PE(lambda e: e.transpose(psX[:, 0:128], la_[:, 0:128], g.ident_f[:]), [la_, g.ident_f], [psX])
        PE(lambda e: e.transpose(psX[:, 128:256], la_[:, 128:256], g.ident_f[:]), [la_, g.ident_f], [psX])
        V(lambda e: e.tensor_copy(laT_[:], psX[:, 0:256]), [psX], [laT_])
        pw = nextM()
        PE(lambda e: e.matmul(pw[:], laT_[0:64, 0:128], w2a2[0:64, :], start=True, stop=True), [laT_, w2a2], [pw])
        ld_ = nextS()
        V(lambda e: e.tensor_tensor(ld_[:], pw[:], w0b[:], ALU.add), [pw, w0b], [ld_])
        A(lambda e: e.activation(ld_[:], ld_[:], AF.Sigmoid), [ld_], [ld_])
        V(lambda e: e.tensor_scalar_mul(ld_[:], ld_[:], -0.6065306597126334), [ld_], [ld_])
        stream_store(1, ld_, t0)
        pa = nextM()
        PE(lambda e: e.matmul(pa[:], laT_[64:128, 0:128], w2a2[64:128, :], start=True, stop=True), [laT_, w2a2], [pa])
        a_ = av[it % 2]
        V(lambda e: e.tensor_tensor(a_[:], pa[:], a0b[:], ALU.add), [pa, a0b], [a_])
        A(lambda e: e.activation(a_[:], a_[:], AF.Sigmoid), [a_], [a_])
        if own:
            pg = nextM()
            PE(lambda e: e.matmul(pg[:], laT_[:, 128:256], g2t[:], start=True, stop=True), [laT_, g2t], [pg])
            gg = nextS()
            A(lambda e: e.copy(gg[:], pg[:]), [pg], [gg])
            to = t0 - OWN0 * 128
            kb.dma("sp", g.GR[to:to + 128, :], gg[:], [gg], [g.GR], gg)
        pr_ = nextM()
        mm(pr_, 512, 1536, xn, prev_c0=0)
        r_ = nextS()
        A(lambda e: e.copy(r_[:], pr_[:]), [pr_], [r_])
        stream_store(0, r_, t0)
        pk = nextM()
        mm(pk, 512, 2048, xn, prev_c0=512)
        i = cnt["b"] % NB
        cnt["b"] += 1
        a1, sq_ = t1[i], ssq[i]
        kkn = nextS()
        V(lambda e: e.tensor_tensor(kkn[:], pk[:], kkb[:], ALU.mult), [pk, kkb], [kkn])
        V(lambda e: e.tensor_tensor(a1[:], kkn[:], kkn[:], ALU.mult), [kkn], [a1])
        V(lambda e: e.tensor_reduce(sq_[:, 0:8], a1[:].rearrange("p (h d) -> p h d", h=8), AX.X, ALU.add), [a1], [sq_])
        V(lambda e: e.tensor_scalar_add(sq_[:, 0:8], sq_[:, 0:8], 1e-24), [sq_], [sq_])
        A(lambda e: e.activation(sq_[:, 8:16], sq_[:, 0:8], AF.Sqrt), [sq_], [sq_])
        V(lambda e: e.reciprocal(sq_[:, 8:16], sq_[:, 8:16]), [sq_], [sq_])
        V(lambda e: e.scalar_tensor_tensor(kkn[:].rearrange("p (h d) -> p h d", h=8), kkn[:].rearrange("p (h d) -> p h d", h=8), -1.0,
                                           bc(sq_[:, 8:16].unsqueeze(2), [128, 8, 64]), ALU.mult, ALU.mult), [kkn, sq_], [kkn])
        stream_store(4, kkn, t0)
        b_ = nextS()
        V(lambda e: e.scalar_tensor_tensor(b_[:], kkn[:], -1.0, a_[:], ALU.mult, ALU.mult), [kkn, a_], [b_])
        stream_store(5, b_, t0)
        k_ = nextS()
        V(lambda e: e.scalar_tensor_tensor(a1[:], a_[:], -1.0, kab[:], ALU.add, ALU.mult), [a_, kab], [a1])
        V(lambda e: e.scalar_tensor_tensor(k_[:], a1[:], 1.0, pk[:], ALU.add, ALU.mult), [a1, pk], [k_])
        stream_store(2, k_, t0)
        pv_ = nextM()
        mm(pv_, 512, 2560, xn, prev_c0=1024)
        v_ = nextS()
        if own:
            A(lambda e: e.copy(v_[:], pv_[:]), [pv_], [v_])
        else:
            V(lambda e: e.tensor_scalar_mul(v_[:], pv_[:], g.pv[:, 0:1]), [pv_, g.pv], [v_])
        stream_store(3, v_, t0)
        pmk = nextM()
        mm(pmk, 512, 512, xn)
        qk_post(pmk, kg, False)
        pmv = nextM()
        mm(pmv, 512, 1024, xn)
        vb = gsb[cnt["g"] % 3]
        cnt["g"] += 1
        A(lambda e: e.copy(vb[:], pmv[:]), [pmv], [vb])
        kb.dma("sp", g.VM[t0:t0 + 128, :], vb[:], [vb], [g.VM], vb)
        if own:
            pmq = nextM()
            mm(pmq, 512, 0, xn)
            qk_post(pmq, qg, True)
            to = t0 - OWN0 * 128
            for gi in range(4):
                pg_ = nextM()
                mm(pg_, 512, 3328 + gi * 512, xn)
                gb_ = gsb[cnt["g"] % 3]
                cnt["g"] += 1
                A(lambda e, gb_=gb_, pg_=pg_: e.activation(gb_[:], pg_[:], AF.Sigmoid), [pg_], [gb_])
                kb.dma("sp", g.GATES[to:to + 128, gi * 512:(gi + 1) * 512], gb_[:], [gb_], [g.GATES], gb_)
    for it in range(ntiles):
        tile_body(it)
    kb.end()


def phase_b(kb, g, nqb=16):
    kb.begin()
    V = lambda fn, r, w: kb.op("dve", fn, r, w)
    A = lambda fn, r, w: kb.op("act", fn, r, w)
    PE = lambda fn, r, w: kb.op("pe", fn, r, w)
    PL = lambda fn, r, w: kb.op("pool", fn, r, w)
    NKB = NT // 256
    QB0 = OWN0 // 2
    kmTb = kb.sbuf("kmTb", [128, 4, NKB], BF16)
    V(lambda e: e.tensor_copy(kmTb[:], g.kmT[:]), [g.kmT], [kmTb])
    pastm = kb.sbuf("pastm", [128, 16, NKB], F32)
    pfx = kb.sbuf("pfx", [128, 1], F32)
    PL(lambda e: e.memset(pastm[:], 0.0), [], [pastm])
    for qbl in range(16):
        PL(lambda e, qbl=qbl: e.memset(pastm[:, qbl, QB0 + qbl:NKB], -1e30), [], [pastm])
    V(lambda e: e.tensor_scalar(pfx[:], g.pv[:], -1.0, 1e30, ALU.add, ALU.mult), [g.pv], [pfx])
    V(lambda e: e.tensor_scalar(pastm[:, :, 0:QB0], pastm[:, :, 0:QB0], pfx[:, 0:1], None, ALU.add), [pastm, pfx], [pastm])
    tri = kb.sbuf("tri", [128, 2, 256], BF16)
    PL(lambda e: e.memset(tri[:], 1.0), [], [tri])
    for kc in range(2):
        PL(lambda e, kc=kc: e.affine_select(out=tri[:, kc, :], in_=tri[:, kc, :], pattern=[[1, 256]], compare_op=ALU.is_ge,
                                            fill=0.0, base=-kc * 128, channel_multiplier=-1), [tri], [tri])
    KTs = [kb.sbuf(f"KTs{i}", [128, NT], BF16) for i in range(2)]
    QTs = [kb.sbuf(f"QTs{i}", [128, NOWN], BF16) for i in range(2)]
    Vs = [kb.sbuf(f"Vs{i}", [128, NT // 128, 2, 65], BF16) for i in range(2)]
    for i in range(2):
        PL(lambda e, i=i: e.memset(Vs[i][:, :, :, 64:65], 1.0), [], [Vs[i]])
    sel = [kb.sbuf(f"sel{i}", [128, 32, 2, NKB], F32) for i in range(2)]
    gsm = [kb.sbuf(f"gsm{i}", [128, NKB], F32) for i in range(2)]
    g8 = [kb.sbuf(f"g8{i}", [128, 8], F32) for i in range(2)]
    m1 = [kb.sbuf(f"m1{i}", [128, NKB], F32) for i in range(2)]
    psS = [kb.psum(f"psS{i}", [128, 512], F32) for i in range(3)]
    psO = [kb.psum(f"psO{i}", [128, 2, 65], F32) for i in range(3)]
    psG = [kb.psum(f"psG{i}", [128, 64], F32) for i in range(2)]
    pts = [kb.sbuf(f"pts{i}", [128, 2, 256], BF16) for i in range(4)]
    acc = [kb.sbuf(f"acc{i}", [128, 2, 65], F32) for i in range(2)]
    rc = [kb.sbuf(f"rc{i}", [128, 2], F32) for i in range(2)]
    ob = [kb.sbuf(f"ob{i}", [128, 2, 64], BF16) for i in range(4)]
    cn = {"s": 0, "o": 0, "p": 0, "a": 0, "b": 0, "g": 0}
    vview = g.VM.t.rearrange("(c p) (h d) -> p c h d", p=128, d=64)

    def pair_body(pr):
        KT_, QT_, V_, sel_ = KTs[pr % 2], QTs[pr % 2], Vs[pr % 2], sel[pr % 2]
        kb.dma("sp", KT_[:], g.KT.t[pr], [g.KT], [KT_], KT_)
        kb.dma("sp", QT_[:], g.QT.t[pr], [g.QT], [QT_], QT_)
        for cq in range(4):
            c0 = cq * (NT // 512)
            c1 = c0 + NT // 512
            for h2 in range(2):
                kb.dma("sp", V_[:, c0:c1, h2, 0:64], vview[:, c0:c1, 2 * pr + h2, :], [g.VM], [V_], V_)
        for qt in range(2 * nqb):
            qbl = qt // 2
            for h2 in range(2):
                def selbody(qt=qt, qbl=qbl, h2=h2):
                    i = cn["g"] % 2
                    cn["g"] += 1
                    pg, gs_, g8_, m1_ = psG[i], gsm[i], g8[i], m1[i]
                    rows = slice(h2 * 64, (h2 + 1) * 64)
                    PE(lambda e: e.matmul(pg[:, 0:NKB], QT_[rows, qt * 128:(qt + 1) * 128], kmTb[rows, pr, :], start=True, stop=True),
                       [QT_, kmTb], [pg])
                    V(lambda e: e.tensor_tensor(gs_[:], pg[:, 0:NKB], pastm[:, qbl, :], ALU.add), [pg, pastm], [gs_])
                    V(lambda e: e.max(out=g8_[:], in_=gs_[:]), [gs_], [g8_])
                    V(lambda e: e.tensor_scalar(m1_[:], gs_[:], g8_[:, 2:3], None, ALU.is_ge), [gs_, g8_], [m1_])
                    V(lambda e: e.scalar_tensor_tensor(sel_[:, qt, h2, :], gs_[:], -1e29, m1_[:], ALU.is_gt, ALU.mult), [gs_, m1_], [sel_])
                    V(lambda e: e.memset(sel_[:, qt, h2, QB0 + qbl:QB0 + qbl + 1], 1.0), [], [sel_])
                selbody()
        for h2 in range(2):
            rows = slice(h2 * 64, (h2 + 1) * 64)
            for qbl in range(nqb):
                def qb_body(h2=h2, rows=rows, qbl=qbl):
                    qb = QB0 + qbl
                    acc_ = acc[cn["a"] % 2]
                    rc_ = rc[cn["a"] % 2]
                    cn["a"] += 1
                    for kblk in range(qb + 1):
                        def kb_body(kblk=kblk):
                            pS = psS[cn["s"] % 3]
                            cn["s"] += 1
                            pO = psO[cn["o"] % 3]
                            cn["o"] += 1
                            pt = pts[cn["p"] % 4]
                            cn["p"] += 1
                            for kc in range(2):
                                c = kblk * 2 + kc
                                PE(lambda e, kc=kc, c=c: e.matmul(pS[:, kc * 256:(kc + 1) * 256], KT_[rows, c * 128:(c + 1) * 128],
                                                                  QT_[rows, qbl * 256:(qbl + 1) * 256], start=True, stop=True),
                                   [KT_, QT_], [pS])
                            A(lambda e: e.activation(pt[:].rearrange("p a b -> p (a b)"), pS[:], AF.Exp, scale=0.125), [pS], [pt])
                            if kblk == qb:
                                V(lambda e: e.tensor_tensor(pt[:], pt[:], tri[:], ALU.mult), [pt, tri], [pt])
                            for qt in range(2):
                                for kc in range(2):
                                    c = kblk * 2 + kc
                                    PE(lambda e, qt=qt, kc=kc, c=c: e.matmul(pO[:, qt, :], pt[:, kc, qt * 128:(qt + 1) * 128], V_[:, c, h2, :],
                                                                             start=(kc == 0), stop=(kc == 1)), [pt, V_], [pO])
                            for qt in range(2):
                                sc = sel_[:, qbl * 2 + qt, h2, kblk:kblk + 1]
                                if kblk == 0:
                                    V(lambda e, qt=qt, sc=sc: e.tensor_scalar_mul(acc_[:, qt, :], pO[:, qt, :], sc), [pO, sel_], [acc_])
                                else:
                                    V(lambda e, qt=qt, sc=sc: e.scalar_tensor_tensor(acc_[:, qt, :], pO[:, qt, :], sc, acc_[:, qt, :],
                                                                                     ALU.mult, ALU.add), [pO, sel_, acc_], [acc_])
                        kb_body()
                    ob_ = ob[cn["b"] % 4]
                    cn["b"] += 1
                    V(lambda e: e.reciprocal(rc_[:], acc_[:, :, 64]), [acc_], [rc_])
                    for qt in range(2):
                        V(lambda e, qt=qt: e.tensor_scalar_mul(ob_[:, qt, :], acc_[:, qt, 0:64], rc_[:, qt:qt + 1]), [acc_, rc_], [ob_])
                    hh = pr * 2 + h2
                    dst = g.OM.t[qbl * 256:(qbl + 1) * 256, hh * 64:(hh + 1) * 64].rearrange("(a p) d -> p a d", p=128)
                    kb.dma("sp", dst, ob_[:], [ob_], [g.OM], ob_)
                qb_body()

    for pr in range(4):
        pair_body(pr)
    kb.end()


def phase_c(kb, g, nchunks=NT // 64):
    kb.begin()
    V = lambda fn, r, w: kb.op("dve", fn, r, w)
    A = lambda fn, r, w: kb.op("act", fn, r, w)
    PE = lambda fn, r, w: kb.op("pe", fn, r, w)
    PL = lambda fn, r, w: kb.op("pool", fn, r, w)
    I_ = g.ident_f
    Lbd = kb.sbuf("Lbd", [128, 128], F32)
    Msu = kb.sbuf("Msu", [128, 128], F32)
    Msl = kb.sbuf("Msl", [128, 128], F32)
    Obd = kb.sbuf("Obd", [128, 128], F32)
    ind2 = kb.sbuf("ind2", [128, 2], F32)
    for t_, op_ in ((Lbd, ALU.is_ge), (Msu, ALU.is_gt)):
        PL(lambda e, t_=t_: e.memset(t_[:], 1.0), [], [t_])
        PL(lambda e, t_=t_, op_=op_: e.affine_select(out=t_[:], in_=t_[:], pattern=[[1, 128]], compare_op=op_, fill=0.0,
                                                     base=0, channel_multiplier=-1), [t_], [t_])
        PL(lambda e, t_=t_: e.memset(t_[0:64, 64:128], 0.0), [], [t_])
    PL(lambda e: e.memset(Msl[:], 1.0), [], [Msl])
    PL(lambda e: e.affine_select(out=Msl[:], in_=Msl[:], pattern=[[-1, 128]], compare_op=ALU.is_gt, fill=0.0,
                                 base=0, channel_multiplier=1), [Msl], [Msl])
    PL(lambda e: e.memset(Msl[64:128, 0:64], 0.0), [], [Msl])
    PL(lambda e: e.memset(Obd[:], 0.0), [], [Obd])
    PL(lambda e: e.memset(Obd[0:64, 0:64], 1.0), [], [Obd])
    PL(lambda e: e.memset(Obd[64:128, 64:128], 1.0), [], [Obd])
    PL(lambda e: e.memset(ind2[:], 0.0), [], [ind2])
    PL(lambda e: e.memset(ind2[0:64, 0:1], 1.0), [], [ind2])
    PL(lambda e: e.memset(ind2[64:128, 1:2], 1.0), [], [ind2])
    cst = kb.sbuf("cst", [128, 3, 4, 64], F32)
    for hp in range(4):
        for h2 in range(2):
            hh = hp * 2 + h2
            for j, src in enumerate((g.rwkv_ln_g, g.rwkv_ln_b, g.rwkv_r_k)):
                kb.dma("sp", cst[h2 * 64:(h2 + 1) * 64, j, hp, :], src.t[:, hh * 64:(hh + 1) * 64].partition_broadcast(64),
                       [src], [cst], cst)
    ldb = [kb.sbuf(f"cld{i}", [128, 4, 6, 64], F32) for i in range(2)]
    gtb = [kb.sbuf(f"cgt{i}", [128, 4, 64], F32) for i in range(2)]
    E = kb.sbuf("cE", [128, 4, 4, 64], F32)
    X = kb.sbuf("cX", [128, 4, 4, 64], F32)
    TA = kb.sbuf("cTA", [128, 4, 4, 64], F32)
    BK = kb.sbuf("cBK", [128, 4, 2, 64], F32)
    Vbd = kb.sbuf("cVbd", [128, 4, 128], F32)
    Ubd = kb.sbuf("cUbd", [128, 4, 128], F32)
    TT = kb.sbuf("cTT", [64, 4, 4, 128], F32)
    AA = [kb.sbuf(f"cAA{i}", [128, 4, 2, 128], F32) for i in range(2)]
    AXm = kb.sbuf("cAX", [128, 4, 3, 128], F32)
    Y = [kb.sbuf(f"cY{i}", [128, 4, 128], F32) for i in range(2)]
    WT = kb.sbuf("cWT", [64, 4, 128], F32)
    PC = kb.sbuf("cPC", [64, 4, 2], F32)
    hs = [kb.sbuf(f"ch{i}", [64, 4, 128], F32) for i in range(2)]
    htmp = kb.sbuf("chtmp", [64, 4, 128], F32)
    O = kb.sbuf("cO", [128, 4, 64], F32)
    stt = kb.sbuf("cst2", [128, 8, 4], F32)
    t1 = kb.sbuf("ct1", [128, 4, 64], F32)
    t2 = kb.sbuf("ct2", [128, 4, 64], F32)
    obb = [kb.sbuf(f"cob{i}", [128, 4, 64], BF16) for i in range(2)]
    psA = kb.psum("cps", [128, 4, 2, 512], F32)

    class PB:
        def __init__(self, b):
            self.buf = Buf(None, f"cpsbank{b}")
            self.b = b

        def s(self, hp, sl, rows=slice(0, 128)):
            return psA.t[rows, hp, self.b, sl]

        def all(self, sl, rows=slice(0, 128)):
            return psA.t[rows, :, self.b, sl]
    P0, P1 = PB(0), PB(1)
    PL(lambda e: e.memset(Vbd[:], 0.0), [], [Vbd])
    PL(lambda e: e.memset(Ubd[:], 0.0), [], [Ubd])
    PL(lambda e: e.memset(hs[0][:], 0.0), [], [hs[0]])
    b4 = lambda m: bc(m[:].unsqueeze(1), [128, 4, 128])
    orw4 = g.ORW.t.rearrange("t (hp h2 n) -> t hp h2 n", hp=4, h2=2)
    gr4 = g.GR.t.rearrange("t (hp h2 n) -> t hp h2 n", hp=4, h2=2)

    def chunk(c):
        own = c >= (NT - NOWN) // 64
        ld = ldb[c % 2]
        gt = gtb[c % 2]
        hcur, hnew = hs[c % 2], hs[(c + 1) % 2]
        to = c * 64 - (NT - NOWN)
        for hp in range(4):
            for h2 in range(2):
                src = g.RS.t[:, hp * 2 + h2, c * 64:(c + 1) * 64, :].rearrange("s t n -> t s n")
                kb.dma("sp", ld[h2 * 64:(h2 + 1) * 64, hp, :, :], src, [g.RS], [ld], ld)
        if own:
            for h2 in range(2):
                kb.dma("sp", gt[h2 * 64:(h2 + 1) * 64, :, :], gr4[to:to + 64, :, h2, :], [g.GR], [gt], gt)
        for hp in range(4):
            PE(lambda e, hp=hp: e.matmul(P0.s(hp, slice(0, 64)), Lbd[:], ld[:, hp, 1, :], start=True, stop=True), [Lbd, ld], [P0.buf])
            PE(lambda e, hp=hp: e.matmul(P0.s(hp, slice(64, 128)), Obd[:], ld[:, hp, 1, :], start=True, stop=True), [Obd, ld], [P0.buf])
            PE(lambda e, hp=hp: e.matmul(P0.s(hp, slice(128, 130), slice(0, 64)), ld[:, hp, 1, :], ind2[:], start=True, stop=True),
               [ld, ind2], [P0.buf])
        V(lambda e: e.tensor_copy(E[:, :, 0, :], P0.all(slice(0, 64))), [P0.buf], [E])
        V(lambda e: e.tensor_scalar_mul(E[:, :, 1, :], P0.all(slice(0, 64)), -1.0), [P0.buf], [E])
        V(lambda e: e.tensor_tensor(E[:, :, 2, :], P0.all(slice(0, 64)), ld[:, :, 1, :], ALU.subtract), [P0.buf, ld], [E])
        V(lambda e: e.tensor_tensor(E[:, :, 3, :], P0.all(slice(64, 128)), E[:, :, 0, :], ALU.subtract), [P0.buf, E], [E])
        A(lambda e: e.activation(X[:], E[:], AF.Exp), [E], [X])
        A(lambda e: e.activation(PC[:], P0.all(slice(128, 130), slice(0, 64)), AF.Exp), [P0.buf], [PC])
        V(lambda e: e.tensor_tensor(TA[:, :, 0, :], ld[:, :, 4, :], X[:, :, 2, :], ALU.mult), [ld, X], [TA])
        V(lambda e: e.tensor_tensor(TA[:, :, 1, :], ld[:, :, 0, :], X[:, :, 0, :], ALU.mult), [ld, X], [TA])
        V(lambda e: e.tensor_tensor(TA[:, :, 2, :], ld[:, :, 5, :], X[:, :, 1, :], ALU.mult), [ld, X], [TA])
        V(lambda e: e.tensor_tensor(TA[:, :, 3, :], ld[:, :, 2, :], X[:, :, 1, :], ALU.mult), [ld, X], [TA])
        PL(lambda e: e.tensor_tensor(BK[:, :, 0, :], ld[:, :, 5, :], X[:, :, 3, :], ALU.mult), [ld, X], [BK])
        PL(lambda e: e.tensor_tensor(BK[:, :, 1, :], ld[:, :, 2, :], X[:, :, 3, :], ALU.mult), [ld, X], [BK])
        PL(lambda e: e.tensor_copy(Vbd[0:64, :, 0:64], ld[0:64, :, 3, :]), [ld], [Vbd])
        PL(lambda e: e.tensor_copy(Vbd[64:128, :, 64:128], ld[64:128, :, 3, :]), [ld], [Vbd])
        for hp in range(4):
            for j in range(4):
                PE(lambda e, hp=hp, j=j: e.transpose(P1.s(hp, slice(j * 128, (j + 1) * 128), slice(0, 64)), TA[:, hp, j, :], I_[:]),
                   [TA, I_], [P1.buf])
        A(lambda e: e.copy(TT[:].rearrange("p a j t -> p a (j t)"), P1.all(slice(0, 512), slice(0, 64))), [P1.buf], [TT])
        for hp in range(4):
            AtT, RtT, BtT, KtT = TT[:, hp, 0, :], TT[:, hp, 1, :], TT[:, hp, 2, :], TT[:, hp, 3, :]
            PE(lambda e, hp=hp, a=AtT, b=BtT: e.matmul(P0.s(hp, slice(0, 128)), a, b, start=True, stop=True), [TT], [P0.buf])
            PE(lambda e, hp=hp, a=BtT, b=AtT: e.matmul(P0.s(hp, slice(128, 256)), a, b, start=True, stop=True), [TT], [P0.buf])
            PE(lambda e, hp=hp, a=KtT, b=AtT: e.matmul(P0.s(hp, slice(256, 384)), a, b, start=True, stop=True), [TT], [P0.buf])
            PE(lambda e, hp=hp, a=BtT, b=RtT: e.matmul(P0.s(hp, slice(384, 512)), a, b, start=True, stop=True), [TT], [P0.buf])
            PE(lambda e, hp=hp, a=KtT, b=RtT: e.matmul(P1.s(hp, slice(0, 128)), a, b, start=True, stop=True), [TT], [P1.buf])
        V(lambda e: e.tensor_tensor(AA[0][:, :, 0, :], P0.all(slice(0, 128)), b4(Msl), ALU.mult), [P0.buf, Msl], [AA[0]])
        V(lambda e: e.tensor_tensor(AA[0][:, :, 1, :], P0.all(slice(128, 256)), b4(Msu), ALU.mult), [P0.buf, Msu], [AA[0]])
        V(lambda e: e.tensor_tensor(AXm[:, :, 0, :], P0.all(slice(256, 384)), b4(Msu), ALU.mult), [P0.buf, Msu], [AXm])
        V(lambda e: e.tensor_tensor(AXm[:, :, 1, :], P0.all(slice(384, 512)), b4(Lbd), ALU.mult), [P0.buf, Lbd], [AXm])
        V(lambda e: e.tensor_tensor(AXm[:, :, 2, :], P1.all(slice(0, 128)), b4(Lbd), ALU.mult), [P1.buf, Lbd], [AXm])
        for hp in range(4):
            PE(lambda e, hp=hp: e.matmul(P1.s(hp, slice(128, 192)), AXm[:, hp, 0, :], ld[:, hp, 3, :], start=True, stop=True), [AXm, ld], [P1.buf])
        A(lambda e: e.copy(Y[0][:, :, 0:64], TA[:, :, 0, :]), [TA], [Y[0]])
        A(lambda e: e.copy(Y[0][:, :, 64:128], P1.all(slice(128, 192))), [P1.buf], [Y[0]])
        for lev in range(6):
            a, b = lev % 2, (lev + 1) % 2
            pp = P0 if lev % 2 == 0 else P1
            for hp in range(4):
                PE(lambda e, hp=hp, a=a, pp=pp: e.matmul(pp.s(hp, slice(0, 128)), AA[a][:, hp, 1, :], Y[a][:, hp, :], start=True, stop=True),
                   [AA[a], Y[a]], [pp.buf])
            V(lambda e, a=a, b=b, pp=pp: e.tensor_tensor(Y[b][:], pp.all(slice(0, 128)), Y[a][:], ALU.add), [pp.buf, Y[a]], [Y[b]])
            if lev < 5:
                for hp in range(4):
                    PE(lambda e, hp=hp, a=a, pp=pp: e.matmul(pp.s(hp, slice(128, 256)), AA[a][:, hp, 1, :], AA[a][:, hp, 0, :], start=True, stop=True),
                       [AA[a]], [pp.buf])
                    PE(lambda e, hp=hp, a=a, pp=pp: e.matmul(pp.s(hp, slice(256, 384)), AA[a][:, hp, 0, :], AA[a][:, hp, 1, :], start=True, stop=True),
                       [AA[a]], [pp.buf])
                A(lambda e, b=b, pp=pp: e.copy(AA[b][:].rearrange("p h a t -> p h (a t)"), pp.all(slice(128, 384))), [pp.buf], [AA[b]])
        Xf = Y[0]
        for hp in range(4):
            PE(lambda e, hp=hp: e.transpose(P0.s(hp, slice(0, 128), slice(0, 64)), Xf[:, hp, 0:64], I_[:]), [Xf, I_], [P0.buf])
        A(lambda e: e.copy(WT[:], P0.all(slice(0, 128), slice(0, 64))), [P0.buf], [WT])
        for hp in range(4):
            PE(lambda e, hp=hp: e.matmul(P0.s(hp, slice(128, 256)), WT[:, hp, :], hcur[:, hp, :], start=True, stop=True), [WT, hcur], [P0.buf])
        V(lambda e: e.tensor_tensor(Ubd[0:64, :, 0:64], P0.all(slice(128, 192), slice(0, 64)), Xf[0:64, :, 64:128], ALU.add), [P0.buf, Xf], [Ubd])
        V(lambda e: e.tensor_tensor(Ubd[64:128, :, 64:128], P0.all(slice(192, 256), slice(64, 128)), Xf[64:128, :, 64:128], ALU.add),
          [P0.buf, Xf], [Ubd])
        if own:
            for hp in range(4):
                PE(lambda e, hp=hp: e.matmul(P1.s(hp, slice(0, 128)), TT[:, hp, 1, :], hcur[:, hp, :], start=True, stop=False), [TT, hcur], [P1.buf])
                PE(lambda e, hp=hp: e.matmul(P1.s(hp, slice(0, 128)), AXm[:, hp, 1, :], Ubd[:, hp, :], start=False, stop=False), [AXm, Ubd], [P1.buf])
                PE(lambda e, hp=hp: e.matmul(P1.s(hp, slice(0, 128)), AXm[:, hp, 2, :], Vbd[:, hp, :], start=False, stop=True), [AXm, Vbd], [P1.buf])
            A(lambda e: e.copy(O[0:64, :, :], P1.all(slice(0, 64), slice(0, 64))), [P1.buf], [O])
            A(lambda e: e.copy(O[64:128, :, :], P1.all(slice(64, 128), slice(64, 128))), [P1.buf], [O])
        for hp in range(4):
            PE(lambda e, hp=hp: e.matmul(P0.s(hp, slice(256, 384), slice(0, 64)), BK[:, hp, 0, :], Ubd[:, hp, :], start=True, stop=False),
               [BK, Ubd], [P0.buf])
            PE(lambda e, hp=hp: e.matmul(P0.s(hp, slice(256, 384), slice(0, 64)), BK[:, hp, 1, :], Vbd[:, hp, :], start=False, stop=True),
               [BK, Vbd], [P0.buf])
        V(lambda e: e.tensor_tensor(htmp[:].rearrange("p h (a v) -> p h a v", a=2), hcur[:].rearrange("p h (a v) -> p h a v", a=2),
                                    bc(PC[:].unsqueeze(3), [64, 4, 2, 64]), ALU.mult), [hcur, PC], [htmp])
        V(lambda e: e.tensor_tensor(hnew[:], htmp[:], P0.all(slice(256, 384), slice(0, 64)), ALU.add), [htmp, P0.buf], [hnew])
        if not own:
            return
        ob = obb[c % 2]
        b64 = lambda ap: bc(ap.unsqueeze(2), [128, 4, 64])
        V(lambda e: e.tensor_reduce(stt[:, 0, :], O[:], AX.X, ALU.add), [O], [stt])
        V(lambda e: e.tensor_scalar_mul(stt[:, 1, :], stt[:, 0, :], 1.0 / 64.0), [stt], [stt])
        V(lambda e: e.tensor_tensor(t1[:], O[:], b64(stt[:, 1, :]), ALU.subtract), [O, stt], [t1])
        A(lambda e: e.activation(t2[:], t1[:], AF.Square), [t1], [t2])
        V(lambda e: e.tensor_reduce(stt[:, 2, :], t2[:], AX.X, ALU.add), [t2], [stt])
        V(lambda e: e.tensor_scalar(stt[:, 3, :], stt[:, 2, :], 1.0 / 64.0, GN_EPS_, ALU.mult, ALU.add), [stt], [stt])
        A(lambda e: e.activation(stt[:, 4, :], stt[:, 3, :], AF.Sqrt), [stt], [stt])
        V(lambda e: e.reciprocal(stt[:, 4, :], stt[:, 4, :]), [stt], [stt])
        V(lambda e: e.tensor_tensor(t1[:], t1[:], b64(stt[:, 4, :]), ALU.mult), [t1, stt], [t1])
        V(lambda e: e.tensor_tensor(t1[:], t1[:], cst[:, 0], ALU.mult), [t1, cst], [t1])
        V(lambda e: e.tensor_tensor(t1[:], t1[:], cst[:, 1], ALU.add), [t1, cst], [t1])
        PL(lambda e: e.tensor_tensor(t2[:], ld[:, :, 0, :], ld[:, :, 2, :], ALU.mult), [ld], [t2])
        PL(lambda e: e.tensor_tensor(t2[:], t2[:], cst[:, 2], ALU.mult), [t2, cst], [t2])
        V(lambda e: e.tensor_reduce(stt[:, 5, :], t2[:], AX.X, ALU.add), [t2], [stt])
        V(lambda e: e.tensor_tensor(t2[:], ld[:, :, 3, :], b64(stt[:, 5, :]), ALU.mult), [ld, stt], [t2])
        V(lambda e: e.tensor_tensor(t1[:], t1[:], t2[:], ALU.add), [t1, t2], [t1])
        V(lambda e: e.tensor_tensor(ob[:], t1[:], gt[:], ALU.mult), [t1, gt], [ob])
        for h2 in range(2):
            kb.dma("sp", orw4[to:to + 64, :, h2, :], ob[h2 * 64:(h2 + 1) * 64, :, :], [ob], [g.ORW], ob)

    for c in range(nchunks):
        chunk(c)
    kb.end()


GN_EPS_ = 64e-5


def phase_p(kb, g, nblk=128):
    kb.begin()
    V = lambda fn, r, w: kb.op("dve", fn, r, w)
    A = lambda fn, r, w: kb.op("act", fn, r, w)
    PE = lambda fn, r, w: kb.op("pe", fn, r, w)
    PL = lambda fn, r, w: kb.op("pool", fn, r, w)
    uf = [kb.sbuf(f"uf{i}", [128, 1024], F32) for i in range(3)]
    ub = [kb.sbuf(f"ub{i}", [128, 1024], BF16) for i in range(3)]
    ut = [kb.sbuf(f"ut{i}", [128, 8, 128], BF16) for i in range(3)]
    vf = [kb.sbuf(f"vf{i}", [128, 1024], F32) for i in range(3)]
    vb = [kb.sbuf(f"vb{i}", [128, 1024], BF16) for i in range(3)]
    pb = [kb.psum(f"ppb{i}", [128, 1024], BF16) for i in range(3)]

    def body(b):
        i = b % 3
        kb.dma("sp", uf[i][:], g.peer_u[b * 128:(b + 1) * 128, :], [g.peer_u], [uf[i]], uf[i])
        kb.dma("sp", vf[i][:], g.peer_v[b * 128:(b + 1) * 128, :], [g.peer_v], [vf[i]], vf[i])
        V(lambda e: e.tensor_copy(ub[i][:], uf[i][:]), [uf[i]], [ub[i]])
        PL(lambda e: e.tensor_copy(vb[i][:], vf[i][:]), [vf[i]], [vb[i]])
        kb.dma("sp", g.VB[b * 128:(b + 1) * 128, :], vb[i][:], [vb[i]], [g.VB], vb[i])
        for k in range(8):
            PE(lambda e, k=k: e.transpose(pb[i][:, k * 128:(k + 1) * 128], ub[i][:, k * 128:(k + 1) * 128], g.ident_b[:]),
               [ub[i], g.ident_b], [pb[i]])
        A(lambda e: e.copy(ut[i][:].rearrange("p k e -> p (k e)"), pb[i][:]), [pb[i]], [ut[i]])
        kb.dma("sp", g.UT.t[:, :, b * 128:(b + 1) * 128].rearrange("k p e -> p k e"), ut[i][:], [ut[i]], [g.UT], ut[i])
    for b in range(nblk):
        body(b)
    kb.end()


def phase_d(kb, g, ntl=NOWN // 128):
    kb.begin()
    V = lambda fn, r, w: kb.op("dve", fn, r, w)
    A = lambda fn, r, w: kb.op("act", fn, r, w)
    PE = lambda fn, r, w: kb.op("pe", fn, r, w)
    PL = lambda fn, r, w: kb.op("pool", fn, r, w)
    Wpm = kb.sbuf("Wpm", [128, 4, 1024], BF16)
    Wpr = kb.sbuf("Wpr", [128, 4, 1024], BF16)
    Wo = kb.sbuf("Wo", [128, 8, 1024], BF16)
    Wq = kb.sbuf("Wq", [128, 8, 2048], BF16)
    skT = kb.sbuf("skT", [128, 16, 128], BF16)
    kb.begin()
    stg = [kb.sbuf(f"dstg{i}", [128, 4, 1024], F32) for i in range(2)]
    psk = kb.psum("psk", [128, 512], F32)
    n = [0]

    def ldw(dst, src, k0, nk, c0, nc_):
        s_ = stg[n[0] % 2]
        n[0] += 1
        kb.dma("sp", s_[:, 0:nk, 0:nc_], src.t.rearrange("(k p) n -> p k n", p=128)[:, k0:k0 + nk, c0:c0 + nc_], [src], [s_], s_)
        if n[0] % 2:
            V(lambda e: e.tensor_copy(dst[:, k0:k0 + nk, c0:c0 + nc_], s_[:, 0:nk, 0:nc_]), [s_], [dst])
        else:
            A(lambda e: e.copy(dst[:, k0:k0 + nk, c0:c0 + nc_], s_[:, 0:nk, 0:nc_]), [s_], [dst])
    ldw(Wpm, g.w_proj_moba, 0, 4, 0, 1024)
    ldw(Wpr, g.w_proj_rwkv, 0, 4, 0, 1024)
    ldw(Wo, g.w_out, 0, 4, 0, 1024)
    ldw(Wo, g.w_out, 4, 4, 0, 1024)
    for k0 in (0, 4):
        for c0 in (0, 1024):
            ldw(Wq, g.peer_wq, k0, 4, c0, 1024)
    for hp in range(16):
        s_ = stg[n[0] % 2]
        n[0] += 1
        kb.dma("sp", s_[:, 0, 0:128], g.peer_sk[hp], [g.peer_sk], [s_], s_)
        PE(lambda e, s_=s_: e.transpose(psk[:, 0:128], s_[:, 0, 0:128], g.ident_f[:]), [s_, g.ident_f], [psk])
        V(lambda e, hp=hp: e.tensor_copy(skT[:, hp, :], psk[:, 0:128]), [psk], [skT])
    kb.end()

    NB = 2
    om = [kb.sbuf(f"om{i}", [128, 2, 512], BF16) for i in range(NB)]
    gts = [kb.sbuf(f"gts{i}", [128, 2048], BF16) for i in range(NB)]
    xo = [kb.sbuf(f"xo{i}", [128, 1024], F32) for i in range(NB)]
    oT = [kb.sbuf(f"oT{i}", [128, 8, 128], BF16) for i in range(NB)]
    m1 = [kb.sbuf(f"dm1{i}", [128, 1024], F32) for i in range(NB)]
    mix = [kb.sbuf(f"mix{i}", [128, 1024], BF16) for i in range(NB)]
    mixT = [kb.sbuf(f"mixT{i}", [128, 8, 128], BF16) for i in range(NB)]
    h1 = [kb.sbuf(f"h1{i}", [128, 1024], F32) for i in range(NB)]
    junk = kb.sbuf("djunk", [128, 1024], BF16)
    stt = [kb.sbuf(f"dstt{i}", [128, 4], F32) for i in range(NB)]
    xn2T = [kb.sbuf(f"xn2T{i}", [128, 8, 128], BF16) for i in range(NB)]
    qT = [kb.sbuf(f"qT{i}", [128, 16, 128], BF16) for i in range(NB)]
    S = [kb.sbuf(f"S{i}", [128, 16, 128], F32) for i in range(NB)]
    S2 = [kb.sbuf(f"S2{i}", [128, 128], F32) for i in range(NB)]
    top = [kb.sbuf(f"top{i}", [128, 16, 16], F32) for i in range(NB)]
    cand = [kb.sbuf(f"cand{i}", [128, 8, 256], F32) for i in range(NB)]
    c2 = [kb.sbuf(f"c2{i}", [128, 256], F32) for i in range(NB)]
    t16 = [kb.sbuf(f"t16{i}", [128, 8, 16], F32) for i in range(NB)]
    thr = [kb.sbuf(f"thr{i}", [128, 4, 8], F32) for i in range(NB)]
    pB = [kb.psum(f"dpB{i}", [128, 1024], BF16) for i in range(2)]
    pM = [kb.psum(f"dpM{i}", [128, 512], F32) for i in range(4)]
    pT = [kb.psum(f"dpT{i}", [128, 512], F32) for i in range(2)]
    cn = {"m": 0}

    def nM():
        cn["m"] += 1
        return pM[cn["m"] % 4]

    def body(it):
        i = it % NB
        t0 = it * 128
        om_, g_, x_, oT_, m1_, mix_, mixT_, h1_, st_, xn_, qT_, S_, S2_, top_, cand_, c2_, t16_, thr_ = (
            om[i], gts[i], xo[i], oT[i], m1[i], mix[i], mixT[i], h1[i], stt[i], xn2T[i], qT[i], S[i], S2[i], top[i], cand[i],
            c2[i], t16[i], thr[i])
        kb.dma("sp", om_[:, 0, :], g.OM[t0:t0 + 128, :], [g.OM], [om_], om_)
        kb.dma("sp", om_[:, 1, :], g.ORW[t0:t0 + 128, :], [g.ORW], [om_], om_)
        kb.dma("sp", g_[:], g.GATES[t0:t0 + 128, :], [g.GATES], [g_], g_)
        kb.dma("sp", x_[:], g.xs[NT - NOWN + t0:NT - NOWN + t0 + 128, :], [g.xs], [x_], x_)
        pb = pB[it % 2]
        for j in range(8):
            PE(lambda e, j=j: e.transpose(pb[:, j * 128:(j + 1) * 128], om_[:, j // 4, (j % 4) * 128:(j % 4 + 1) * 128], g.ident_b[:]),
               [om_, g.ident_b], [pb])
        A(lambda e: e.copy(oT_[:].rearrange("p k t -> p (k t)"), pb[:]), [pb], [oT_])
        for br, W_ in ((0, Wpm), (1, Wpr)):
            for hf in range(2):
                p_ = nM()
                for k in range(4):
                    PE(lambda e, k=k, p_=p_, W_=W_, br=br, hf=hf: e.matmul(p_[:], oT_[:, br * 4 + k, :], W_[:, k, hf * 512:(hf + 1) * 512],
                                                                           start=(k == 0), stop=(k == 3)), [oT_, W_], [p_])
                cs_ = slice(hf * 512, (hf + 1) * 512)
                gs_ = slice(br * 1024 + hf * 512, br * 1024 + (hf + 1) * 512)
                if br == 0:
                    V(lambda e, p_=p_, cs_=cs_, gs_=gs_: e.tensor_tensor(m1_[:, cs_], p_[:], g_[:, gs_], ALU.mult), [p_, g_], [m1_])
                else:
                    V(lambda e, p_=p_, cs_=cs_, gs_=gs_: e.tensor_tensor(h1_[:, cs_], p_[:], g_[:, gs_], ALU.mult), [p_, g_], [h1_])
                    PL(lambda e, cs_=cs_: e.tensor_tensor(mix_[:, cs_], m1_[:, cs_], h1_[:, cs_], ALU.add), [m1_, h1_], [mix_])
        pb2 = pB[(it + 1) % 2]
        for j in range(8):
            PE(lambda e, j=j: e.transpose(pb2[:, j * 128:(j + 1) * 128], mix_[:, j * 128:(j + 1) * 128], g.ident_b[:]),
               [mix_, g.ident_b], [pb2])
        A(lambda e: e.copy(mixT_[:].rearrange("p k t -> p (k t)"), pb2[:]), [pb2], [mixT_])
        for hf in range(2):
            p_ = nM()
            for k in range(8):
                PE(lambda e, k=k, p_=p_, hf=hf: e.matmul(p_[:], mixT_[:, k, :], Wo[:, k, hf * 512:(hf + 1) * 512],
                                                         start=(k == 0), stop=(k == 7)), [mixT_, Wo], [p_])
            cs_ = slice(hf * 512, (hf + 1) * 512)
            V(lambda e, p_=p_, cs_=cs_: e.tensor_tensor(h1_[:, cs_], p_[:], g.gt_bc[:, 0, cs_], ALU.mult), [p_, g.gt_bc], [h1_])
            V(lambda e, cs_=cs_: e.tensor_tensor(h1_[:, cs_], h1_[:, cs_], x_[:, cs_], ALU.add), [h1_, x_], [h1_])
        kb.dma("sp", g.H1[t0:t0 + 128, :], h1_[:], [h1_], [g.H1], h1_)
        A(lambda e: e.activation(junk[:], h1_[:], AF.Square, accum_out=st_[:, 0:1]), [h1_], [junk, st_])
        V(lambda e: e.tensor_scalar(st_[:, 1:2], st_[:, 0:1], 1.0 / 1024.0, 1e-6, ALU.mult, ALU.add), [st_], [st_])
        A(lambda e: e.activation(st_[:, 2:3], st_[:, 1:2], AF.Sqrt), [st_], [st_])
        V(lambda e: e.reciprocal(st_[:, 2:3], st_[:, 2:3]), [st_], [st_])
        V(lambda e: e.tensor_scalar_mul(m1_[:], h1_[:], st_[:, 2:3]), [h1_, st_], [m1_])
        for k in range(8):
            pt = pT[k // 4]
            PE(lambda e, k=k, pt=pt: e.transpose(pt[:, (k % 4) * 128:(k % 4 + 1) * 128], m1_[:, k * 128:(k + 1) * 128], g.ident_f[:]),
               [m1_, g.ident_f], [pt])
            A(lambda e, k=k, pt=pt: e.activation(xn_[:, k, :], pt[:, (k % 4) * 128:(k % 4 + 1) * 128], AF.Identity,
                                                 bias=g.modc[:, 24 + k:25 + k], scale=g.sc2p[:, k:k + 1]), [pt, g.modc, g.sc2p], [xn_])
        kb.dma("sp", g.XN2T.t[:, :, t0:t0 + 128].rearrange("k p t -> p k t"), xn_[:], [xn_], [g.XN2T], xn_)
        for q4 in range(4):
            p_ = nM()
            for jj in range(4):
                hp = q4 * 4 + jj
                for k in range(8):
                    PE(lambda e, k=k, p_=p_, hp=hp, jj=jj: e.matmul(p_[:, jj * 128:(jj + 1) * 128], Wq[:, k, hp * 128:(hp + 1) * 128], xn_[:, k, :],
                                                                  start=(k == 0), stop=(k == 7)), [Wq, xn_], [p_])
            A(lambda e, p_=p_, q4=q4: e.copy(qT_[:, q4 * 4:(q4 + 1) * 4, :].rearrange("p a t -> p (a t)"), p_[:]), [p_], [qT_])
        for q4 in range(4):
            p_ = nM()
            for jj in range(4):
                hp = q4 * 4 + jj
                PE(lambda e, p_=p_, hp=hp, jj=jj: e.matmul(p_[:, jj * 128:(jj + 1) * 128], qT_[:, hp, :], skT[:, hp, :], start=True, stop=True),
                   [qT_, skT], [p_])
            V(lambda e, p_=p_, q4=q4: e.tensor_copy(S_[:, q4 * 4:(q4 + 1) * 4, :].rearrange("p a n -> p (a n)"), p_[:]), [p_], [S_])
        kb.dma("sp", g.SC[t0:t0 + 128, :, :], S_[:], [S_], [g.SC], S_)
        for hp in range(16):
            V(lambda e, hp=hp: e.max(out=top_[:, hp, 0:8], in_=S_[:, hp, :]), [S_], [top_])
            V(lambda e, hp=hp: e.match_replace(out=S2_[:], in_to_replace=top_[:, hp, 0:8], in_values=S_[:, hp, :], imm_value=-1e30),
              [S_, top_], [S2_])
            V(lambda e, hp=hp: e.max(out=top_[:, hp, 8:16], in_=S2_[:]), [S2_], [top_])
        t4 = top_[:].rearrange("p (h two) a -> p h two a", two=2)
        V(lambda e: e.tensor_tensor(cand_[:].rearrange("p h (a b) -> p h a b", b=16), bc(t4[:, :, 0, :].unsqueeze(3), [128, 8, 16, 16]),
                                    bc(t4[:, :, 1, :].unsqueeze(2), [128, 8, 16, 16]), ALU.add), [top_], [cand_])
        for h in range(8):
            V(lambda e, h=h: e.max(out=t16_[:, h, 0:8], in_=cand_[:, h, :]), [cand_], [t16_])
            V(lambda e, h=h: e.match_replace(out=c2_[:], in_to_replace=t16_[:, h, 0:8], in_values=cand_[:, h, :], imm_value=-1e30),
              [cand_, t16_], [c2_])
            V(lambda e, h=h: e.max(out=t16_[:, h, 8:16], in_=c2_[:]), [c2_], [t16_])
        V(lambda e: e.tensor_copy(thr_[:, 0, :], t16_[:, :, 15]), [t16_], [thr_])
        V(lambda e: e.tensor_scalar_mul(thr_[:, 1, :], t16_[:, :, 0], -1.0), [t16_], [thr_])
        V(lambda e: e.tensor_tensor(t16_[:], t16_[:], bc(thr_[:, 1, :].unsqueeze(2), [128, 8, 16]), ALU.add), [t16_, thr_], [t16_])
        A(lambda e: e.activation(t16_[:], t16_[:], AF.Exp), [t16_], [t16_])
        V(lambda e: e.tensor_reduce(thr_[:, 3, :], t16_[:], AX.X, ALU.add), [t16_], [thr_])
        V(lambda e: e.reciprocal(thr_[:, 2, :], thr_[:, 3, :]), [thr_], [thr_])
        kb.dma("sp", g.TH[t0:t0 + 128, :, :], thr_[:], [thr_], [g.TH], thr_)
    for it in range(ntl):
        body(it)
    kb.end()


def phase_e(kb, g, ngroups=NOWN // 256, nec=16):
    kb.begin()
    V = lambda fn, r, w: kb.op("dve", fn, r, w)
    A = lambda fn, r, w: kb.op("act", fn, r, w)
    PE = lambda fn, r, w: kb.op("pe", fn, r, w)
    PL = lambda fn, r, w: kb.op("pool", fn, r, w)
    UTs = [kb.sbuf(f"UTs{i}", [128, 8, 1024], BF16) for i in range(2)]
    VBs = [kb.sbuf(f"VBs{i}", [128, 8, 1024], BF16) for i in range(2)]
    xn = [kb.sbuf(f"exn{i}", [128, 8, 256], BF16) for i in range(2)]
    Ssb = [kb.sbuf(f"eS{i}", [128, 2, 16, 128], F32) for i in range(1)]
    th = [kb.sbuf(f"eth{i}", [128, 2, 4, 8], F32) for i in range(2)]
    Dg = [kb.sbuf(f"eDg{i}", [128, 2, 8, 128], BF16) for i in range(2)]
    Zt = [kb.sbuf(f"eZ{i}", [128, 1024], F32) for i in range(3)]
    Et = [kb.sbuf(f"eE{i}", [128, 1024], BF16) for i in range(3)]
    Mt = [[[kb.sbuf(f"eM{b}_{tt}_{h}", [128, 1024], BF16) for h in range(8)] for tt in range(2)] for b in range(2)]
    gl = [kb.sbuf(f"egl{i}", [128, 256], BF16) for i in range(3)]
    hd = [kb.sbuf(f"ehd{i}", [128, 256], BF16) for i in range(3)]
    h1t = [kb.sbuf(f"eh1{i}", [128, 2, 1024], F32) for i in range(1)]
    fo = [kb.sbuf(f"efo{i}", [128, 1024], F32) for i in range(2)]
    pO = [kb.psum(f"epO{i}", [128, 512], F32) for i in range(4)]
    pS = [kb.psum(f"epS{i}", [128, 512], F32) for i in range(2)]
    pG = [kb.psum(f"epG{i}", [128, 512], F32) for i in range(2)]
    cn = {"z": 0, "s": 0, "g": 0, "h": 0}
    utv = g.UT.t.rearrange("k p e -> p k e")
    vbv = g.VB.t.rearrange("(b p) d -> p b d", p=128)

    class G_:
        pass

    def setup_group(tg):
        c = G_()
        i = tg % 2
        c.t0 = tg * 256
        c.xn, c.S, c.th, c.Dg, c.h1 = xn[i], Ssb[0], th[i], Dg[i], h1t[0]
        t0 = c.t0
        kb.dma("sp", c.xn[:], g.XN2T.t[:, :, t0:t0 + 256].rearrange("k p t -> p k t"), [g.XN2T], [c.xn], c.xn)
        for tt in range(2):
            kb.dma("sp", c.S[:, tt], g.SC[t0 + tt * 128:t0 + (tt + 1) * 128, :, :], [g.SC], [c.S], c.S)
            kb.dma("sp", c.th[:, tt], g.TH[t0 + tt * 128:t0 + (tt + 1) * 128, :, :], [g.TH], [c.th], c.th)
            for h in range(8):
                V(lambda e, tt=tt, h=h: e.tensor_scalar_mul(c.Dg[:, tt, h, :], g.ident_f[:], c.th[:, tt, 2, h:h + 1]), [g.ident_f, c.th], [c.Dg])
        return c

    def gate_tasks(c, k, ec):
        w = k % 2
        U_, Vb_ = UTs[w], VBs[w]
        tasks = []

        def loadw():
            kb.dma("sp", U_[:], utv[:, :, ec * 1024:(ec + 1) * 1024], [g.UT], [U_], U_)
            kb.dma("sp", Vb_[:], vbv[:, ec * 8:(ec + 1) * 8, :], [g.VB], [Vb_], Vb_)
        for tt in range(2):
            for h in range(8):
                def gate(tt=tt, h=h, first=(tt == 0 and h == 0)):
                    if first:
                        loadw()
                    z = cn["z"] % 3
                    cn["z"] += 1
                    Z_, E_, M_ = Zt[z], Et[z], Mt[w][tt][h]
                    PL(lambda e: e.tensor_tensor(Z_[:].rearrange("p (a b) -> p a b", b=128),
                                                 bc(c.S[:, tt, 2 * h, ec * 8:(ec + 1) * 8].unsqueeze(2), [128, 8, 128]),
                                                 bc(c.S[:, tt, 2 * h + 1, :].unsqueeze(1), [128, 8, 128]), ALU.add), [c.S], [Z_])
                    A(lambda e: e.activation(E_[:], Z_[:], AF.Exp, bias=c.th[:, tt, 1, h:h + 1]), [Z_, c.th], [E_])
                    V(lambda e: e.scalar_tensor_tensor(M_[:], Z_[:], c.th[:, tt, 0, h:h + 1], E_[:], ALU.is_ge, ALU.mult),
                      [Z_, c.th, E_], [M_])
                tasks.append(gate)
        return tasks

    def blk_tasks(c, k, ec):
        w = k % 2
        U_, Vb_ = UTs[w], VBs[w]
        tasks = []
        for ib in range(8):
            def blk(ib=ib):
                eb = ec * 8 + ib
                ps_ = pS[cn["s"] % 2]
                pg_ = pG[cn["g"] % 2]
                cn["s"] += 1
                cn["g"] += 1
                for kk in range(8):
                    PE(lambda e, kk=kk: e.matmul(ps_[:, 0:256], U_[:, kk, ib * 128:(ib + 1) * 128], c.xn[:, kk, :], start=(kk == 0), stop=(kk == 7)),
                       [U_, c.xn], [ps_])
                for tt in range(2):
                    for h in range(8):
                        M_ = Mt[w][tt][h]
                        PE(lambda e, tt=tt, h=h, M_=M_: e.matmul(pg_[:, tt * 128:(tt + 1) * 128], M_[:, ib * 128:(ib + 1) * 128], c.Dg[:, tt, h, :],
                                                               start=(h == 0), stop=(h == 7)), [M_, c.Dg], [pg_])
                j = cn["h"] % 3
                cn["h"] += 1
                A(lambda e: e.activation(gl[j][:], ps_[:, 0:256], AF.Gelu), [ps_], [gl[j]])
                V(lambda e: e.tensor_tensor(hd[j][:], gl[j][:], pg_[:, 0:256], ALU.mult), [gl[j], pg_], [hd[j]])
                for tt in range(2):
                    for hf in range(2):
                        PE(lambda e, tt=tt, hf=hf: e.matmul(pO[tt * 2 + hf][:], hd[j][:, tt * 128:(tt + 1) * 128], Vb_[:, ib, hf * 512:(hf + 1) * 512],
                                                          start=(eb == 0), stop=(eb == nec * 8 - 1)), [hd[j], Vb_], [pO[tt * 2 + hf]])
            tasks.append(blk)
        return tasks

    def finalize(c):
        t0 = c.t0
        for tt in range(2):
            kb.dma("sp", c.h1[:, tt, :], g.H1[t0 + tt * 128:t0 + (tt + 1) * 128, :], [g.H1], [c.h1], c.h1)
        for tt in range(2):
            f_ = fo[tt]
            for hf in range(2):
                cs_ = slice(hf * 512, (hf + 1) * 512)
                V(lambda e, tt=tt, hf=hf, cs_=cs_, f_=f_: e.tensor_tensor(f_[:, cs_], pO[tt * 2 + hf][:], g.gt_bc[:, 1, cs_], ALU.mult),
                  [pO[tt * 2 + hf], g.gt_bc], [f_])
                V(lambda e, tt=tt, cs_=cs_, f_=f_: e.tensor_tensor(f_[:, cs_], f_[:, cs_], c.h1[:, tt, cs_], ALU.add), [f_, c.h1], [f_])
            kb.dma("sp", g.out[t0 + tt * 128:t0 + (tt + 1) * 128, :], f_[:], [f_], [g.out], f_)

    chunks = [(tg, ec) for tg in range(ngroups) for ec in range(nec)]
    ctxs = {}

    def ctx_of(tg):
        if tg not in ctxs:
            ctxs[tg] = setup_group(tg)
        return ctxs[tg]
    tg0, ec0 = chunks[0]
    for t in gate_tasks(ctx_of(tg0), 0, ec0):
        t()
    for k, (tg, ec) in enumerate(chunks):
        c = ctx_of(tg)
        nxt = None
        if k + 1 < len(chunks):
            tgn, ecn = chunks[k + 1]
            nxt = gate_tasks(ctx_of(tgn), k + 1, ecn)
        bt = blk_tasks(c, k, ec)
        for ib in range(8):
            bt[ib]()
            if nxt is not None:
                nxt[2 * ib]()
                nxt[2 * ib + 1]()
        if ec == nec - 1:
            finalize(c)
    kb.end()


def build(dbg=False):
    nc = bass.Bass("TRN2", target_bir_lowering=False)
    gst = ExitStack()
    with gst:
        kb = KB(nc, gst)
        g = declare(kb, dbg)
        phase0(kb, g)
        phase_a(kb, g)
        phase_p(kb, g)
        phase_b(kb, g)
        phase_c(kb, g)
        phase_d(kb, g)
        phase_e(kb, g)
        kb.begin()
        kb.wait_all("sp", [g.out])
        kb.end()
    return nc


def host_inputs(inputs, core, shared):
    b, half = core // 2, core % 2
    x = np.asarray(inputs["x"], dtype=np.float32)
    pos = np.asarray(inputs["positions"]).astype(np.int32)
    xs = np.zeros((NT, 1024), np.float32)
    ps = np.zeros((NT, 1), np.int32)
    if half == 1:
        xs[:] = x[b]
        ps[:, 0] = pos[b]
    else:
        xs[NOWN:] = x[b, :NOWN]
        ps[NOWN:, 0] = pos[b, :NOWN]
    m = dict(shared)
    m["xs"] = xs
    m["pos"] = ps
    m["cc"] = np.ascontiguousarray(np.asarray(inputs["c"], np.float32)[b].reshape(8, 128))
    m["pv"] = np.full((128, 1), float(half), np.float32)
    return m


def shared_inputs(inputs):
    m = {}
    invf = (500000.0 ** (-(np.arange(8, dtype=np.float32) * 2.0) / 16.0)).astype(np.float32)
    m["invf"] = np.ascontiguousarray(np.broadcast_to(invf[None, :], (128, 8)))

    def w(name, shape, key=None):
        m[name] = np.ascontiguousarray(np.asarray(inputs[key or name], np.float32).reshape(shape))
    w("w_ada", (1024, 6144)); w("b_ada", (1, 6144)); w("norm1_g", (1, 1024)); w("w_in", (1024, 5376))
    w("q_norm_g", (1, 64)); w("k_norm_g", (1, 64)); w("rwkv_mu", (1, 1792)); w("rwkv_w0", (1, 512))
    w("rwkv_w2", (64, 512)); w("rwkv_a0", (1, 512)); w("rwkv_a2", (64, 512)); w("rwkv_g2", (128, 512))
    w("rwkv_k_k", (1, 512)); w("rwkv_k_a", (1, 512)); w("rwkv_r_k", (1, 512)); w("rwkv_ln_g", (1, 512))
    w("rwkv_ln_b", (1, 512)); w("w_proj_moba", (512, 1024)); w("w_proj_rwkv", (512, 1024)); w("w_out", (1024, 1024))
    w("norm2_g", (1, 1024)); w("peer_wq", (1024, 2048)); w("peer_sk", (16, 128, 128), "peer_subkeys")
    w("peer_u", (16384, 1024)); w("peer_v", (16384, 1024))
    return m


def kernel(**inputs):
    nc = build(False)
    shared = shared_inputs(inputs)
    in_maps = [host_inputs(inputs, c, shared) for c in range(8)]
    res = run_bass_kernel_spmd(nc, in_maps, core_ids=list(range(8)))
    out = np.zeros((4, 8192, 1024), np.float32)
    for c in range(8):
        b, half = c // 2, c % 2
        out[b, half * NOWN:(half + 1) * NOWN] = np.asarray(res.results[c]["out"], np.float32)
    return out
```

```python
import numpy as np
import concourse.bass as bass
import concourse.mybir as mybir
from concourse.bass_utils import run_bass_kernel_spmd
from contextlib import ExitStack

F32 = mybir.dt.float32
BF16 = mybir.dt.bfloat16
I32 = mybir.dt.int32
AF = mybir.ActivationFunctionType
ALU = mybir.AluOpType
AX = mybir.AxisListType
EPOCH = 24000
P = 128


class Buf:
    def __init__(self, t, name):
        self.t = t
        self.name = name
        self.ws = {}
        self.rs = {}
        self.dkey = None
        self.dcnt = 0

    def __getitem__(self, k):
        return self.t[k]


class KB:
    ENG = ("pe", "act", "dve", "pool", "sp")

    def __init__(self, nc, stack):
        self.nc = nc
        self.stack = stack
        self.phs = []
        self.scope_bufs = []
        self.dpool = []
        self.ops = {e: [] for e in self.ENG}
        self.cnt = {e: 0 for e in self.ENG}
        self.known = {e: {} for e in self.ENG}
        self.sems = {}
        self.nsem = 0
        self.nins = 0

    def sem(self, key):
        if key not in self.sems:
            self.sems[key] = self.stack.enter_context(self.nc.semaphore(f"s{self.nsem}"))
            self.nsem += 1
        return self.sems[key]

    def gsbuf(self, name, shape, dt):
        return Buf(self.stack.enter_context(self.nc.sbuf_tensor(name, list(shape), dt)), name)

    def sbuf(self, name, shape, dt):
        self.nsem += 0
        self.uid = getattr(self, "uid", 0) + 1
        name = f"{name}_u{self.uid}"
        b = Buf(self.phs[-1].enter_context(self.nc.sbuf_tensor(name, list(shape), dt)), name)
        self.scope_bufs[-1].append(b)
        return b

    def psum(self, name, shape, dt):
        self.uid = getattr(self, "uid", 0) + 1
        name = f"{name}_u{self.uid}"
        return Buf(self.phs[-1].enter_context(self.nc.psum_tensor(name, list(shape), dt)), name)

    def dram(self, name, shape, dt, kind="Internal"):
        return Buf(self.nc.dram_tensor(name, list(shape), dt, kind=kind).ap(), name)

    def _deps(self, eng, reads, writes):
        d = {}
        for b in reads:
            for k, v in b.ws.items():
                if d.get(k, 0) < v:
                    d[k] = v
        for b in writes:
            for k, v in b.ws.items():
                if d.get(k, 0) < v:
                    d[k] = v
            for k, v in b.rs.items():
                if d.get(k, 0) < v:
                    d[k] = v
        kn = self.known[eng]
        for k, v in d.items():
            if eng == "pe" and k[0] == "pe":
                continue
            if kn.get(k, 0) >= v:
                continue
            kn[k] = v
            self.ops[eng].append(("wait", k, v))

    def op(self, eng, fn, reads=(), writes=()):
        self._deps(eng, reads, writes)
        self.cnt[eng] += 1
        n = self.cnt[eng]
        key = (eng, (n - 1) // EPOCH)
        val = (n - 1) % EPOCH + 1
        self.sem(key)
        self.ops[eng].append(("op", fn, key))
        for b in reads:
            if b.rs.get(key, 0) < val:
                b.rs[key] = val
        for b in writes:
            if b.ws.get(key, 0) < val:
                b.ws[key] = val

    def dma(self, q, out_ap, in_ap, reads, writes, sb, **kw):
        self._deps(q, reads, writes)
        if sb.dkey is None:
            if self.dpool:
                sb.dkey, sb.dcnt = self.dpool.pop()
            else:
                sb.dkey = ("d", sb.name)
                self.sem(sb.dkey)
        sb.dcnt += 16
        key, val = sb.dkey, sb.dcnt
        self.ops[q].append(("dma", out_ap, in_ap, key, kw))
        for b in reads:
            if b.rs.get(key, 0) < val:
                b.rs[key] = val
        for b in writes:
            if b.ws.get(key, 0) < val:
                b.ws[key] = val

    def wait_all(self, eng, bufs):
        self._deps(eng, bufs, ())

    def begin(self):
        st = ExitStack()
        st.__enter__()
        self.phs.append(st)
        self.scope_bufs.append([])

    def end(self):
        nc = self.nc
        mine = self.scope_bufs.pop()
        for b in mine:
            if b.dkey is not None:
                if self.known["sp"].get(b.dkey, 0) < b.dcnt:
                    self.known["sp"][b.dkey] = b.dcnt
                    self.ops["sp"].append(("wait", b.dkey, b.dcnt))
                self.dpool.append((b.dkey, b.dcnt))
        with nc.Block() as blk:
            def run(e, name):
                pend = None
                for o in self.ops[name]:
                    if o[0] == "wait":
                        if pend is not None:
                            self.nins += 1
                            e.wait_ge(self.sems[pend[1]], pend[2])
                        pend = o
                        continue
                    self.nins += 1
                    if o[0] == "op":
                        ins = o[1](e)
                        if pend is not None:
                            ins._wait_ge(self.sems[pend[1]], pend[2])
                        ins.then_inc(self.sems[o[2]], 1)
                    else:
                        ins = e.dma_start(out=o[1], in_=o[2], **o[4])
                        if pend is not None:
                            ins._wait_ge(self.sems[pend[1]], pend[2])
                        ins.then_inc(self.sems[o[3]], 16)
                    pend = None
                if pend is not None:
                    self.nins += 1
                    e.wait_ge(self.sems[pend[1]], pend[2])
                self.ops[name] = []

            @blk.tensor
            def _(e):
                run(e, "pe")

            @blk.scalar
            def _(e):
                run(e, "act")

            @blk.vector
            def _(e):
                run(e, "dve")

            @blk.gpsimd
            def _(e):
                run(e, "pool")

            @blk.sync
            def _(e):
                run(e, "sp")
        self.phs.pop().__exit__(None, None, None)


NT = 8192
NOWN = 4096
NTILE = NT // P
OWN0 = (NT - NOWN) // P


def bc(ap, shape):
    return ap.to_broadcast(list(shape))


class Ctx:
    pass


def declare(kb, dbg):
    g = Ctx()
    g.dbg = dbg
    sk = "ExternalOutput" if dbg else "Internal"
    EI = "ExternalInput"
    g.xs = kb.dram("xs", [NT, 1024], F32, EI)
    g.pos = kb.dram("pos", [NT, 1], I32, EI)
    g.cc = kb.dram("cc", [8, 128], F32, EI)
    g.pvd = kb.dram("pv", [128, 1], F32, EI)
    g.invf = kb.dram("invf", [128, 8], F32, EI)
    g.w_ada = kb.dram("w_ada", [1024, 6144], F32, EI)
    g.b_ada = kb.dram("b_ada", [1, 6144], F32, EI)
    g.norm1_g = kb.dram("norm1_g", [1, 1024], F32, EI)
    g.w_in = kb.dram("w_in", [1024, 5376], F32, EI)
    g.q_norm_g = kb.dram("q_norm_g", [1, 64], F32, EI)
    g.k_norm_g = kb.dram("k_norm_g", [1, 64], F32, EI)
    g.rwkv_mu = kb.dram("rwkv_mu", [1, 1792], F32, EI)
    g.rwkv_w0 = kb.dram("rwkv_w0", [1, 512], F32, EI)
    g.rwkv_w2 = kb.dram("rwkv_w2", [64, 512], F32, EI)
    g.rwkv_a0 = kb.dram("rwkv_a0", [1, 512], F32, EI)
    g.rwkv_a2 = kb.dram("rwkv_a2", [64, 512], F32, EI)
    g.rwkv_g2 = kb.dram("rwkv_g2", [128, 512], F32, EI)
    g.rwkv_k_k = kb.dram("rwkv_k_k", [1, 512], F32, EI)
    g.rwkv_k_a = kb.dram("rwkv_k_a", [1, 512], F32, EI)
    g.rwkv_r_k = kb.dram("rwkv_r_k", [1, 512], F32, EI)
    g.rwkv_ln_g = kb.dram("rwkv_ln_g", [1, 512], F32, EI)
    g.rwkv_ln_b = kb.dram("rwkv_ln_b", [1, 512], F32, EI)
    g.w_proj_moba = kb.dram("w_proj_moba", [512, 1024], F32, EI)
    g.w_proj_rwkv = kb.dram("w_proj_rwkv", [512, 1024], F32, EI)
    g.w_out = kb.dram("w_out", [1024, 1024], F32, EI)
    g.norm2_g = kb.dram("norm2_g", [1, 1024], F32, EI)
    g.peer_wq = kb.dram("peer_wq", [1024, 2048], F32, EI)
    g.peer_sk = kb.dram("peer_sk", [16, 128, 128], F32, EI)
    g.peer_u = kb.dram("peer_u", [16384, 1024], F32, EI)
    g.peer_v = kb.dram("peer_v", [16384, 1024], F32, EI)
    g.out = kb.dram("out", [NOWN, 1024], F32, "ExternalOutput")
    g.QT = kb.dram("QT", [4, 128, NOWN], BF16, sk)
    g.KT = kb.dram("KT", [4, 128, NT], BF16, sk)
    g.VM = kb.dram("VM", [NT, 512], BF16, sk)
    g.RS = kb.dram("RS", [6, 8, NT, 64], F32, sk)
    g.GR = kb.dram("GR", [NOWN, 512], F32, sk)
    g.GATES = kb.dram("GATES", [NOWN, 2048], BF16, sk)
    g.OM = kb.dram("OM", [NOWN, 512], BF16, sk)
    g.ORW = kb.dram("ORW", [NOWN, 512], BF16, sk)
    g.H1 = kb.dram("H1", [NOWN, 1024], F32, sk)
    g.XN2T = kb.dram("XN2T", [8, 128, NOWN], BF16, sk)
    g.SC = kb.dram("SC", [NOWN, 16, 128], F32, sk)
    g.TH = kb.dram("TH", [NOWN, 4, 8], F32, sk)
    g.UT = kb.dram("UT", [8, 128, 16384], BF16, "Internal")
    g.VB = kb.dram("VB", [16384, 1024], BF16, "Internal")
    g.ident_f = kb.gsbuf("ident_f", [128, 128], F32)
    g.ident_b = kb.gsbuf("ident_b", [128, 128], BF16)
    g.ones_f = kb.gsbuf("ones_f", [128, 128], F32)
    g.modc = kb.gsbuf("modc", [128, 48], F32)
    g.sc1p = kb.gsbuf("sc1p", [128, 8], F32)
    g.sc2p = kb.gsbuf("sc2p", [128, 8], F32)
    g.gt_bc = kb.gsbuf("gt_bc", [128, 2, 1024], F32)
    g.pv = kb.gsbuf("pvt", [128, 1], F32)
    g.kmT = kb.gsbuf("kmT", [128, 4, NT // 256], F32)
    g.npi = kb.gsbuf("npi", [128, 1], F32)
    return g


def phase0(kb, g):
    kb.begin()
    V = lambda fn, r, w: kb.op("dve", fn, r, w)
    A = lambda fn, r, w: kb.op("act", fn, r, w)
    PE = lambda fn, r, w: kb.op("pe", fn, r, w)
    PL = lambda fn, r, w: kb.op("pool", fn, r, w)
    PL(lambda e: e.memset(g.ones_f[:], 1.0), [], [g.ones_f])
    PL(lambda e: e.memset(g.npi[:], -float(np.pi)), [], [g.npi])
    PL(lambda e: e.memset(g.ident_f[:], 1.0), [], [g.ident_f])
    PL(lambda e: e.affine_select(out=g.ident_f[:], in_=g.ident_f[:], pattern=[[-1, 128]],
                                 compare_op=ALU.is_equal, fill=0.0, base=0, channel_multiplier=1),
       [g.ident_f], [g.ident_f])
    V(lambda e: e.tensor_copy(g.ident_b[:], g.ident_f[:]), [g.ident_f], [g.ident_b])
    kb.dma("sp", g.pv[:], g.pvd[:], [g.pvd], [g.pv], g.pv)
    c8 = kb.sbuf("c8", [8, 128], F32)
    scT = kb.sbuf("scT", [128, 8], F32)
    modrow = kb.sbuf("modrow", [1, 6144], F32)
    brow = kb.sbuf("brow", [1, 6144], F32)
    grow = kb.sbuf("grow", [1, 2, 1024], F32)
    gcol = kb.sbuf("gcol", [128, 16], F32)
    wst = [kb.sbuf(f"wst{i}", [128, 8, 512], F32) for i in range(2)]
    ps = kb.psum("p0ps", [128, 512], F32)
    ps2 = kb.psum("p0ps2", [128, 512], F32)
    kb.dma("sp", c8[:], g.cc[:], [g.cc], [c8], c8)
    kb.dma("sp", brow[:], g.b_ada[:], [g.b_ada], [brow], brow)
    kb.dma("sp", grow[:, 0, :], g.norm1_g[:], [g.norm1_g], [grow], grow)
    kb.dma("sp", grow[:, 1, :], g.norm2_g[:], [g.norm2_g], [grow], grow)
    PE(lambda e: e.transpose(ps[:, 0:8], c8[:], g.ident_f[0:8, 0:8]), [c8, g.ident_f], [ps])
    A(lambda e: e.activation(scT[:], ps[:, 0:8], AF.Silu), [ps], [scT])
    wv = g.w_ada.t.rearrange("(k p) n -> p k n", p=128)
    for jg in range(12):
        wb_ = wst[jg % 2]
        kb.dma("sp", wb_[:], wv[:, :, jg * 512:(jg + 1) * 512], [g.w_ada], [wb_], wb_)
        for k in range(8):
            PE(lambda e, k=k, wb_=wb_: e.matmul(ps2[0:1, :], scT[:, k:k + 1], wb_[:, k, :],
                                                  start=(k == 0), stop=(k == 7)), [scT, wb_], [ps2])
        V(lambda e, jg=jg: e.tensor_tensor(modrow[:, jg * 512:(jg + 1) * 512], ps2[0:1, :],
                                           brow[:, jg * 512:(jg + 1) * 512], ALU.add), [ps2, brow], [modrow])
    for j in range(48):
        PE(lambda e, j=j: e.matmul(ps[:, 16 + j:17 + j], modrow[0:1, j * 128:(j + 1) * 128], g.ones_f[0:1, 0:1],
                                   start=True, stop=True), [modrow, g.ones_f], [ps])
    V(lambda e: e.tensor_copy(g.modc[:], ps[:, 16:64]), [ps], [g.modc])
    for j in range(16):
        PE(lambda e, j=j: e.matmul(ps[:, 64 + j:65 + j], grow[0:1, j // 8, (j % 8) * 128:(j % 8 + 1) * 128],
                                   g.ones_f[0:1, 0:1], start=True, stop=True), [grow, g.ones_f], [ps])
    V(lambda e: e.tensor_copy(gcol[:], ps[:, 64:80]), [ps], [gcol])
    V(lambda e: e.scalar_tensor_tensor(g.sc1p[:], g.modc[:, 8:16], 1.0, gcol[:, 0:8], ALU.add, ALU.mult),
      [g.modc, gcol], [g.sc1p])
    V(lambda e: e.scalar_tensor_tensor(g.sc2p[:], g.modc[:, 32:40], 1.0, gcol[:, 8:16], ALU.add, ALU.mult),
      [g.modc, gcol], [g.sc2p])
    for i, c0 in enumerate((16 * 128, 40 * 128)):
        for hh in range(2):
            PE(lambda e, c0=c0, hh=hh: e.matmul(ps2[:, :], g.ones_f[0:1, :], modrow[0:1, c0 + hh * 512:c0 + (hh + 1) * 512],
                                                start=True, stop=True), [g.ones_f, modrow], [ps2])
            A(lambda e, i=i, hh=hh: e.copy(g.gt_bc[:, i, hh * 512:(hh + 1) * 512], ps2[:, :]), [ps2], [g.gt_bc])
    kb.end()


def phase_a(kb, g, ntiles=NTILE):
    kb.begin()
    V = lambda fn, r, w: kb.op("dve", fn, r, w)
    A = lambda fn, r, w: kb.op("act", fn, r, w)
    PE = lambda fn, r, w: kb.op("pe", fn, r, w)
    PL = lambda fn, r, w: kb.op("pool", fn, r, w)
    Wb = kb.sbuf("Wb", [128, 8, 5376], BF16)
    Wm = kb.sbuf("Wm", [128, 8, 1792], BF16)
    kb.begin()
    mu_bc = kb.sbuf("mu_bc", [128, 1792], F32)
    omu_bc = kb.sbuf("omu_bc", [128, 1792], F32)
    wst = [kb.sbuf(f"awst{i}", [128, 8, 256], F32) for i in range(2)]
    kb.dma("sp", mu_bc[:], g.rwkv_mu.t.partition_broadcast(128), [g.rwkv_mu], [mu_bc], mu_bc)
    V(lambda e: e.tensor_scalar(omu_bc[:], mu_bc[:], -1.0, 1.0, ALU.mult, ALU.add), [mu_bc], [omu_bc])
    wv = g.w_in.t.rearrange("(k p) n -> p k n", p=128)
    engs = ["dve", "act", "pool"]
    for pc in range(21):
        c0 = pc * 256
        st = wst[pc % 2]
        kb.dma("sp", st[:], wv[:, :, c0:c0 + 256], [g.w_in], [st], st)
        if 1536 <= c0 < 3328:
            m0 = c0 - 1536
            V(lambda e, st=st, c0=c0, m0=m0: e.tensor_tensor(Wb[:, :, c0:c0 + 256], st[:],
              bc(omu_bc[:, m0:m0 + 256].unsqueeze(1), [128, 8, 256]), ALU.mult), [st, omu_bc], [Wb])
            PL(lambda e, st=st, m0=m0: e.tensor_tensor(Wm[:, :, m0:m0 + 256], st[:],
               bc(mu_bc[:, m0:m0 + 256].unsqueeze(1), [128, 8, 256]), ALU.mult), [st, mu_bc], [Wm])
        else:
            en = engs[pc % 2]
            if en == "act":
                A(lambda e, st=st, c0=c0: e.copy(Wb[:, :, c0:c0 + 256], st[:]), [st], [Wb])
            else:
                V(lambda e, st=st, c0=c0: e.tensor_copy(Wb[:, :, c0:c0 + 256], st[:]), [st], [Wb])
    kb.end()
    def rowbc(name, src, n):
        t = kb.sbuf(name, [128, n], F32)
        kb.dma("sp", t[:], src.t.partition_broadcast(128), [src], [t], t)
        return t
    qg = rowbc("qg", g.q_norm_g, 64)
    kg = rowbc("kg", g.k_norm_g, 64)
    w0b = rowbc("w0b", g.rwkv_w0, 512)
    a0b = rowbc("a0b", g.rwkv_a0, 512)
    kkb = rowbc("kkb", g.rwkv_k_k, 512)
    kab = rowbc("kab", g.rwkv_k_a, 512)
    invf = kb.sbuf("invf_t", [128, 8], F32)
    kb.dma("sp", invf[:], g.invf[:], [g.invf], [invf], invf)
    w2a2 = kb.sbuf("w2a2", [128, 512], F32)
    g2t = kb.sbuf("g2t", [128, 512], F32)
    kb.dma("sp", w2a2[0:64, :], g.rwkv_w2[:], [g.rwkv_w2], [w2a2], w2a2)
    kb.dma("sp", w2a2[64:128, :], g.rwkv_a2[:], [g.rwkv_a2], [w2a2], w2a2)
    kb.dma("sp", g2t[:], g.rwkv_g2[:], [g.rwkv_g2], [g2t], g2t)
    onesc = kb.sbuf("onesc", [128, 1], F32)
    PL(lambda e: e.memset(onesc[:], 1.0 / 256.0), [], [onesc])

    xt = [kb.sbuf(f"xt{i}", [128, 1024], F32) for i in range(2)]
    junk = kb.sbuf("junk", [128, 1024], BF16)
    xnT = [kb.sbuf(f"xnT{i}", [128, 8, 129], BF16) for i in range(2)]
    stt = [kb.sbuf(f"stt{i}", [128, 4], F32) for i in range(2)]
    posi = [kb.sbuf(f"posi{i}", [128, 1], I32) for i in range(2)]
    cs = [kb.sbuf(f"cs{i}", [128, 5, 16], F32) for i in range(2)]
    csi = [kb.sbuf(f"csi{i}", [128, 16], I32) for i in range(2)]
    psT = [kb.psum(f"psT{i}", [128, 512], F32) for i in range(2)]
    psM = [kb.psum(f"psM{i}", [128, 512], F32) for i in range(4)]
    psB = kb.psum("psB", [128, 1024], BF16)
    psX = kb.psum("psX", [128, 512], F32)
    NB = 3
    t1 = [kb.sbuf(f"t1_{i}", [128, 512], F32) for i in range(NB)]
    t2 = [kb.sbuf(f"t2_{i}", [128, 512], F32) for i in range(NB)]
    ssq = [kb.sbuf(f"ssq{i}", [128, 16], F32) for i in range(NB)]
    rp = [kb.sbuf(f"rp{i}", [128, 4, 8, 8], F32) for i in range(NB)]
    qf = [kb.sbuf(f"qf{i}", [128, 512], BF16) for i in range(NB)]
    qTs = [kb.sbuf(f"qTs{i}", [128, 4, 128], BF16) for i in range(NB)]
    kmp = [kb.sbuf(f"kmp{i}", [128, 4], F32) for i in range(2)]
    la = [kb.sbuf(f"la{i}", [128, 256], F32) for i in range(2)]
    laT = [kb.sbuf(f"laT{i}", [128, 256], F32) for i in range(2)]
    av = [kb.sbuf(f"av{i}", [128, 512], F32) for i in range(2)]
    sto = [kb.sbuf(f"sto{i}", [128, 512], F32) for i in range(8)]
    gsb = [kb.sbuf(f"gsb{i}", [128, 512], BF16) for i in range(3)]
    cnt = {"m": 0, "b": 0, "s": 0, "g": 0}

    def nextM():
        cnt["m"] += 1
        return psM[cnt["m"] % 4]

    def nextS():
        cnt["s"] += 1
        return sto[cnt["s"] % 8]

    def mm(ps_, ncols, col0, xn, prev_c0=None):
        for k in range(8):
            PE(lambda e, k=k: e.matmul(ps_[:, 0:ncols], xn[:, k, 1:129], Wb[:, k, col0:col0 + ncols],
                                       start=(k == 0), stop=(k == 7 and prev_c0 is None)), [xn, Wb], [ps_])
        if prev_c0 is not None:
            for k in range(8):
                PE(lambda e, k=k: e.matmul(ps_[:, 0:ncols], xn[:, k, 0:128], Wm[:, k, prev_c0:prev_c0 + ncols],
                                           start=False, stop=(k == 7)), [xn, Wm], [ps_])

    def stream_store(si, src, t0):
        dst = g.RS.t[si, :, t0:t0 + 128, :].rearrange("h t n -> t h n")
        kb.dma("sp", dst, src[:].rearrange("p (h n) -> p h n", h=8), [src], [g.RS], src)

    def tile_body(it):
        own = it >= OWN0
        t0 = it * 128
        x_ = xt[it % 2]
        xn = xnT[it % 2]
        xnp = xnT[(it + 1) % 2]
        st_ = stt[it % 2]
        kb.dma("sp", x_[:], g.xs[t0:t0 + 128, :], [g.xs], [x_], x_)
        pi = posi[it % 2]
        kb.dma("sp", pi[:], g.pos[t0:t0 + 128, :], [g.pos], [pi], pi)
        A(lambda e, x_=x_, st_=st_: e.activation(junk[:], x_[:], AF.Square, accum_out=st_[:, 0:1]), [x_], [junk, st_])
        V(lambda e, st_=st_: e.tensor_scalar(st_[:, 1:2], st_[:, 0:1], 1.0 / 1024.0, 1e-6, ALU.mult, ALU.add), [st_], [st_])
        A(lambda e, st_=st_: e.activation(st_[:, 2:3], st_[:, 1:2], AF.Sqrt), [st_], [st_])
        V(lambda e, st_=st_: e.reciprocal(st_[:, 2:3], st_[:, 2:3]), [st_], [st_])
        V(lambda e, x_=x_, st_=st_: e.tensor_scalar_mul(x_[:], x_[:], st_[:, 2:3]), [x_, st_], [x_])
        for k in range(8):
            pt = psT[k // 4]
            PE(lambda e, k=k, pt=pt, x_=x_: e.transpose(pt[:, (k % 4) * 128:(k % 4 + 1) * 128], x_[:, k * 128:(k + 1) * 128],
                                                        g.ident_f[:]), [x_, g.ident_f], [pt])
            A(lambda e, k=k, pt=pt, xn=xn: e.activation(xn[:, k, 1:129], pt[:, (k % 4) * 128:(k % 4 + 1) * 128], AF.Identity,
                                                       bias=g.modc[:, k:k + 1], scale=g.sc1p[:, k:k + 1]),
              [pt, g.modc, g.sc1p], [xn])
        if it == 0:
            V(lambda e, xn=xn: e.memset(xn[:, :, 0:1], 0.0), [], [xn])
        elif it == OWN0:
            V(lambda e, xn=xn, xnp=xnp: e.tensor_scalar_mul(xn[:, :, 0:1], xnp[:, :, 128:129], g.pv[:, 0:1]), [xnp, g.pv], [xn])
        else:
            V(lambda e, xn=xn, xnp=xnp: e.tensor_copy(xn[:, :, 0:1], xnp[:, :, 128:129]), [xnp], [xn])
        c_ = cs[it % 2]
        ci_ = csi[it % 2]
        PI = float(np.pi)
        V(lambda e: e.tensor_copy(c_[:, 1, 0:1], pi[:]), [pi], [c_])
        V(lambda e: e.tensor_scalar_mul(c_[:, 0, 0:8], invf[:], c_[:, 1, 0:1]), [invf, c_], [c_])
        V(lambda e: e.tensor_scalar_add(c_[:, 0, 8:16], c_[:, 0, 0:8], 0.5 * PI), [c_], [c_])
        V(lambda e: e.tensor_scalar_mul(c_[:, 1, :], c_[:, 0, :], 1.0 / (2 * PI)), [c_], [c_])
        V(lambda e: e.tensor_copy(ci_[:], c_[:, 1, :]), [c_], [ci_])
        V(lambda e: e.tensor_copy(c_[:, 1, :], ci_[:]), [ci_], [c_])
        V(lambda e: e.scalar_tensor_tensor(c_[:, 2, :], c_[:, 1, :], -2 * PI, c_[:, 0, :], ALU.mult, ALU.add), [c_], [c_])
        V(lambda e: e.tensor_single_scalar(c_[:, 1, :], c_[:, 2, :], PI, ALU.is_gt), [c_], [c_])
        V(lambda e: e.scalar_tensor_tensor(c_[:, 3, :], c_[:, 1, :], -2 * PI, c_[:, 2, :], ALU.mult, ALU.add), [c_], [c_])
        V(lambda e: e.tensor_single_scalar(c_[:, 1, :], c_[:, 3, :], -PI, ALU.is_lt), [c_], [c_])
        V(lambda e: e.scalar_tensor_tensor(c_[:, 2, :], c_[:, 1, :], 2 * PI, c_[:, 3, :], ALU.mult, ALU.add), [c_], [c_])
        A(lambda e: e.activation(c_[:, 4, :], c_[:, 2, :], AF.Sin), [c_], [c_])

        def qk_post(ps_, gb, is_q):
            i = cnt["b"] % NB
            cnt["b"] += 1
            a1, a2, sq_, rp_, qf_, qT_ = t1[i], t2[i], ssq[i], rp[i], qf[i], qTs[i]
            A(lambda e: e.activation(a1[:], ps_[:], AF.Square), [ps_], [a1])
            V(lambda e: e.tensor_reduce(sq_[:, 0:8], a1[:].rearrange("p (h d) -> p h d", h=8), AX.X, ALU.add), [a1], [sq_])
            V(lambda e: e.tensor_scalar(sq_[:, 0:8], sq_[:, 0:8], 1.0 / 64.0, 1e-6, ALU.mult, ALU.add), [sq_], [sq_])
            A(lambda e: e.activation(sq_[:, 8:16], sq_[:, 0:8], AF.Sqrt), [sq_], [sq_])
            V(lambda e: e.reciprocal(sq_[:, 8:16], sq_[:, 8:16]), [sq_], [sq_])
            V(lambda e: e.tensor_tensor(a2[:].rearrange("p (h d) -> p h d", h=8), ps_[:].rearrange("p (h d) -> p h d", h=8),
                                        bc(sq_[:, 8:16].unsqueeze(2), [128, 8, 64]), ALU.mult), [ps_, sq_], [a2])
            V(lambda e: e.tensor_tensor(a2[:].rearrange("p (h d) -> p h d", h=8), a2[:].rearrange("p (h d) -> p h d", h=8),
                                        bc(gb[:].unsqueeze(1), [128, 8, 64]), ALU.mult), [a2, gb], [a2])
            v3 = a2[:].rearrange("p (h d) -> p h d", h=8)
            x1, x2 = v3[:, :, 0:8], v3[:, :, 8:16]
            sinb = bc(c_[:, 4, 0:8].unsqueeze(1), [128, 8, 8])
            cosb = bc(c_[:, 4, 8:16].unsqueeze(1), [128, 8, 8])
            V(lambda e: e.tensor_tensor(rp_[:, 0], x1, cosb, ALU.mult), [a2, c_], [rp_])
            V(lambda e: e.tensor_tensor(rp_[:, 1], x2, sinb, ALU.mult), [a2, c_], [rp_])
            V(lambda e: e.tensor_tensor(rp_[:, 2], x2, cosb, ALU.mult), [a2, c_], [rp_])
            V(lambda e: e.tensor_tensor(rp_[:, 3], x1, sinb, ALU.mult), [a2, c_], [rp_])
            V(lambda e: e.tensor_tensor(x1, rp_[:, 0], rp_[:, 1], ALU.subtract), [rp_], [a2])
            V(lambda e: e.tensor_tensor(x2, rp_[:, 2], rp_[:, 3], ALU.add), [rp_], [a2])
            A(lambda e: e.copy(qf_[:], a2[:]), [a2], [qf_])
            for pr in range(4):
                PE(lambda e, pr=pr: e.transpose(psB[:, pr * 128:(pr + 1) * 128], qf_[:, pr * 128:(pr + 1) * 128], g.ident_b[:]),
                   [qf_, g.ident_b], [psB])
            V(lambda e: e.tensor_copy(qT_[:].rearrange("p c t -> p (c t)"), psB[:, 0:512]), [psB], [qT_])
            if is_q:
                to = t0 - OWN0 * 128
                kb.dma("sp", g.QT.t[:, :, to:to + 128].rearrange("c p t -> p c t"), qT_[:], [qT_], [g.QT], qT_)
            else:
                kb.dma("sp", g.KT.t[:, :, t0:t0 + 128].rearrange("c p t -> p c t"), qT_[:], [qT_], [g.KT], qT_)
                km_ = kmp[it % 2]
                for pr in range(4):
                    PE(lambda e, pr=pr: e.matmul(psX[:, 256 + pr:257 + pr], a2[:, pr * 128:(pr + 1) * 128], onesc[:],
                                                 start=True, stop=True), [a2, onesc], [psX])
                V(lambda e: e.tensor_copy(km_[:], psX[:, 256:260]), [psX], [km_])
                if it % 2 == 1:
                    V(lambda e: e.tensor_tensor(g.kmT[:, :, it // 2], kmp[0][:], kmp[1][:], ALU.add), [kmp[0], kmp[1]], [g.kmT])

        psl = nextM()
        mm(psl, 256, 3072, xn, prev_c0=1536)
        la_ = la[it % 2]
        laT_ = laT[it % 2]
        A(lambda e: e.activation(la_[:, 0:64], psl[:, 0:64], AF.Tanh), [psl], [la_])
        V(lambda e: e.tensor_copy(la_[:, 64:128], psl[:, 64:128]), [psl], [la_])
        A(lambda e: e.activation(la_[:, 128:256], psl[:, 128:256], AF.Sigmoid), [psl], [la_])
        PE(lambda e: e.transpose(psX[:, 0:128], la_[:, 0:128], g.ident_f[:]), [la_, g.ident_f], [psX])
        PE(lambda e: e.transpose(psX[:, 128:256], la_[:, 128:256], g.ident_f[:]), [la_, g.ident_f], [psX])
        V(lambda e: e.tensor_copy(laT_[:], psX[:, 0:256]), [psX], [laT_])
        pw = nextM()
        PE(lambda e: e.matmul(pw[:], laT_[0:64, 0:128], w2a2[0:64, :], start=True, stop=True), [laT_, w2a2], [pw])
        ld_ = nextS()
        V(lambda e: e.tensor_tensor(ld_[:], pw[:], w0b[:], ALU.add), [pw, w0b], [ld_])
        A(lambda e: e.activation(ld_[:], ld_[:], AF.Sigmoid), [ld_], [ld_])
        V(lambda e: e.tensor_scalar_mul(ld_[:], ld_[:], -0.6065306597126334), [ld_], [ld_])
        stream_store(1, ld_, t0)
        pa = nextM()
        PE(lambda e: e.matmul(pa[:], laT_[64:128, 0:128], w2a2[64:128, :], start=True, stop=True), [laT_, w2a2], [pa])
        a_ = av[it % 2]
        V(lambda e: e.tensor_tensor(a_[:], pa[:], a0b[:], ALU.add), [pa, a0b], [a_])
        A(lambda e: e.activation(a_[:], a_[:], AF.Sigmoid), [a_], [a_])
        if own:
            pg = nextM()
            PE(lambda e: e.matmul(pg[:], laT_[:, 128:256], g2t[:], start=True, stop=True), [laT_, g2t], [pg])
            gg = nextS()
            A(lambda e: e.copy(gg[:], pg[:]), [pg], [gg])
            to = t0 - OWN0 * 128
            kb.dma("sp", g.GR[to:to + 128, :], gg[:], [gg], [g.GR], gg)
        pr_ = nextM()
        mm(pr_, 512, 1536, xn, prev_c0=0)
        r_ = nextS()
        A(lambda e: e.copy(r_[:], pr_[:]), [pr_], [r_])
        stream_store(0, r_, t0)
        pk = nextM()
        mm(pk, 512, 2048, xn, prev_c0=512)
        i = cnt["b"] % NB
        cnt["b"] += 1
        a1, sq_ = t1[i], ssq[i]
        kkn = nextS()
        V(lambda e: e.tensor_tensor(kkn[:], pk[:], kkb[:], ALU.mult), [pk, kkb], [kkn])
        V(lambda e: e.tensor_tensor(a1[:], kkn[:], kkn[:], ALU.mult), [kkn], [a1])
        V(lambda e: e.tensor_reduce(sq_[:, 0:8], a1[:].rearrange("p (h d) -> p h d", h=8), AX.X, ALU.add), [a1], [sq_])
        V(lambda e: e.tensor_scalar_add(sq_[:, 0:8], sq_[:, 0:8], 1e-24), [sq_], [sq_])
        A(lambda e: e.activation(sq_[:, 8:16], sq_[:, 0:8], AF.Sqrt), [sq_], [sq_])
        V(lambda e: e.reciprocal(sq_[:, 8:16], sq_[:, 8:16]), [sq_], [sq_])
        V(lambda e: e.scalar_tensor_tensor(kkn[:].rearrange("p (h d) -> p h d", h=8), kkn[:].rearrange("p (h d) -> p h d", h=8), -1.0,
                                           bc(sq_[:, 8:16].unsqueeze(2), [128, 8, 64]), ALU.mult, ALU.mult), [kkn, sq_], [kkn])
        stream_store(4, kkn, t0)
        b_ = nextS()
        V(lambda e: e.scalar_tensor_tensor(b_[:], kkn[:], -1.0, a_[:], ALU.mult, ALU.mult), [kkn, a_], [b_])
        stream_store(5, b_, t0)
        k_ = nextS()
        V(lambda e: e.scalar_tensor_tensor(a1[:], a_[:], -1.0, kab[:], ALU.add, ALU.mult), [a_, kab], [a1])
        V(lambda e: e.scalar_tensor_tensor(k_[:], a1[:], 1.0, pk[:], ALU.add, ALU.mult), [a1, pk], [k_])
        stream_store(2, k_, t0)
        pv_ = nextM()
        mm(pv_, 512, 2560, xn, prev_c0=1024)
        v_ = nextS()
        if own:
            A(lambda e: e.copy(v_[:], pv_[:]), [pv_], [v_])
        else:
            V(lambda e: e.tensor_scalar_mul(v_[:], pv_[:], g.pv[:, 0:1]), [pv_, g.pv], [v_])
        stream_store(3, v_, t0)
        pmk = nextM()
        mm(pmk, 512, 512, xn)
        qk_post(pmk, kg, False)
        pmv = nextM()
        mm(pmv, 512, 1024, xn)
        vb = gsb[cnt["g"] % 3]
        cnt["g"] += 1
        A(lambda e: e.copy(vb[:], pmv[:]), [pmv], [vb])
        kb.dma("sp", g.VM[t0:t0 + 128, :], vb[:], [vb], [g.VM], vb)
        if own:
            pmq = nextM()
            mm(pmq, 512, 0, xn)
            qk_post(pmq, qg, True)
            to = t0 - OWN0 * 128
            for gi in range(4):
                pg_ = nextM()
                mm(pg_, 512, 3328 + gi * 512, xn)
                gb_ = gsb[cnt["g"] % 3]
                cnt["g"] += 1
                A(lambda e, gb_=gb_, pg_=pg_: e.activation(gb_[:], pg_[:], AF.Sigmoid), [pg_], [gb_])
                kb.dma("sp", g.GATES[to:to + 128, gi * 512:(gi + 1) * 512], gb_[:], [gb_], [g.GATES], gb_)
    for it in range(ntiles):
        tile_body(it)
    kb.end()


def phase_b(kb, g, nqb=16):
    kb.begin()
    V = lambda fn, r, w: kb.op("dve", fn, r, w)
    A = lambda fn, r, w: kb.op("act", fn, r, w)
    PE = lambda fn, r, w: kb.op("pe", fn, r, w)
    PL = lambda fn, r, w: kb.op("pool", fn, r, w)
    NKB = NT // 256
    QB0 = OWN0 // 2
    kmTb = kb.sbuf("kmTb", [128, 4, NKB], BF16)
    V(lambda e: e.tensor_copy(kmTb[:], g.kmT[:]), [g.kmT], [kmTb])
    pastm = kb.sbuf("pastm", [128, 16, NKB], F32)
    pfx = kb.sbuf("pfx", [128, 1], F32)
    PL(lambda e: e.memset(pastm[:], 0.0), [], [pastm])
    for qbl in range(16):
        PL(lambda e, qbl=qbl: e.memset(pastm[:, qbl, QB0 + qbl:NKB], -1e30), [], [pastm])
    V(lambda e: e.tensor_scalar(pfx[:], g.pv[:], -1.0, 1e30, ALU.add, ALU.mult), [g.pv], [pfx])
    V(lambda e: e.tensor_scalar(pastm[:, :, 0:QB0], pastm[:, :, 0:QB0], pfx[:, 0:1], None, ALU.add), [pastm, pfx], [pastm])
    tri = kb.sbuf("tri", [128, 2, 256], BF16)
    PL(lambda e: e.memset(tri[:], 1.0), [], [tri])
    for kc in range(2):
        PL(lambda e, kc=kc: e.affine_select(out=tri[:, kc, :], in_=tri[:, kc, :], pattern=[[1, 256]], compare_op=ALU.is_ge,
                                            fill=0.0, base=-kc * 128, channel_multiplier=-1), [tri], [tri])
    KTs = [kb.sbuf(f"KTs{i}", [128, NT], BF16) for i in range(2)]
    QTs = [kb.sbuf(f"QTs{i}", [128, NOWN], BF16) for i in range(2)]
    Vs = [kb.sbuf(f"Vs{i}", [128, NT // 128, 2, 65], BF16) for i in range(2)]
    for i in range(2):
        PL(lambda e, i=i: e.memset(Vs[i][:, :, :, 64:65], 1.0), [], [Vs[i]])
    sel = [kb.sbuf(f"sel{i}", [128, 32, 2, NKB], F32) for i in range(2)]
    gsm = [kb.sbuf(f"gsm{i}", [128, NKB], F32) for i in range(2)]
    g8 = [kb.sbuf(f"g8{i}", [128, 8], F32) for i in range(2)]
    m1 = [kb.sbuf(f"m1{i}", [128, NKB], F32) for i in range(2)]
    psS = [kb.psum(f"psS{i}", [128, 512], F32) for i in range(3)]
    psO = [kb.psum(f"psO{i}", [128, 2, 65], F32) for i in range(3)]
    psG = [kb.psum(f"psG{i}", [128, 64], F32) for i in range(2)]
    pts = [kb.sbuf(f"pts{i}", [128, 2, 256], BF16) for i in range(4)]
    acc = [kb.sbuf(f"acc{i}", [128, 2, 65], F32) for i in range(2)]
    rc = [kb.sbuf(f"rc{i}", [128, 2], F32) for i in range(2)]
    ob = [kb.sbuf(f"ob{i}", [128, 2, 64], BF16) for i in range(4)]
    cn = {"s": 0, "o": 0, "p": 0, "a": 0, "b": 0, "g": 0}
    vview = g.VM.t.rearrange("(c p) (h d) -> p c h d", p=128, d=64)

    def pair_body(pr):
        KT_, QT_, V_, sel_ = KTs[pr % 2], QTs[pr % 2], Vs[pr % 2], sel[pr % 2]
        kb.dma("sp", KT_[:], g.KT.t[pr], [g.KT], [KT_], KT_)
        kb.dma("sp", QT_[:], g.QT.t[pr], [g.QT], [QT_], QT_)
        for cq in range(4):
            c0 = cq * (NT // 512)
            c1 = c0 + NT // 512
            for h2 in range(2):
                kb.dma("sp", V_[:, c0:c1, h2, 0:64], vview[:, c0:c1, 2 * pr + h2, :], [g.VM], [V_], V_)
        for qt in range(2 * nqb):
            qbl = qt // 2
            for h2 in range(2):
                def selbody(qt=qt, qbl=qbl, h2=h2):
                    i = cn["g"] % 2
                    cn["g"] += 1
                    pg, gs_, g8_, m1_ = psG[i], gsm[i], g8[i], m1[i]
                    rows = slice(h2 * 64, (h2 + 1) * 64)
                    PE(lambda e: e.matmul(pg[:, 0:NKB], QT_[rows, qt * 128:(qt + 1) * 128], kmTb[rows, pr, :], start=True, stop=True),
                       [QT_, kmTb], [pg])
                    V(lambda e: e.tensor_tensor(gs_[:], pg[:, 0:NKB], pastm[:, qbl, :], ALU.add), [pg, pastm], [gs_])
                    V(lambda e: e.max(out=g8_[:], in_=gs_[:]), [gs_], [g8_])
                    V(lambda e: e.tensor_scalar(m1_[:], gs_[:], g8_[:, 2:3], None, ALU.is_ge), [gs_, g8_], [m1_])
                    V(lambda e: e.scalar_tensor_tensor(sel_[:, qt, h2, :], gs_[:], -1e29, m1_[:], ALU.is_gt, ALU.mult), [gs_, m1_], [sel_])
                    V(lambda e: e.memset(sel_[:, qt, h2, QB0 + qbl:QB0 + qbl + 1], 1.0), [], [sel_])
                selbody()
        for h2 in range(2):
            rows = slice(h2 * 64, (h2 + 1) * 64)
            for qbl in range(nqb):
                def qb_body(h2=h2, rows=rows, qbl=qbl):
                    qb = QB0 + qbl
                    acc_ = acc[cn["a"] % 2]
                    rc_ = rc[cn["a"] % 2]
                    cn["a"] += 1
                    for kblk in range(qb + 1):
                        def kb_body(kblk=kblk):
                            pS = psS[cn["s"] % 3]
                            cn["s"] += 1
                            pO = psO[cn["o"] % 3]
                            cn["o"] += 1
                            pt = pts[cn["p"] % 4]
                            cn["p"] += 1
                            for kc in range(2):
                                c = kblk * 2 + kc
                                PE(lambda e, kc=kc, c=c: e.matmul(pS[:, kc * 256:(kc + 1) * 256], KT_[rows, c * 128:(c + 1) * 128],
                                                                  QT_[rows, qbl * 256:(qbl + 1) * 256], start=True, stop=True),
                                   [KT_, QT_], [pS])
                            A(lambda e: e.activation(pt[:].rearrange("p a b -> p (a b)"), pS[:], AF.Exp, scale=0.125), [pS], [pt])
                            if kblk == qb:
                                V(lambda e: e.tensor_tensor(pt[:], pt[:], tri[:], ALU.mult), [pt, tri], [pt])
                            for qt in range(2):
                                for kc in range(2):
                                    c = kblk * 2 + kc
                                    PE(lambda e, qt=qt, kc=kc, c=c: e.matmul(pO[:, qt, :], pt[:, kc, qt * 128:(qt + 1) * 128], V_[:, c, h2, :],
                                                                             start=(kc == 0), stop=(kc == 1)), [pt, V_], [pO])
                            for qt in range(2):
                                sc = sel_[:, qbl * 2 + qt, h2, kblk:kblk + 1]
                                if kblk == 0:
                                    V(lambda e, qt=qt, sc=sc: e.tensor_scalar_mul(acc_[:, qt, :], pO[:, qt, :], sc), [pO, sel_], [acc_])
                                else:
                                    V(lambda e, qt=qt, sc=sc: e.scalar_tensor_tensor(acc_[:, qt, :], pO[:, qt, :], sc, acc_[:, qt, :],
                                                                                     ALU.mult, ALU.add), [pO, sel_, acc_], [acc_])
                        kb_body()
                    ob_ = ob[cn["b"] % 4]
                    cn["b"] += 1
                    V(lambda e: e.reciprocal(rc_[:], acc_[:, :, 64]), [acc_], [rc_])
                    for qt in range(2):
                        V(lambda e, qt=qt: e.tensor_scalar_mul(ob_[:, qt, :], acc_[:, qt, 0:64], rc_[:, qt:qt + 1]), [acc_, rc_], [ob_])
                    hh = pr * 2 + h2
                    dst = g.OM.t[qbl * 256:(qbl + 1) * 256, hh * 64:(hh + 1) * 64].rearrange("(a p) d -> p a d", p=128)
                    kb.dma("sp", dst, ob_[:], [ob_], [g.OM], ob_)
                qb_body()

    for pr in range(4):
        pair_body(pr)
    kb.end()


def phase_c(kb, g, nchunks=NT // 64):
    kb.begin()
    V = lambda fn, r, w: kb.op("dve", fn, r, w)
    A = lambda fn, r, w: kb.op("act", fn, r, w)
    PE = lambda fn, r, w: kb.op("pe", fn, r, w)
    PL = lambda fn, r, w: kb.op("pool", fn, r, w)
    I_ = g.ident_f
    Lbd = kb.sbuf("Lbd", [128, 128], F32)
    Msu = kb.sbuf("Msu", [128, 128], F32)
    Msl = kb.sbuf("Msl", [128, 128], F32)
    Obd = kb.sbuf("Obd", [128, 128], F32)
    ind2 = kb.sbuf("ind2", [128, 2], F32)
    for t_, op_ in ((Lbd, ALU.is_ge), (Msu, ALU.is_gt)):
        PL(lambda e, t_=t_: e.memset(t_[:], 1.0), [], [t_])
        PL(lambda e, t_=t_, op_=op_: e.affine_select(out=t_[:], in_=t_[:], pattern=[[1, 128]], compare_op=op_, fill=0.0,
                                                     base=0, channel_multiplier=-1), [t_], [t_])
        PL(lambda e, t_=t_: e.memset(t_[0:64, 64:128], 0.0), [], [t_])
    PL(lambda e: e.memset(Msl[:], 1.0), [], [Msl])
    PL(lambda e: e.affine_select(out=Msl[:], in_=Msl[:], pattern=[[-1, 128]], compare_op=ALU.is_gt, fill=0.0,
                                 base=0, channel_multiplier=1), [Msl], [Msl])
    PL(lambda e: e.memset(Msl[64:128, 0:64], 0.0), [], [Msl])
    PL(lambda e: e.memset(Obd[:], 0.0), [], [Obd])
    PL(lambda e: e.memset(Obd[0:64, 0:64], 1.0), [], [Obd])
    PL(lambda e: e.memset(Obd[64:128, 64:128], 1.0), [], [Obd])
    PL(lambda e: e.memset(ind2[:], 0.0), [], [ind2])
    PL(lambda e: e.memset(ind2[0:64, 0:1], 1.0), [], [ind2])
    PL(lambda e: e.memset(ind2[64:128, 1:2], 1.0), [], [ind2])
    cst = kb.sbuf("cst", [128, 3, 4, 64], F32)
    for hp in range(4):
        for h2 in range(2):
            hh = hp * 2 + h2
            for j, src in enumerate((g.rwkv_ln_g, g.rwkv_ln_b, g.rwkv_r_k)):
                kb.dma("sp", cst[h2 * 64:(h2 + 1) * 64, j, hp, :], src.t[:, hh * 64:(hh + 1) * 64].partition_broadcast(64),
                       [src], [cst], cst)
    ldb = [kb.sbuf(f"cld{i}", [128, 4, 6, 64], F32) for i in range(2)]
    gtb = [kb.sbuf(f"cgt{i}", [128, 4, 64], F32) for i in range(2)]
    E = kb.sbuf("cE", [128, 4, 4, 64], F32)
    X = kb.sbuf("cX", [128, 4, 4, 64], F32)
    TA = kb.sbuf("cTA", [128, 4, 4, 64], F32)
    BK = kb.sbuf("cBK", [128, 4, 2, 64], F32)
    Vbd = kb.sbuf("cVbd", [128, 4, 128], F32)
    Ubd = kb.sbuf("cUbd", [128, 4, 128], F32)
    TT = kb.sbuf("cTT", [64, 4, 4, 128], F32)
    AA = [kb.sbuf(f"cAA{i}", [128, 4, 2, 128], BF16) for i in range(2)]
    AXm = kb.sbuf("cAX", [128, 4, 3, 128], F32)
    Y = [kb.sbuf(f"cY{i}", [128, 4, 128], BF16) for i in range(2)]
    Yf = kb.sbuf("cYf", [128, 4, 128], F32)
    WT = kb.sbuf("cWT", [64, 4, 128], F32)
    PC = kb.sbuf("cPC", [64, 4, 2], F32)
    hs = [kb.sbuf(f"ch{i}", [64, 4, 128], F32) for i in range(2)]
    htmp = kb.sbuf("chtmp", [64, 4, 128], F32)
    O = kb.sbuf("cO", [128, 4, 64], F32)
    stt = kb.sbuf("cst2", [128, 8, 4], F32)
    t1 = kb.sbuf("ct1", [128, 4, 64], F32)
    t2 = kb.sbuf("ct2", [128, 4, 64], F32)
    obb = [kb.sbuf(f"cob{i}", [128, 4, 64], BF16) for i in range(2)]
    psA = kb.psum("cps", [128, 4, 2, 512], F32)

    class PB:
        def __init__(self, b):
            self.buf = Buf(None, f"cpsbank{b}")
            self.b = b

        def s(self, hp, sl, rows=slice(0, 128)):
            return psA.t[rows, hp, self.b, sl]

        def all(self, sl, rows=slice(0, 128)):
            return psA.t[rows, :, self.b, sl]
    P0, P1 = PB(0), PB(1)
    PL(lambda e: e.memset(Vbd[:], 0.0), [], [Vbd])
    PL(lambda e: e.memset(Ubd[:], 0.0), [], [Ubd])
    PL(lambda e: e.memset(hs[0][:], 0.0), [], [hs[0]])
    b4 = lambda m: bc(m[:].unsqueeze(1), [128, 4, 128])
    orw4 = g.ORW.t.rearrange("t (hp h2 n) -> t hp h2 n", hp=4, h2=2)
    gr4 = g.GR.t.rearrange("t (hp h2 n) -> t hp h2 n", hp=4, h2=2)

    def chunk(c):
        own = c >= (NT - NOWN) // 64
        ld = ldb[c % 2]
        gt = gtb[c % 2]
        hcur, hnew = hs[c % 2], hs[(c + 1) % 2]
        to = c * 64 - (NT - NOWN)
        for hp in range(4):
            for h2 in range(2):
                src = g.RS.t[:, hp * 2 + h2, c * 64:(c + 1) * 64, :].rearrange("s t n -> t s n")
                kb.dma("sp", ld[h2 * 64:(h2 + 1) * 64, hp, :, :], src, [g.RS], [ld], ld)
        if own:
            for h2 in range(2):
                kb.dma("sp", gt[h2 * 64:(h2 + 1) * 64, :, :], gr4[to:to + 64, :, h2, :], [g.GR], [gt], gt)
        for hp in range(4):
            PE(lambda e, hp=hp: e.matmul(P0.s(hp, slice(0, 64)), Lbd[:], ld[:, hp, 1, :], start=True, stop=True), [Lbd, ld], [P0.buf])
            PE(lambda e, hp=hp: e.matmul(P0.s(hp, slice(64, 128)), Obd[:], ld[:, hp, 1, :], start=True, stop=True), [Obd, ld], [P0.buf])
            PE(lambda e, hp=hp: e.matmul(P0.s(hp, slice(128, 130), slice(0, 64)), ld[:, hp, 1, :], ind2[:], start=True, stop=True),
               [ld, ind2], [P0.buf])
        V(lambda e: e.tensor_copy(E[:, :, 0, :], P0.all(slice(0, 64))), [P0.buf], [E])
        V(lambda e: e.tensor_scalar_mul(E[:, :, 1, :], P0.all(slice(0, 64)), -1.0), [P0.buf], [E])
        V(lambda e: e.tensor_tensor(E[:, :, 2, :], P0.all(slice(0, 64)), ld[:, :, 1, :], ALU.subtract), [P0.buf, ld], [E])
        V(lambda e: e.tensor_tensor(E[:, :, 3, :], P0.all(slice(64, 128)), E[:, :, 0, :], ALU.subtract), [P0.buf, E], [E])
        A(lambda e: e.activation(X[:], E[:], AF.Exp), [E], [X])
        A(lambda e: e.activation(PC[:], P0.all(slice(128, 130), slice(0, 64)), AF.Exp), [P0.buf], [PC])
        V(lambda e: e.tensor_tensor(TA[:, :, 0, :], ld[:, :, 4, :], X[:, :, 2, :], ALU.mult), [ld, X], [TA])
        V(lambda e: e.tensor_tensor(TA[:, :, 1, :], ld[:, :, 0, :], X[:, :, 0, :], ALU.mult), [ld, X], [TA])
        V(lambda e: e.tensor_tensor(TA[:, :, 2, :], ld[:, :, 5, :], X[:, :, 1, :], ALU.mult), [ld, X], [TA])
        V(lambda e: e.tensor_tensor(TA[:, :, 3, :], ld[:, :, 2, :], X[:, :, 1, :], ALU.mult), [ld, X], [TA])
        PL(lambda e: e.tensor_tensor(BK[:, :, 0, :], ld[:, :, 5, :], X[:, :, 3, :], ALU.mult), [ld, X], [BK])
        PL(lambda e: e.tensor_tensor(BK[:, :, 1, :], ld[:, :, 2, :], X[:, :, 3, :], ALU.mult), [ld, X], [BK])
        PL(lambda e: e.tensor_copy(Vbd[0:64, :, 0:64], ld[0:64, :, 3, :]), [ld], [Vbd])
        PL(lambda e: e.tensor_copy(Vbd[64:128, :, 64:128], ld[64:128, :, 3, :]), [ld], [Vbd])
        for hp in range(4):
            for j in range(4):
                PE(lambda e, hp=hp, j=j: e.transpose(P1.s(hp, slice(j * 128, (j + 1) * 128), slice(0, 64)), TA[:, hp, j, :], I_[:]),
                   [TA, I_], [P1.buf])
        A(lambda e: e.copy(TT[:].rearrange("p a j t -> p a (j t)"), P1.all(slice(0, 512), slice(0, 64))), [P1.buf], [TT])
        for hp in range(4):
            AtT, RtT, BtT, KtT = TT[:, hp, 0, :], TT[:, hp, 1, :], TT[:, hp, 2, :], TT[:, hp, 3, :]
            PE(lambda e, hp=hp, a=AtT, b=BtT: e.matmul(P0.s(hp, slice(0, 128)), a, b, start=True, stop=True), [TT], [P0.buf])
            PE(lambda e, hp=hp, a=BtT, b=AtT: e.matmul(P0.s(hp, slice(128, 256)), a, b, start=True, stop=True), [TT], [P0.buf])
            PE(lambda e, hp=hp, a=KtT, b=AtT: e.matmul(P0.s(hp, slice(256, 384)), a, b, start=True, stop=True), [TT], [P0.buf])
            PE(lambda e, hp=hp, a=BtT, b=RtT: e.matmul(P0.s(hp, slice(384, 512)), a, b, start=True, stop=True), [TT], [P0.buf])
            PE(lambda e, hp=hp, a=KtT, b=RtT: e.matmul(P1.s(hp, slice(0, 128)), a, b, start=True, stop=True), [TT], [P1.buf])
        V(lambda e: e.tensor_tensor(AA[0][:, :, 0, :], P0.all(slice(0, 128)), b4(Msl), ALU.mult), [P0.buf, Msl], [AA[0]])
        V(lambda e: e.tensor_tensor(AA[0][:, :, 1, :], P0.all(slice(128, 256)), b4(Msu), ALU.mult), [P0.buf, Msu], [AA[0]])
        V(lambda e: e.tensor_tensor(AXm[:, :, 0, :], P0.all(slice(256, 384)), b4(Msu), ALU.mult), [P0.buf, Msu], [AXm])
        V(lambda e: e.tensor_tensor(AXm[:, :, 1, :], P0.all(slice(384, 512)), b4(Lbd), ALU.mult), [P0.buf, Lbd], [AXm])
        V(lambda e: e.tensor_tensor(AXm[:, :, 2, :], P1.all(slice(0, 128)), b4(Lbd), ALU.mult), [P1.buf, Lbd], [AXm])
        for hp in range(4):
            PE(lambda e, hp=hp: e.matmul(P1.s(hp, slice(128, 192)), AXm[:, hp, 0, :], ld[:, hp, 3, :], start=True, stop=True), [AXm, ld], [P1.buf])
        A(lambda e: e.copy(Y[0][:, :, 0:64], TA[:, :, 0, :]), [TA], [Y[0]])
        A(lambda e: e.copy(Y[0][:, :, 64:128], P1.all(slice(128, 192))), [P1.buf], [Y[0]])
        for lev in range(6):
            a, b = lev % 2, (lev + 1) % 2
            pp = P0 if lev % 2 == 0 else P1
            for hp in range(4):
                PE(lambda e, hp=hp, a=a, pp=pp: e.matmul(pp.s(hp, slice(0, 128)), AA[a][:, hp, 1, :], Y[a][:, hp, :], start=True, stop=True),
                   [AA[a], Y[a]], [pp.buf])
            if lev < 5:
                V(lambda e, a=a, b=b, pp=pp: e.tensor_tensor(Y[b][:], pp.all(slice(0, 128)), Y[a][:], ALU.add), [pp.buf, Y[a]], [Y[b]])
            else:
                V(lambda e, a=a, pp=pp: e.tensor_tensor(Yf[:], pp.all(slice(0, 128)), Y[a][:], ALU.add), [pp.buf, Y[a]], [Yf])
            if lev < 5:
                for hp in range(4):
                    PE(lambda e, hp=hp, a=a, pp=pp: e.matmul(pp.s(hp, slice(128, 256)), AA[a][:, hp, 1, :], AA[a][:, hp, 0, :], start=True, stop=True),
                       [AA[a]], [pp.buf])
                    PE(lambda e, hp=hp, a=a, pp=pp: e.matmul(pp.s(hp, slice(256, 384)), AA[a][:, hp, 0, :], AA[a][:, hp, 1, :], start=True, stop=True),
                       [AA[a]], [pp.buf])
                A(lambda e, b=b, pp=pp: e.copy(AA[b][:].rearrange("p h a t -> p h (a t)"), pp.all(slice(128, 384))), [pp.buf], [AA[b]])
        Xf = Yf
        for hp in range(4):
            PE(lambda e, hp=hp: e.transpose(P0.s(hp, slice(0, 128), slice(0, 64)), Xf[:, hp, 0:64], I_[:]), [Xf, I_], [P0.buf])
        A(lambda e: e.copy(WT[:], P0.all(slice(0, 128), slice(0, 64))), [P0.buf], [WT])
        for hp in range(4):
            PE(lambda e, hp=hp: e.matmul(P0.s(hp, slice(128, 256)), WT[:, hp, :], hcur[:, hp, :], start=True, stop=True), [WT, hcur], [P0.buf])
        V(lambda e: e.tensor_tensor(Ubd[0:64, :, 0:64], P0.all(slice(128, 192), slice(0, 64)), Xf[0:64, :, 64:128], ALU.add), [P0.buf, Xf], [Ubd])
        V(lambda e: e.tensor_tensor(Ubd[64:128, :, 64:128], P0.all(slice(192, 256), slice(64, 128)), Xf[64:128, :, 64:128], ALU.add),
          [P0.buf, Xf], [Ubd])
        if own:
            for hp in range(4):
                PE(lambda e, hp=hp: e.matmul(P1.s(hp, slice(0, 128)), TT[:, hp, 1, :], hcur[:, hp, :], start=True, stop=False), [TT, hcur], [P1.buf])
                PE(lambda e, hp=hp: e.matmul(P1.s(hp, slice(0, 128)), AXm[:, hp, 1, :], Ubd[:, hp, :], start=False, stop=False), [AXm, Ubd], [P1.buf])
                PE(lambda e, hp=hp: e.matmul(P1.s(hp, slice(0, 128)), AXm[:, hp, 2, :], Vbd[:, hp, :], start=False, stop=True), [AXm, Vbd], [P1.buf])
            A(lambda e: e.copy(O[0:64, :, :], P1.all(slice(0, 64), slice(0, 64))), [P1.buf], [O])
            A(lambda e: e.copy(O[64:128, :, :], P1.all(slice(64, 128), slice(64, 128))), [P1.buf], [O])
        for hp in range(4):
            PE(lambda e, hp=hp: e.matmul(P0.s(hp, slice(256, 384), slice(0, 64)), BK[:, hp, 0, :], Ubd[:, hp, :], start=True, stop=False),
               [BK, Ubd], [P0.buf])
            PE(lambda e, hp=hp: e.matmul(P0.s(hp, slice(256, 384), slice(0, 64)), BK[:, hp, 1, :], Vbd[:, hp, :], start=False, stop=True),
               [BK, Vbd], [P0.buf])
        V(lambda e: e.tensor_tensor(htmp[:].rearrange("p h (a v) -> p h a v", a=2), hcur[:].rearrange("p h (a v) -> p h a v", a=2),
                                    bc(PC[:].unsqueeze(3), [64, 4, 2, 64]), ALU.mult), [hcur, PC], [htmp])
        V(lambda e: e.tensor_tensor(hnew[:], htmp[:], P0.all(slice(256, 384), slice(0, 64)), ALU.add), [htmp, P0.buf], [hnew])
        if not own:
            return
        ob = obb[c % 2]
        b64 = lambda ap: bc(ap.unsqueeze(2), [128, 4, 64])
        V(lambda e: e.tensor_reduce(stt[:, 0, :], O[:], AX.X, ALU.add), [O], [stt])
        V(lambda e: e.tensor_scalar_mul(stt[:, 1, :], stt[:, 0, :], 1.0 / 64.0), [stt], [stt])
        V(lambda e: e.tensor_tensor(t1[:], O[:], b64(stt[:, 1, :]), ALU.subtract), [O, stt], [t1])
        A(lambda e: e.activation(t2[:], t1[:], AF.Square), [t1], [t2])
        V(lambda e: e.tensor_reduce(stt[:, 2, :], t2[:], AX.X, ALU.add), [t2], [stt])
        V(lambda e: e.tensor_scalar(stt[:, 3, :], stt[:, 2, :], 1.0 / 64.0, GN_EPS_, ALU.mult, ALU.add), [stt], [stt])
        A(lambda e: e.activation(stt[:, 4, :], stt[:, 3, :], AF.Sqrt), [stt], [stt])
        V(lambda e: e.reciprocal(stt[:, 4, :], stt[:, 4, :]), [stt], [stt])
        V(lambda e: e.tensor_tensor(t1[:], t1[:], b64(stt[:, 4, :]), ALU.mult), [t1, stt], [t1])
        V(lambda e: e.tensor_tensor(t1[:], t1[:], cst[:, 0], ALU.mult), [t1, cst], [t1])
        V(lambda e: e.tensor_tensor(t1[:], t1[:], cst[:, 1], ALU.add), [t1, cst], [t1])
        PL(lambda e: e.tensor_tensor(t2[:], ld[:, :, 0, :], ld[:, :, 2, :], ALU.mult), [ld], [t2])
        PL(lambda e: e.tensor_tensor(t2[:], t2[:], cst[:, 2], ALU.mult), [t2, cst], [t2])
        V(lambda e: e.tensor_reduce(stt[:, 5, :], t2[:], AX.X, ALU.add), [t2], [stt])
        V(lambda e: e.tensor_tensor(t2[:], ld[:, :, 3, :], b64(stt[:, 5, :]), ALU.mult), [ld, stt], [t2])
        V(lambda e: e.tensor_tensor(t1[:], t1[:], t2[:], ALU.add), [t1, t2], [t1])
        V(lambda e: e.tensor_tensor(ob[:], t1[:], gt[:], ALU.mult), [t1, gt], [ob])
        for h2 in range(2):
            kb.dma("sp", orw4[to:to + 64, :, h2, :], ob[h2 * 64:(h2 + 1) * 64, :, :], [ob], [g.ORW], ob)

    for c in range(nchunks):
        chunk(c)
    kb.end()


GN_EPS_ = 64e-5


def phase_p(kb, g, nblk=128):
    kb.begin()
    V = lambda fn, r, w: kb.op("dve", fn, r, w)
    A = lambda fn, r, w: kb.op("act", fn, r, w)
    PE = lambda fn, r, w: kb.op("pe", fn, r, w)
    PL = lambda fn, r, w: kb.op("pool", fn, r, w)
    uf = [kb.sbuf(f"uf{i}", [128, 1024], F32) for i in range(3)]
    ub = [kb.sbuf(f"ub{i}", [128, 1024], BF16) for i in range(3)]
    ut = [kb.sbuf(f"ut{i}", [128, 8, 128], BF16) for i in range(3)]
    vf = [kb.sbuf(f"vf{i}", [128, 1024], F32) for i in range(3)]
    vb = [kb.sbuf(f"vb{i}", [128, 1024], BF16) for i in range(3)]
    pb = [kb.psum(f"ppb{i}", [128, 1024], BF16) for i in range(3)]

    def body(b):
        i = b % 3
        kb.dma("sp", uf[i][:], g.peer_u[b * 128:(b + 1) * 128, :], [g.peer_u], [uf[i]], uf[i])
        kb.dma("sp", vf[i][:], g.peer_v[b * 128:(b + 1) * 128, :], [g.peer_v], [vf[i]], vf[i])
        V(lambda e: e.tensor_copy(ub[i][:], uf[i][:]), [uf[i]], [ub[i]])
        PL(lambda e: e.tensor_copy(vb[i][:], vf[i][:]), [vf[i]], [vb[i]])
        kb.dma("sp", g.VB[b * 128:(b + 1) * 128, :], vb[i][:], [vb[i]], [g.VB], vb[i])
        for k in range(8):
            PE(lambda e, k=k: e.transpose(pb[i][:, k * 128:(k + 1) * 128], ub[i][:, k * 128:(k + 1) * 128], g.ident_b[:]),
               [ub[i], g.ident_b], [pb[i]])
        A(lambda e: e.copy(ut[i][:].rearrange("p k e -> p (k e)"), pb[i][:]), [pb[i]], [ut[i]])
        kb.dma("sp", g.UT.t[:, :, b * 128:(b + 1) * 128].rearrange("k p e -> p k e"), ut[i][:], [ut[i]], [g.UT], ut[i])
    for b in range(nblk):
        body(b)
    kb.end()


def phase_d(kb, g, ntl=NOWN // 128):
    kb.begin()
    V = lambda fn, r, w: kb.op("dve", fn, r, w)
    A = lambda fn, r, w: kb.op("act", fn, r, w)
    PE = lambda fn, r, w: kb.op("pe", fn, r, w)
    PL = lambda fn, r, w: kb.op("pool", fn, r, w)
    Wpm = kb.sbuf("Wpm", [128, 4, 1024], BF16)
    Wpr = kb.sbuf("Wpr", [128, 4, 1024], BF16)
    Wo = kb.sbuf("Wo", [128, 8, 1024], BF16)
    Wq = kb.sbuf("Wq", [128, 8, 2048], BF16)
    skT = kb.sbuf("skT", [128, 16, 128], BF16)
    kb.begin()
    stg = [kb.sbuf(f"dstg{i}", [128, 4, 1024], F32) for i in range(2)]
    psk = kb.psum("psk", [128, 512], F32)
    n = [0]

    def ldw(dst, src, k0, nk, c0, nc_):
        s_ = stg[n[0] % 2]
        n[0] += 1
        kb.dma("sp", s_[:, 0:nk, 0:nc_], src.t.rearrange("(k p) n -> p k n", p=128)[:, k0:k0 + nk, c0:c0 + nc_], [src], [s_], s_)
        if n[0] % 2:
            V(lambda e: e.tensor_copy(dst[:, k0:k0 + nk, c0:c0 + nc_], s_[:, 0:nk, 0:nc_]), [s_], [dst])
        else:
            A(lambda e: e.copy(dst[:, k0:k0 + nk, c0:c0 + nc_], s_[:, 0:nk, 0:nc_]), [s_], [dst])
    ldw(Wpm, g.w_proj_moba, 0, 4, 0, 1024)
    ldw(Wpr, g.w_proj_rwkv, 0, 4, 0, 1024)
    ldw(Wo, g.w_out, 0, 4, 0, 1024)
    ldw(Wo, g.w_out, 4, 4, 0, 1024)
    for k0 in (0, 4):
        for c0 in (0, 1024):
            ldw(Wq, g.peer_wq, k0, 4, c0, 1024)
    for hp in range(16):
        s_ = stg[n[0] % 2]
        n[0] += 1
        kb.dma("sp", s_[:, 0, 0:128], g.peer_sk[hp], [g.peer_sk], [s_], s_)
        PE(lambda e, s_=s_: e.transpose(psk[:, 0:128], s_[:, 0, 0:128], g.ident_f[:]), [s_, g.ident_f], [psk])
        V(lambda e, hp=hp: e.tensor_copy(skT[:, hp, :], psk[:, 0:128]), [psk], [skT])
    kb.end()

    NB = 2
    om = [kb.sbuf(f"om{i}", [128, 2, 512], BF16) for i in range(NB)]
    gts = [kb.sbuf(f"gts{i}", [128, 2048], BF16) for i in range(NB)]
    xo = [kb.sbuf(f"xo{i}", [128, 1024], F32) for i in range(NB)]
    oT = [kb.sbuf(f"oT{i}", [128, 8, 128], BF16) for i in range(NB)]
    m1 = [kb.sbuf(f"dm1{i}", [128, 1024], F32) for i in range(NB)]
    mix = [kb.sbuf(f"mix{i}", [128, 1024], BF16) for i in range(NB)]
    mixT = [kb.sbuf(f"mixT{i}", [128, 8, 128], BF16) for i in range(NB)]
    h1 = [kb.sbuf(f"h1{i}", [128, 1024], F32) for i in range(NB)]
    junk = kb.sbuf("djunk", [128, 1024], BF16)
    stt = [kb.sbuf(f"dstt{i}", [128, 4], F32) for i in range(NB)]
    xn2T = [kb.sbuf(f"xn2T{i}", [128, 8, 128], BF16) for i in range(NB)]
    qT = [kb.sbuf(f"qT{i}", [128, 16, 128], BF16) for i in range(NB)]
    S = [kb.sbuf(f"S{i}", [128, 16, 128], F32) for i in range(NB)]
    S2 = [kb.sbuf(f"S2{i}", [128, 128], F32) for i in range(NB)]
    top = [kb.sbuf(f"top{i}", [128, 16, 16], F32) for i in range(NB)]
    cand = [kb.sbuf(f"cand{i}", [128, 8, 256], F32) for i in range(NB)]
    c2 = [kb.sbuf(f"c2{i}", [128, 256], F32) for i in range(NB)]
    t16 = [kb.sbuf(f"t16{i}", [128, 8, 16], F32) for i in range(NB)]
    thr = [kb.sbuf(f"thr{i}", [128, 4, 8], F32) for i in range(NB)]
    pB = [kb.psum(f"dpB{i}", [128, 1024], BF16) for i in range(2)]
    pM = [kb.psum(f"dpM{i}", [128, 512], F32) for i in range(4)]
    pT = [kb.psum(f"dpT{i}", [128, 512], F32) for i in range(2)]
    cn = {"m": 0}

    def nM():
        cn["m"] += 1
        return pM[cn["m"] % 4]

    def body(it):
        i = it % NB
        t0 = it * 128
        om_, g_, x_, oT_, m1_, mix_, mixT_, h1_, st_, xn_, qT_, S_, S2_, top_, cand_, c2_, t16_, thr_ = (
            om[i], gts[i], xo[i], oT[i], m1[i], mix[i], mixT[i], h1[i], stt[i], xn2T[i], qT[i], S[i], S2[i], top[i], cand[i],
            c2[i], t16[i], thr[i])
        kb.dma("sp", om_[:, 0, :], g.OM[t0:t0 + 128, :], [g.OM], [om_], om_)
        kb.dma("sp", om_[:, 1, :], g.ORW[t0:t0 + 128, :], [g.ORW], [om_], om_)
        kb.dma("sp", g_[:], g.GATES[t0:t0 + 128, :], [g.GATES], [g_], g_)
        kb.dma("sp", x_[:], g.xs[NT - NOWN + t0:NT - NOWN + t0 + 128, :], [g.xs], [x_], x_)
        pb = pB[it % 2]
        for j in range(8):
            PE(lambda e, j=j: e.transpose(pb[:, j * 128:(j + 1) * 128], om_[:, j // 4, (j % 4) * 128:(j % 4 + 1) * 128], g.ident_b[:]),
               [om_, g.ident_b], [pb])
        A(lambda e: e.copy(oT_[:].rearrange("p k t -> p (k t)"), pb[:]), [pb], [oT_])
        for br, W_ in ((0, Wpm), (1, Wpr)):
            for hf in range(2):
                p_ = nM()
                for k in range(4):
                    PE(lambda e, k=k, p_=p_, W_=W_, br=br, hf=hf: e.matmul(p_[:], oT_[:, br * 4 + k, :], W_[:, k, hf * 512:(hf + 1) * 512],
                                                                           start=(k == 0), stop=(k == 3)), [oT_, W_], [p_])
                cs_ = slice(hf * 512, (hf + 1) * 512)
                gs_ = slice(br * 1024 + hf * 512, br * 1024 + (hf + 1) * 512)
                if br == 0:
                    V(lambda e, p_=p_, cs_=cs_, gs_=gs_: e.tensor_tensor(m1_[:, cs_], p_[:], g_[:, gs_], ALU.mult), [p_, g_], [m1_])
                else:
                    V(lambda e, p_=p_, cs_=cs_, gs_=gs_: e.tensor_tensor(h1_[:, cs_], p_[:], g_[:, gs_], ALU.mult), [p_, g_], [h1_])
                    PL(lambda e, cs_=cs_: e.tensor_tensor(mix_[:, cs_], m1_[:, cs_], h1_[:, cs_], ALU.add), [m1_, h1_], [mix_])
        pb2 = pB[(it + 1) % 2]
        for j in range(8):
            PE(lambda e, j=j: e.transpose(pb2[:, j * 128:(j + 1) * 128], mix_[:, j * 128:(j + 1) * 128], g.ident_b[:]),
               [mix_, g.ident_b], [pb2])
        A(lambda e: e.copy(mixT_[:].rearrange("p k t -> p (k t)"), pb2[:]), [pb2], [mixT_])
        for hf in range(2):
            p_ = nM()
            for k in range(8):
                PE(lambda e, k=k, p_=p_, hf=hf: e.matmul(p_[:], mixT_[:, k, :], Wo[:, k, hf * 512:(hf + 1) * 512],
                                                         start=(k == 0), stop=(k == 7)), [mixT_, Wo], [p_])
            cs_ = slice(hf * 512, (hf + 1) * 512)
            V(lambda e, p_=p_, cs_=cs_: e.tensor_tensor(h1_[:, cs_], p_[:], g.gt_bc[:, 0, cs_], ALU.mult), [p_, g.gt_bc], [h1_])
            V(lambda e, cs_=cs_: e.tensor_tensor(h1_[:, cs_], h1_[:, cs_], x_[:, cs_], ALU.add), [h1_, x_], [h1_])
        kb.dma("sp", g.H1[t0:t0 + 128, :], h1_[:], [h1_], [g.H1], h1_)
        A(lambda e: e.activation(junk[:], h1_[:], AF.Square, accum_out=st_[:, 0:1]), [h1_], [junk, st_])
        V(lambda e: e.tensor_scalar(st_[:, 1:2], st_[:, 0:1], 1.0 / 1024.0, 1e-6, ALU.mult, ALU.add), [st_], [st_])
        A(lambda e: e.activation(st_[:, 2:3], st_[:, 1:2], AF.Sqrt), [st_], [st_])
        V(lambda e: e.reciprocal(st_[:, 2:3], st_[:, 2:3]), [st_], [st_])
        V(lambda e: e.tensor_scalar_mul(m1_[:], h1_[:], st_[:, 2:3]), [h1_, st_], [m1_])
        for k in range(8):
            pt = pT[k // 4]
            PE(lambda e, k=k, pt=pt: e.transpose(pt[:, (k % 4) * 128:(k % 4 + 1) * 128], m1_[:, k * 128:(k + 1) * 128], g.ident_f[:]),
               [m1_, g.ident_f], [pt])
            A(lambda e, k=k, pt=pt: e.activation(xn_[:, k, :], pt[:, (k % 4) * 128:(k % 4 + 1) * 128], AF.Identity,
                                                 bias=g.modc[:, 24 + k:25 + k], scale=g.sc2p[:, k:k + 1]), [pt, g.modc, g.sc2p], [xn_])
        kb.dma("sp", g.XN2T.t[:, :, t0:t0 + 128].rearrange("k p t -> p k t"), xn_[:], [xn_], [g.XN2T], xn_)
        for q4 in range(4):
            p_ = nM()
            for jj in range(4):
                hp = q4 * 4 + jj
                for k in range(8):
                    PE(lambda e, k=k, p_=p_, hp=hp, jj=jj: e.matmul(p_[:, jj * 128:(jj + 1) * 128], Wq[:, k, hp * 128:(hp + 1) * 128], xn_[:, k, :],
                                                                  start=(k == 0), stop=(k == 7)), [Wq, xn_], [p_])
            A(lambda e, p_=p_, q4=q4: e.copy(qT_[:, q4 * 4:(q4 + 1) * 4, :].rearrange("p a t -> p (a t)"), p_[:]), [p_], [qT_])
        for q4 in range(4):
            p_ = nM()
            for jj in range(4):
                hp = q4 * 4 + jj
                PE(lambda e, p_=p_, hp=hp, jj=jj: e.matmul(p_[:, jj * 128:(jj + 1) * 128], qT_[:, hp, :], skT[:, hp, :], start=True, stop=True),
                   [qT_, skT], [p_])
            V(lambda e, p_=p_, q4=q4: e.tensor_copy(S_[:, q4 * 4:(q4 + 1) * 4, :].rearrange("p a n -> p (a n)"), p_[:]), [p_], [S_])
        kb.dma("sp", g.SC[t0:t0 + 128, :, :], S_[:], [S_], [g.SC], S_)
        for hp in range(16):
            V(lambda e, hp=hp: e.max(out=top_[:, hp, 0:8], in_=S_[:, hp, :]), [S_], [top_])
            V(lambda e, hp=hp: e.match_replace(out=S2_[:], in_to_replace=top_[:, hp, 0:8], in_values=S_[:, hp, :], imm_value=-1e30),
              [S_, top_], [S2_])
            V(lambda e, hp=hp: e.max(out=top_[:, hp, 8:16], in_=S2_[:]), [S2_], [top_])
        t4 = top_[:].rearrange("p (h two) a -> p h two a", two=2)
        V(lambda e: e.tensor_tensor(cand_[:].rearrange("p h (a b) -> p h a b", b=16), bc(t4[:, :, 0, :].unsqueeze(3), [128, 8, 16, 16]),
                                    bc(t4[:, :, 1, :].unsqueeze(2), [128, 8, 16, 16]), ALU.add), [top_], [cand_])
        for h in range(8):
            V(lambda e, h=h: e.max(out=t16_[:, h, 0:8], in_=cand_[:, h, :]), [cand_], [t16_])
            V(lambda e, h=h: e.match_replace(out=c2_[:], in_to_replace=t16_[:, h, 0:8], in_values=cand_[:, h, :], imm_value=-1e30),
              [cand_, t16_], [c2_])
            V(lambda e, h=h: e.max(out=t16_[:, h, 8:16], in_=c2_[:]), [c2_], [t16_])
        V(lambda e: e.tensor_copy(thr_[:, 0, :], t16_[:, :, 15]), [t16_], [thr_])
        V(lambda e: e.tensor_scalar_mul(thr_[:, 1, :], t16_[:, :, 0], -1.0), [t16_], [thr_])
        V(lambda e: e.tensor_tensor(t16_[:], t16_[:], bc(thr_[:, 1, :].unsqueeze(2), [128, 8, 16]), ALU.add), [t16_, thr_], [t16_])
        A(lambda e: e.activation(t16_[:], t16_[:], AF.Exp), [t16_], [t16_])
        V(lambda e: e.tensor_reduce(thr_[:, 3, :], t16_[:], AX.X, ALU.add), [t16_], [thr_])
        V(lambda e: e.reciprocal(thr_[:, 2, :], thr_[:, 3, :]), [thr_], [thr_])
        kb.dma("sp", g.TH[t0:t0 + 128, :, :], thr_[:], [thr_], [g.TH], thr_)
    for it in range(ntl):
        body(it)
    kb.end()


def phase_e(kb, g, ngroups=NOWN // 256, nec=16):
    kb.begin()
    V = lambda fn, r, w: kb.op("dve", fn, r, w)
    A = lambda fn, r, w: kb.op("act", fn, r, w)
    PE = lambda fn, r, w: kb.op("pe", fn, r, w)
    PL = lambda fn, r, w: kb.op("pool", fn, r, w)
    UTs = [kb.sbuf(f"UTs{i}", [128, 8, 1024], BF16) for i in range(2)]
    VBs = [kb.sbuf(f"VBs{i}", [128, 8, 1024], BF16) for i in range(2)]
    xn = [kb.sbuf(f"exn{i}", [128, 8, 256], BF16) for i in range(2)]
    Ssb = [kb.sbuf(f"eS{i}", [128, 2, 16, 128], F32) for i in range(1)]
    th = [kb.sbuf(f"eth{i}", [128, 2, 4, 8], F32) for i in range(2)]
    Dg = [kb.sbuf(f"eDg{i}", [128, 2, 8, 128], BF16) for i in range(2)]
    Zt = [kb.sbuf(f"eZ{i}", [128, 1024], F32) for i in range(3)]
    Et = [kb.sbuf(f"eE{i}", [128, 1024], BF16) for i in range(3)]
    Mt = [[[kb.sbuf(f"eM{b}_{tt}_{h}", [128, 1024], BF16) for h in range(8)] for tt in range(2)] for b in range(2)]
    gl = [kb.sbuf(f"egl{i}", [128, 256], BF16) for i in range(3)]
    hd = [kb.sbuf(f"ehd{i}", [128, 256], BF16) for i in range(3)]
    h1t = [kb.sbuf(f"eh1{i}", [128, 2, 1024], F32) for i in range(1)]
    fo = [kb.sbuf(f"efo{i}", [128, 1024], F32) for i in range(2)]
    pO = [kb.psum(f"epO{i}", [128, 512], F32) for i in range(4)]
    pS = [kb.psum(f"epS{i}", [128, 512], F32) for i in range(2)]
    pG = [kb.psum(f"epG{i}", [128, 512], F32) for i in range(2)]
    cn = {"z": 0, "s": 0, "g": 0, "h": 0}
    utv = g.UT.t.rearrange("k p e -> p k e")
    vbv = g.VB.t.rearrange("(b p) d -> p b d", p=128)

    class G_:
        pass

    def setup_group(tg):
        c = G_()
        i = tg % 2
        c.t0 = tg * 256
        c.xn, c.S, c.th, c.Dg, c.h1 = xn[i], Ssb[0], th[i], Dg[i], h1t[0]
        t0 = c.t0
        kb.dma("sp", c.xn[:], g.XN2T.t[:, :, t0:t0 + 256].rearrange("k p t -> p k t"), [g.XN2T], [c.xn], c.xn)
        for tt in range(2):
            kb.dma("sp", c.S[:, tt], g.SC[t0 + tt * 128:t0 + (tt + 1) * 128, :, :], [g.SC], [c.S], c.S)
            kb.dma("sp", c.th[:, tt], g.TH[t0 + tt * 128:t0 + (tt + 1) * 128, :, :], [g.TH], [c.th], c.th)
            for h in range(8):
                V(lambda e, tt=tt, h=h: e.tensor_scalar_mul(c.Dg[:, tt, h, :], g.ident_f[:], c.th[:, tt, 2, h:h + 1]), [g.ident_f, c.th], [c.Dg])
        return c

    def gate_tasks(c, k, ec):
        w = k % 2
        U_, Vb_ = UTs[w], VBs[w]
        tasks = []

        def loadw():
            kb.dma("sp", U_[:], utv[:, :, ec * 1024:(ec + 1) * 1024], [g.UT], [U_], U_)
            kb.dma("sp", Vb_[:], vbv[:, ec * 8:(ec + 1) * 8, :], [g.VB], [Vb_], Vb_)
        for tt in range(2):
            for h in range(8):
                def gate(tt=tt, h=h, first=(tt == 0 and h == 0)):
                    if first:
                        loadw()
                    z = cn["z"] % 3
                    cn["z"] += 1
                    Z_, E_, M_ = Zt[z], Et[z], Mt[w][tt][h]
                    PL(lambda e: e.tensor_tensor(Z_[:].rearrange("p (a b) -> p a b", b=128),
                                                 bc(c.S[:, tt, 2 * h, ec * 8:(ec + 1) * 8].unsqueeze(2), [128, 8, 128]),
                                                 bc(c.S[:, tt, 2 * h + 1, :].unsqueeze(1), [128, 8, 128]), ALU.add), [c.S], [Z_])
                    A(lambda e: e.activation(E_[:], Z_[:], AF.Exp, bias=c.th[:, tt, 1, h:h + 1]), [Z_, c.th], [E_])
                    V(lambda e: e.scalar_tensor_tensor(M_[:], Z_[:], c.th[:, tt, 0, h:h + 1], E_[:], ALU.is_ge, ALU.mult),
                      [Z_, c.th, E_], [M_])
                tasks.append(gate)
        return tasks

    def blk_tasks(c, k, ec):
        w = k % 2
        U_, Vb_ = UTs[w], VBs[w]
        tasks = []
        for ib in range(8):
            def blk(ib=ib):
                eb = ec * 8 + ib
                ps_ = pS[cn["s"] % 2]
                pg_ = pG[cn["g"] % 2]
                cn["s"] += 1
                cn["g"] += 1
                for kk in range(8):
                    PE(lambda e, kk=kk: e.matmul(ps_[:, 0:256], U_[:, kk, ib * 128:(ib + 1) * 128], c.xn[:, kk, :], start=(kk == 0), stop=(kk == 7)),
                       [U_, c.xn], [ps_])
                for tt in range(2):
                    for h in range(8):
                        M_ = Mt[w][tt][h]
                        PE(lambda e, tt=tt, h=h, M_=M_: e.matmul(pg_[:, tt * 128:(tt + 1) * 128], M_[:, ib * 128:(ib + 1) * 128], c.Dg[:, tt, h, :],
                                                               start=(h == 0), stop=(h == 7)), [M_, c.Dg], [pg_])
                j = cn["h"] % 3
                cn["h"] += 1
                A(lambda e: e.activation(gl[j][:], ps_[:, 0:256], AF.Gelu), [ps_], [gl[j]])
                V(lambda e: e.tensor_tensor(hd[j][:], gl[j][:], pg_[:, 0:256], ALU.mult), [gl[j], pg_], [hd[j]])
                for tt in range(2):
                    for hf in range(2):
                        PE(lambda e, tt=tt, hf=hf: e.matmul(pO[tt * 2 + hf][:], hd[j][:, tt * 128:(tt + 1) * 128], Vb_[:, ib, hf * 512:(hf + 1) * 512],
                                                          start=(eb == 0), stop=(eb == nec * 8 - 1)), [hd[j], Vb_], [pO[tt * 2 + hf]])
            tasks.append(blk)
        return tasks

    def finalize(c):
        t0 = c.t0
        for tt in range(2):
            kb.dma("sp", c.h1[:, tt, :], g.H1[t0 + tt * 128:t0 + (tt + 1) * 128, :], [g.H1], [c.h1], c.h1)
        for tt in range(2):
            f_ = fo[tt]
            for hf in range(2):
                cs_ = slice(hf * 512, (hf + 1) * 512)
                V(lambda e, tt=tt, hf=hf, cs_=cs_, f_=f_: e.tensor_tensor(f_[:, cs_], pO[tt * 2 + hf][:], g.gt_bc[:, 1, cs_], ALU.mult),
                  [pO[tt * 2 + hf], g.gt_bc], [f_])
                V(lambda e, tt=tt, cs_=cs_, f_=f_: e.tensor_tensor(f_[:, cs_], f_[:, cs_], c.h1[:, tt, cs_], ALU.add), [f_, c.h1], [f_])
            kb.dma("sp", g.out[t0 + tt * 128:t0 + (tt + 1) * 128, :], f_[:], [f_], [g.out], f_)

    chunks = [(tg, ec) for tg in range(ngroups) for ec in range(nec)]
    ctxs = {}

    def ctx_of(tg):
        if tg not in ctxs:
            ctxs[tg] = setup_group(tg)
        return ctxs[tg]
    tg0, ec0 = chunks[0]
    for t in gate_tasks(ctx_of(tg0), 0, ec0):
        t()
    for k, (tg, ec) in enumerate(chunks):
        c = ctx_of(tg)
        nxt = None
        if k + 1 < len(chunks):
            tgn, ecn = chunks[k + 1]
            nxt = gate_tasks(ctx_of(tgn), k + 1, ecn)
        bt = blk_tasks(c, k, ec)
        for ib in range(8):
            bt[ib]()
            if nxt is not None:
                nxt[2 * ib]()
                nxt[2 * ib + 1]()
        if ec == nec - 1:
            finalize(c)
    kb.end()


def build(dbg=False):
    nc = bass.Bass("TRN2", target_bir_lowering=False)
    gst = ExitStack()
    with gst:
        kb = KB(nc, gst)
        g = declare(kb, dbg)
        phase0(kb, g)
        phase_a(kb, g)
        phase_p(kb, g)
        phase_b(kb, g)
        phase_c(kb, g)
        phase_d(kb, g)
        phase_e(kb, g)
        kb.begin()
        kb.wait_all("sp", [g.out])
        kb.end()
    return nc


def host_inputs(inputs, core, shared):
    b, half = core // 2, core % 2
    x = np.asarray(inputs["x"], dtype=np.float32)
    pos = np.asarray(inputs["positions"]).astype(np.int32)
    xs = np.zeros((NT, 1024), np.float32)
    ps = np.zeros((NT, 1), np.int32)
    if half == 1:
        xs[:] = x[b]
        ps[:, 0] = pos[b]
    else:
        xs[NOWN:] = x[b, :NOWN]
        ps[NOWN:, 0] = pos[b, :NOWN]
    m = dict(shared)
    m["xs"] = xs
    m["pos"] = ps
    m["cc"] = np.ascontiguousarray(np.asarray(inputs["c"], np.float32)[b].reshape(8, 128))
    m["pv"] = np.full((128, 1), float(half), np.float32)
    return m


def shared_inputs(inputs):
    m = {}
    invf = (500000.0 ** (-(np.arange(8, dtype=np.float32) * 2.0) / 16.0)).astype(np.float32)
    m["invf"] = np.ascontiguousarray(np.broadcast_to(invf[None, :], (128, 8)))

    def w(name, shape, key=None):
        m[name] = np.ascontiguousarray(np.asarray(inputs[key or name], np.float32).reshape(shape))
    w("w_ada", (1024, 6144)); w("b_ada", (1, 6144)); w("norm1_g", (1, 1024)); w("w_in", (1024, 5376))
    w("q_norm_g", (1, 64)); w("k_norm_g", (1, 64)); w("rwkv_mu", (1, 1792)); w("rwkv_w0", (1, 512))
    w("rwkv_w2", (64, 512)); w("rwkv_a0", (1, 512)); w("rwkv_a2", (64, 512)); w("rwkv_g2", (128, 512))
    w("rwkv_k_k", (1, 512)); w("rwkv_k_a", (1, 512)); w("rwkv_r_k", (1, 512)); w("rwkv_ln_g", (1, 512))
    w("rwkv_ln_b", (1, 512)); w("w_proj_moba", (512, 1024)); w("w_proj_rwkv", (512, 1024)); w("w_out", (1024, 1024))
    w("norm2_g", (1, 1024)); w("peer_wq", (1024, 2048)); w("peer_sk", (16, 128, 128), "peer_subkeys")
    w("peer_u", (16384, 1024)); w("peer_v", (16384, 1024))
    return m


def kernel(**inputs):
    nc = build(False)
    shared = shared_inputs(inputs)
    in_maps = [host_inputs(inputs, c, shared) for c in range(8)]
    res = run_bass_kernel_spmd(nc, in_maps, core_ids=list(range(8)))
    out = np.zeros((4, 8192, 1024), np.float32)
    for c in range(8):
        b, half = c // 2, c % 2
        out[b, half * NOWN:(half + 1) * NOWN] = np.asarray(res.results[c]["out"], np.float32)
    return out
```

```python
import numpy as np
import concourse.bass as bass
import concourse.mybir as mybir
from concourse.bass_utils import run_bass_kernel_spmd
from contextlib import ExitStack

F32 = mybir.dt.float32
BF16 = mybir.dt.bfloat16
I32 = mybir.dt.int32
AF = mybir.ActivationFunctionType
ALU = mybir.AluOpType
AX = mybir.AxisListType
EPOCH = 24000
P = 128


class Buf:
    def __init__(self, t, name):
        self.t = t
        self.name = name
        self.ws = {}
        self.rs = {}
        self.dkey = None
        self.dcnt = 0

    def __getitem__(self, k):
        return self.t[k]


class KB:
    ENG = ("pe", "act", "dve", "pool", "sp")

    def __init__(self, nc, stack):
        self.nc = nc
        self.stack = stack
        self.phs = []
        self.scope_bufs = []
        self.dpool = []
        self.ops = {e: [] for e in self.ENG}
        self.cnt = {e: 0 for e in self.ENG}
        self.known = {e: {} for e in self.ENG}
        self.sems = {}
        self.nsem = 0
        self.nins = 0

    def sem(self, key):
        if key not in self.sems:
            self.sems[key] = self.stack.enter_context(self.nc.semaphore(f"s{self.nsem}"))
            self.nsem += 1
        return self.sems[key]

    def gsbuf(self, name, shape, dt):
        return Buf(self.stack.enter_context(self.nc.sbuf_tensor(name, list(shape), dt)), name)

    def sbuf(self, name, shape, dt):
        self.nsem += 0
        self.uid = getattr(self, "uid", 0) + 1
        name = f"{name}_u{self.uid}"
        b = Buf(self.phs[-1].enter_context(self.nc.sbuf_tensor(name, list(shape), dt)), name)
        self.scope_bufs[-1].append(b)
        return b

    def psum(self, name, shape, dt):
        self.uid = getattr(self, "uid", 0) + 1
        name = f"{name}_u{self.uid}"
        return Buf(self.phs[-1].enter_context(self.nc.psum_tensor(name, list(shape), dt)), name)

    def dram(self, name, shape, dt, kind="Internal"):
        return Buf(self.nc.dram_tensor(name, list(shape), dt, kind=kind).ap(), name)

    def _deps(self, eng, reads, writes):
        d = {}
        for b in reads:
            for k, v in b.ws.items():
                if d.get(k, 0) < v:
                    d[k] = v
        for b in writes:
            for k, v in b.ws.items():
                if d.get(k, 0) < v:
                    d[k] = v
            for k, v in b.rs.items():
                if d.get(k, 0) < v:
                    d[k] = v
        kn = self.known[eng]
        for k, v in d.items():
            if eng == "pe" and k[0] == "pe":
                continue
            if kn.get(k, 0) >= v:
                continue
            kn[k] = v
            self.ops[eng].append(("wait", k, v))

    def op(self, eng, fn, reads=(), writes=()):
        self._deps(eng, reads, writes)
        self.cnt[eng] += 1
        n = self.cnt[eng]
        key = (eng, (n - 1) // EPOCH)
        val = (n - 1) % EPOCH + 1
        self.sem(key)
        self.ops[eng].append(("op", fn, key))
        for b in reads:
            if b.rs.get(key, 0) < val:
                b.rs[key] = val
        for b in writes:
            if b.ws.get(key, 0) < val:
                b.ws[key] = val

    def dma(self, q, out_ap, in_ap, reads, writes, sb, **kw):
        self._deps(q, reads, writes)
        if sb.dkey is None:
            if self.dpool:
                sb.dkey, sb.dcnt = self.dpool.pop()
            else:
                sb.dkey = ("d", sb.name)
                self.sem(sb.dkey)
        sb.dcnt += 16
        key, val = sb.dkey, sb.dcnt
        self.ops[q].append(("dma", out_ap, in_ap, key, kw))
        for b in reads:
            if b.rs.get(key, 0) < val:
                b.rs[key] = val
        for b in writes:
            if b.ws.get(key, 0) < val:
                b.ws[key] = val

    def wait_all(self, eng, bufs):
        self._deps(eng, bufs, ())

    def begin(self):
        st = ExitStack()
        st.__enter__()
        self.phs.append(st)
        self.scope_bufs.append([])

    def end(self):
        nc = self.nc
        mine = self.scope_bufs.pop()
        for b in mine:
            if b.dkey is not None:
                if self.known["sp"].get(b.dkey, 0) < b.dcnt:
                    self.known["sp"][b.dkey] = b.dcnt
                    self.ops["sp"].append(("wait", b.dkey, b.dcnt))
                self.dpool.append((b.dkey, b.dcnt))
        with nc.Block() as blk:
            def run(e, name):
                pend = None
                for o in self.ops[name]:
                    if o[0] == "wait":
                        if pend is not None:
                            self.nins += 1
                            e.wait_ge(self.sems[pend[1]], pend[2])
                        pend = o
                        continue
                    self.nins += 1
                    if o[0] == "op":
                        ins = o[1](e)
                        if pend is not None:
                            ins._wait_ge(self.sems[pend[1]], pend[2])
                        ins.then_inc(self.sems[o[2]], 1)
                    else:
                        ins = e.dma_start(out=o[1], in_=o[2], **o[4])
                        if pend is not None:
                            ins._wait_ge(self.sems[pend[1]], pend[2])
                        ins.then_inc(self.sems[o[3]], 16)
                    pend = None
                if pend is not None:
                    self.nins += 1
                    e.wait_ge(self.sems[pend[1]], pend[2])
                self.ops[name] = []

            @blk.tensor
            def _(e):
                run(e, "pe")

            @blk.scalar
            def _(e):
                run(e, "act")

            @blk.vector
            def _(e):
                run(e, "dve")

            @blk.gpsimd
            def _(e):
                run(e, "pool")

            @blk.sync
            def _(e):
                run(e, "sp")
        self.phs.pop().__exit__(None, None, None)


NT = 8192
NOWN = 4096
NTILE = NT // P
OWN0 = (NT - NOWN) // P


def bc(ap, shape):
    return ap.to_broadcast(list(shape))


class Ctx:
    pass


def declare(kb, dbg):
    g = Ctx()
    g.dbg = dbg
    sk = "ExternalOutput" if dbg else "Internal"
    EI = "ExternalInput"
    g.xs = kb.dram("xs", [NT, 1024], F32, EI)
    g.pos = kb.dram("pos", [NT, 1], I32, EI)
    g.cc = kb.dram("cc", [8, 128], F32, EI)
    g.pvd = kb.dram("pv", [128, 1], F32, EI)
    g.invf = kb.dram("invf", [128, 8], F32, EI)
    g.w_ada = kb.dram("w_ada", [1024, 6144], F32, EI)
    g.b_ada = kb.dram("b_ada", [1, 6144], F32, EI)
    g.norm1_g = kb.dram("norm1_g", [1, 1024], F32, EI)
    g.w_in = kb.dram("w_in", [1024, 5376], F32, EI)
    g.q_norm_g = kb.dram("q_norm_g", [1, 64], F32, EI)
    g.k_norm_g = kb.dram("k_norm_g", [1, 64], F32, EI)
    g.rwkv_mu = kb.dram("rwkv_mu", [1, 1792], F32, EI)
    g.rwkv_w0 = kb.dram("rwkv_w0", [1, 512], F32, EI)
    g.rwkv_w2 = kb.dram("rwkv_w2", [64, 512], F32, EI)
    g.rwkv_a0 = kb.dram("rwkv_a0", [1, 512], F32, EI)
    g.rwkv_a2 = kb.dram("rwkv_a2", [64, 512], F32, EI)
    g.rwkv_g2 = kb.dram("rwkv_g2", [128, 512], F32, EI)
    g.rwkv_k_k = kb.dram("rwkv_k_k", [1, 512], F32, EI)
    g.rwkv_k_a = kb.dram("rwkv_k_a", [1, 512], F32, EI)
    g.rwkv_r_k = kb.dram("rwkv_r_k", [1, 512], F32, EI)
    g.rwkv_ln_g = kb.dram("rwkv_ln_g", [1, 512], F32, EI)
    g.rwkv_ln_b = kb.dram("rwkv_ln_b", [1, 512], F32, EI)
    g.w_proj_moba = kb.dram("w_proj_moba", [512, 1024], F32, EI)
    g.w_proj_rwkv = kb.dram("w_proj_rwkv", [512, 1024], F32, EI)
    g.w_out = kb.dram("w_out", [1024, 1024], F32, EI)
    g.norm2_g = kb.dram("norm2_g", [1, 1024], F32, EI)
    g.peer_wq = kb.dram("peer_wq", [1024, 2048], F32, EI)
    g.peer_sk = kb.dram("peer_sk", [16, 128, 128], F32, EI)
    g.peer_u = kb.dram("peer_u", [16384, 1024], F32, EI)
    g.peer_v = kb.dram("peer_v", [16384, 1024], F32, EI)
    g.out = kb.dram("out", [NOWN, 1024], F32, "ExternalOutput")
    g.QT = kb.dram("QT", [4, 128, NOWN], BF16, sk)
    g.KT = kb.dram("KT", [4, 128, NT], BF16, sk)
    g.VM = kb.dram("VM", [NT, 512], BF16, sk)
    g.RS = kb.dram("RS", [6, 8, NT, 64], F32, sk)
    g.GR = kb.dram("GR", [NOWN, 512], F32, sk)
    g.GATES = kb.dram("GATES", [NOWN, 2048], BF16, sk)
    g.OM = kb.dram("OM", [NOWN, 512], BF16, sk)
    g.ORW = kb.dram("ORW", [NOWN, 512], BF16, sk)
    g.H1 = kb.dram("H1", [NOWN, 1024], F32, sk)
    g.XN2T = kb.dram("XN2T", [8, 128, NOWN], BF16, sk)
    g.SC = kb.dram("SC", [NOWN, 16, 128], F32, sk)
    g.TH = kb.dram("TH", [NOWN, 4, 8], F32, sk)
    g.UT = kb.dram("UT", [8, 128, 16384], BF16, "Internal")
    g.VB = kb.dram("VB", [16384, 1024], BF16, "Internal")
    g.ident_f = kb.gsbuf("ident_f", [128, 128], F32)
    g.ident_b = kb.gsbuf("ident_b", [128, 128], BF16)
    g.ones_f = kb.gsbuf("ones_f", [128, 128], F32)
    g.modc = kb.gsbuf("modc", [128, 48], F32)
    g.sc1p = kb.gsbuf("sc1p", [128, 8], F32)
    g.sc2p = kb.gsbuf("sc2p", [128, 8], F32)
    g.gt_bc = kb.gsbuf("gt_bc", [128, 2, 1024], F32)
    g.pv = kb.gsbuf("pvt", [128, 1], F32)
    g.kmT = kb.gsbuf("kmT", [128, 4, NT // 256], F32)
    g.npi = kb.gsbuf("npi", [128, 1], F32)
    return g


def phase0(kb, g):
    kb.begin()
    V = lambda fn, r, w: kb.op("dve", fn, r, w)
    A = lambda fn, r, w: kb.op("act", fn, r, w)
    PE = lambda fn, r, w: kb.op("pe", fn, r, w)
    PL = lambda fn, r, w: kb.op("pool", fn, r, w)
    PL(lambda e: e.memset(g.ones_f[:], 1.0), [], [g.ones_f])
    PL(lambda e: e.memset(g.npi[:], -float(np.pi)), [], [g.npi])
    PL(lambda e: e.memset(g.ident_f[:], 1.0), [], [g.ident_f])
    PL(lambda e: e.affine_select(out=g.ident_f[:], in_=g.ident_f[:], pattern=[[-1, 128]],
                                 compare_op=ALU.is_equal, fill=0.0, base=0, channel_multiplier=1),
       [g.ident_f], [g.ident_f])
    V(lambda e: e.tensor_copy(g.ident_b[:], g.ident_f[:]), [g.ident_f], [g.ident_b])
    kb.dma("sp", g.pv[:], g.pvd[:], [g.pvd], [g.pv], g.pv)
    c8 = kb.sbuf("c8", [8, 128], F32)
    scT = kb.sbuf("scT", [128, 8], F32)
    modrow = kb.sbuf("modrow", [1, 6144], F32)
    brow = kb.sbuf("brow", [1, 6144], F32)
    grow = kb.sbuf("grow", [1, 2, 1024], F32)
    gcol = kb.sbuf("gcol", [128, 16], F32)
    wst = [kb.sbuf(f"wst{i}", [128, 8, 512], F32) for i in range(2)]
    ps = kb.psum("p0ps", [128, 512], F32)
    ps2 = kb.psum("p0ps2", [128, 512], F32)
    kb.dma("sp", c8[:], g.cc[:], [g.cc], [c8], c8)
    kb.dma("sp", brow[:], g.b_ada[:], [g.b_ada], [brow], brow)
    kb.dma("sp", grow[:, 0, :], g.norm1_g[:], [g.norm1_g], [grow], grow)
    kb.dma("sp", grow[:, 1, :], g.norm2_g[:], [g.norm2_g], [grow], grow)
    PE(lambda e: e.transpose(ps[:, 0:8], c8[:], g.ident_f[0:8, 0:8]), [c8, g.ident_f], [ps])
    A(lambda e: e.activation(scT[:], ps[:, 0:8], AF.Silu), [ps], [scT])
    wv = g.w_ada.t.rearrange("(k p) n -> p k n", p=128)
    for jg in range(12):
        wb_ = wst[jg % 2]
        kb.dma("sp", wb_[:], wv[:, :, jg * 512:(jg + 1) * 512], [g.w_ada], [wb_], wb_)
        for k in range(8):
            PE(lambda e, k=k, wb_=wb_: e.matmul(ps2[0:1, :], scT[:, k:k + 1], wb_[:, k, :],
                                                  start=(k == 0), stop=(k == 7)), [scT, wb_], [ps2])
        V(lambda e, jg=jg: e.tensor_tensor(modrow[:, jg * 512:(jg + 1) * 512], ps2[0:1, :],
                                           brow[:, jg * 512:(jg + 1) * 512], ALU.add), [ps2, brow], [modrow])
    for j in range(48):
        PE(lambda e, j=j: e.matmul(ps[:, 16 + j:17 + j], modrow[0:1, j * 128:(j + 1) * 128], g.ones_f[0:1, 0:1],
                                   start=True, stop=True), [modrow, g.ones_f], [ps])
    V(lambda e: e.tensor_copy(g.modc[:], ps[:, 16:64]), [ps], [g.modc])
    for j in range(16):
        PE(lambda e, j=j: e.matmul(ps[:, 64 + j:65 + j], grow[0:1, j // 8, (j % 8) * 128:(j % 8 + 1) * 128],
                                   g.ones_f[0:1, 0:1], start=True, stop=True), [grow, g.ones_f], [ps])
    V(lambda e: e.tensor_copy(gcol[:], ps[:, 64:80]), [ps], [gcol])
    V(lambda e: e.scalar_tensor_tensor(g.sc1p[:], g.modc[:, 8:16], 1.0, gcol[:, 0:8], ALU.add, ALU.mult),
      [g.modc, gcol], [g.sc1p])
    V(lambda e: e.scalar_tensor_tensor(g.sc2p[:], g.modc[:, 32:40], 1.0, gcol[:, 8:16], ALU.add, ALU.mult),
      [g.modc, gcol], [g.sc2p])
    for i, c0 in enumerate((16 * 128, 40 * 128)):
        for hh in range(2):
            PE(lambda e, c0=c0, hh=hh: e.matmul(ps2[:, :], g.ones_f[0:1, :], modrow[0:1, c0 + hh * 512:c0 + (hh + 1) * 512],
                                                start=True, stop=True), [g.ones_f, modrow], [ps2])
            A(lambda e, i=i, hh=hh: e.copy(g.gt_bc[:, i, hh * 512:(hh + 1) * 512], ps2[:, :]), [ps2], [g.gt_bc])
    kb.end()


def phase_a(kb, g, ntiles=NTILE):
    kb.begin()
    V = lambda fn, r, w: kb.op("dve", fn, r, w)
    A = lambda fn, r, w: kb.op("act", fn, r, w)
    PE = lambda fn, r, w: kb.op("pe", fn, r, w)
    PL = lambda fn, r, w: kb.op("pool", fn, r, w)
    Wb = kb.sbuf("Wb", [128, 8, 5376], BF16)
    Wm = kb.sbuf("Wm", [128, 8, 1792], BF16)
    kb.begin()
    mu_bc = kb.sbuf("mu_bc", [128, 1792], F32)
    omu_bc = kb.sbuf("omu_bc", [128, 1792], F32)
    wst = [kb.sbuf(f"awst{i}", [128, 8, 256], F32) for i in range(2)]
    kb.dma("sp", mu_bc[:], g.rwkv_mu.t.partition_broadcast(128), [g.rwkv_mu], [mu_bc], mu_bc)
    V(lambda e: e.tensor_scalar(omu_bc[:], mu_bc[:], -1.0, 1.0, ALU.mult, ALU.add), [mu_bc], [omu_bc])
    wv = g.w_in.t.rearrange("(k p) n -> p k n", p=128)
    engs = ["dve", "act", "pool"]
    for pc in range(21):
        c0 = pc * 256
        st = wst[pc % 2]
        kb.dma("sp", st[:], wv[:, :, c0:c0 + 256], [g.w_in], [st], st)
        if 1536 <= c0 < 3328:
            m0 = c0 - 1536
            V(lambda e, st=st, c0=c0, m0=m0: e.tensor_tensor(Wb[:, :, c0:c0 + 256], st[:],
              bc(omu_bc[:, m0:m0 + 256].unsqueeze(1), [128, 8, 256]), ALU.mult), [st, omu_bc], [Wb])
            PL(lambda e, st=st, m0=m0: e.tensor_tensor(Wm[:, :, m0:m0 + 256], st[:],
               bc(mu_bc[:, m0:m0 + 256].unsqueeze(1), [128, 8, 256]), ALU.mult), [st, mu_bc], [Wm])
        else:
            en = engs[pc % 2]
            if en == "act":
                A(lambda e, st=st, c0=c0: e.copy(Wb[:, :, c0:c0 + 256], st[:]), [st], [Wb])
            else:
                V(lambda e, st=st, c0=c0: e.tensor_copy(Wb[:, :, c0:c0 + 256], st[:]), [st], [Wb])
    kb.end()
    def rowbc(name, src, n):
        t = kb.sbuf(name, [128, n], F32)
        kb.dma("sp", t[:], src.t.partition_broadcast(128), [src], [t], t)
        return t
    qg = rowbc("qg", g.q_norm_g, 64)
    kg = rowbc("kg", g.k_norm_g, 64)
    w0b = rowbc("w0b", g.rwkv_w0, 512)
    a0b = rowbc("a0b", g.rwkv_a0, 512)
    kkb = rowbc("kkb", g.rwkv_k_k, 512)
    kab = rowbc("kab", g.rwkv_k_a, 512)
    invf = kb.sbuf("invf_t", [128, 8], F32)
    kb.dma("sp", invf[:], g.invf[:], [g.invf], [invf], invf)
    w2a2 = kb.sbuf("w2a2", [128, 512], F32)
    g2t = kb.sbuf("g2t", [128, 512], F32)
    kb.dma("sp", w2a2[0:64, :], g.rwkv_w2[:], [g.rwkv_w2], [w2a2], w2a2)
    kb.dma("sp", w2a2[64:128, :], g.rwkv_a2[:], [g.rwkv_a2], [w2a2], w2a2)
    kb.dma("sp", g2t[:], g.rwkv_g2[:], [g.rwkv_g2], [g2t], g2t)
    onesc = kb.sbuf("onesc", [128, 1], F32)
    PL(lambda e: e.memset(onesc[:], 1.0 / 256.0), [], [onesc])

    xt = [kb.sbuf(f"xt{i}", [128, 1024], F32) for i in range(2)]
    junk = kb.sbuf("junk", [128, 1024], BF16)
    xnT = [kb.sbuf(f"xnT{i}", [128, 8, 129], BF16) for i in range(2)]
    stt = [kb.sbuf(f"stt{i}", [128, 4], F32) for i in range(2)]
    posi = [kb.sbuf(f"posi{i}", [128, 1], I32) for i in range(2)]
    cs = [kb.sbuf(f"cs{i}", [128, 5, 16], F32) for i in range(2)]
    csi = [kb.sbuf(f"csi{i}", [128, 16], I32) for i in range(2)]
    psT = [kb.psum(f"psT{i}", [128, 512], F32) for i in range(2)]
    psM = [kb.psum(f"psM{i}", [128, 512], F32) for i in range(4)]
    psB = kb.psum("psB", [128, 1024], BF16)
    psX = kb.psum("psX", [128, 512], F32)
    NB = 3
    t1 = [kb.sbuf(f"t1_{i}", [128, 512], F32) for i in range(NB)]
    t2 = [kb.sbuf(f"t2_{i}", [128, 512], F32) for i in range(NB)]
    ssq = [kb.sbuf(f"ssq{i}", [128, 16], F32) for i in range(NB)]
    rp = [kb.sbuf(f"rp{i}", [128, 4, 8, 8], F32) for i in range(NB)]
    qf = [kb.sbuf(f"qf{i}", [128, 512], BF16) for i in range(NB)]
    qTs = [kb.sbuf(f"qTs{i}", [128, 4, 128], BF16) for i in range(NB)]
    kmp = [kb.sbuf(f"kmp{i}", [128, 4], F32) for i in range(2)]
    la = [kb.sbuf(f"la{i}", [128, 256], F32) for i in range(2)]
    laT = [kb.sbuf(f"laT{i}", [128, 256], F32) for i in range(2)]
    av = [kb.sbuf(f"av{i}", [128, 512], F32) for i in range(2)]
    sto = [kb.sbuf(f"sto{i}", [128, 512], F32) for i in range(8)]
    gsb = [kb.sbuf(f"gsb{i}", [128, 512], BF16) for i in range(3)]
    cnt = {"m": 0, "b": 0, "s": 0, "g": 0}

    def nextM():
        cnt["m"] += 1
        return psM[cnt["m"] % 4]

    def nextS():
        cnt["s"] += 1
        return sto[cnt["s"] % 8]

    def mm(ps_, ncols, col0, xn, prev_c0=None):
        for k in range(8):
            PE(lambda e, k=k: e.matmul(ps_[:, 0:ncols], xn[:, k, 1:129], Wb[:, k, col0:col0 + ncols],
                                       start=(k == 0), stop=(k == 7 and prev_c0 is None)), [xn, Wb], [ps_])
        if prev_c0 is not None:
            for k in range(8):
                PE(lambda e, k=k: e.matmul(ps_[:, 0:ncols], xn[:, k, 0:128], Wm[:, k, prev_c0:prev_c0 + ncols],
                                           start=False, stop=(k == 7)), [xn, Wm], [ps_])

    def stream_store(si, src, t0):
        dst = g.RS.t[si, :, t0:t0 + 128, :].rearrange("h t n -> t h n")
        kb.dma("sp", dst, src[:].rearrange("p (h n) -> p h n", h=8), [src], [g.RS], src)

    def tile_body(it):
        own = it >= OWN0
        t0 = it * 128
        x_ = xt[it % 2]
        xn = xnT[it % 2]
        xnp = xnT[(it + 1) % 2]
        st_ = stt[it % 2]
        pi = posi[it % 2]

        def ld_in(j):
            kb.dma("sp", xt[j % 2][:], g.xs[j * 128:(j + 1) * 128, :], [g.xs], [xt[j % 2]], xt[j % 2])
            kb.dma("sp", posi[j % 2][:], g.pos[j * 128:(j + 1) * 128, :], [g.pos], [posi[j % 2]], posi[j % 2])
        if it == 0:
            ld_in(0)
        if it + 1 < ntiles:
            ld_in(it + 1)
        A(lambda e, x_=x_, st_=st_: e.activation(junk[:], x_[:], AF.Square, accum_out=st_[:, 0:1]), [x_], [junk, st_])
        V(lambda e, st_=st_: e.tensor_scalar(st_[:, 1:2], st_[:, 0:1], 1.0 / 1024.0, 1e-6, ALU.mult, ALU.add), [st_], [st_])
        A(lambda e, st_=st_: e.activation(st_[:, 2:3], st_[:, 1:2], AF.Sqrt), [st_], [st_])
        V(lambda e, st_=st_: e.reciprocal(st_[:, 2:3], st_[:, 2:3]), [st_], [st_])
        V(lambda e, x_=x_, st_=st_: e.tensor_scalar_mul(x_[:], x_[:], st_[:, 2:3]), [x_, st_], [x_])
        for k in range(8):
            pt = psT[k // 4]
            PE(lambda e, k=k, pt=pt, x_=x_: e.transpose(pt[:, (k % 4) * 128:(k % 4 + 1) * 128], x_[:, k * 128:(k + 1) * 128],
                                                        g.ident_f[:]), [x_, g.ident_f], [pt])
            A(lambda e, k=k, pt=pt, xn=xn: e.activation(xn[:, k, 1:129], pt[:, (k % 4) * 128:(k % 4 + 1) * 128], AF.Identity,
                                                       bias=g.modc[:, k:k + 1], scale=g.sc1p[:, k:k + 1]),
              [pt, g.modc, g.sc1p], [xn])
        if it == 0:
            V(lambda e, xn=xn: e.memset(xn[:, :, 0:1], 0.0), [], [xn])
        elif it == OWN0:
            V(lambda e, xn=xn, xnp=xnp: e.tensor_scalar_mul(xn[:, :, 0:1], xnp[:, :, 128:129], g.pv[:, 0:1]), [xnp, g.pv], [xn])
        else:
            V(lambda e, xn=xn, xnp=xnp: e.tensor_copy(xn[:, :, 0:1], xnp[:, :, 128:129]), [xnp], [xn])
        c_ = cs[it % 2]
        ci_ = csi[it % 2]
        PI = float(np.pi)
        V(lambda e: e.tensor_copy(c_[:, 1, 0:1], pi[:]), [pi], [c_])
        V(lambda e: e.tensor_scalar_mul(c_[:, 0, 0:8], invf[:], c_[:, 1, 0:1]), [invf, c_], [c_])
        V(lambda e: e.tensor_scalar_add(c_[:, 0, 8:16], c_[:, 0, 0:8], 0.5 * PI), [c_], [c_])
        V(lambda e: e.tensor_scalar_mul(c_[:, 1, :], c_[:, 0, :], 1.0 / (2 * PI)), [c_], [c_])
        V(lambda e: e.tensor_copy(ci_[:], c_[:, 1, :]), [c_], [ci_])
        V(lambda e: e.tensor_copy(c_[:, 1, :], ci_[:]), [ci_], [c_])
        V(lambda e: e.scalar_tensor_tensor(c_[:, 2, :], c_[:, 1, :], -2 * PI, c_[:, 0, :], ALU.mult, ALU.add), [c_], [c_])
        V(lambda e: e.tensor_single_scalar(c_[:, 1, :], c_[:, 2, :], PI, ALU.is_gt), [c_], [c_])
        V(lambda e: e.scalar_tensor_tensor(c_[:, 3, :], c_[:, 1, :], -2 * PI, c_[:, 2, :], ALU.mult, ALU.add), [c_], [c_])
        V(lambda e: e.tensor_single_scalar(c_[:, 1, :], c_[:, 3, :], -PI, ALU.is_lt), [c_], [c_])
        V(lambda e: e.scalar_tensor_tensor(c_[:, 2, :], c_[:, 1, :], 2 * PI, c_[:, 3, :], ALU.mult, ALU.add), [c_], [c_])
        A(lambda e: e.activation(c_[:, 4, :], c_[:, 2, :], AF.Sin), [c_], [c_])

        def qk_post(ps_, gb, is_q):
            i = cnt["b"] % NB
            cnt["b"] += 1
            a1, a2, sq_, rp_, qf_, qT_ = t1[i], t2[i], ssq[i], rp[i], qf[i], qTs[i]
            A(lambda e: e.activation(a1[:], ps_[:], AF.Square), [ps_], [a1])
            V(lambda e: e.tensor_reduce(sq_[:, 0:8], a1[:].rearrange("p (h d) -> p h d", h=8), AX.X, ALU.add), [a1], [sq_])
            V(lambda e: e.tensor_scalar(sq_[:, 0:8], sq_[:, 0:8], 1.0 / 64.0, 1e-6, ALU.mult, ALU.add), [sq_], [sq_])
            A(lambda e: e.activation(sq_[:, 8:16], sq_[:, 0:8], AF.Sqrt), [sq_], [sq_])
            V(lambda e: e.reciprocal(sq_[:, 8:16], sq_[:, 8:16]), [sq_], [sq_])
            V(lambda e: e.tensor_tensor(a2[:].rearrange("p (h d) -> p h d", h=8), ps_[:].rearrange("p (h d) -> p h d", h=8),
                                        bc(sq_[:, 8:16].unsqueeze(2), [128, 8, 64]), ALU.mult), [ps_, sq_], [a2])
            V(lambda e: e.tensor_tensor(a2[:].rearrange("p (h d) -> p h d", h=8), a2[:].rearrange("p (h d) -> p h d", h=8),
                                        bc(gb[:].unsqueeze(1), [128, 8, 64]), ALU.mult), [a2, gb], [a2])
            v3 = a2[:].rearrange("p (h d) -> p h d", h=8)
            x1, x2 = v3[:, :, 0:8], v3[:, :, 8:16]
            sinb = bc(c_[:, 4, 0:8].unsqueeze(1), [128, 8, 8])
            cosb = bc(c_[:, 4, 8:16].unsqueeze(1), [128, 8, 8])
            V(lambda e: e.tensor_tensor(rp_[:, 0], x1, cosb, ALU.mult), [a2, c_], [rp_])
            V(lambda e: e.tensor_tensor(rp_[:, 1], x2, sinb, ALU.mult), [a2, c_], [rp_])
            V(lambda e: e.tensor_tensor(rp_[:, 2], x2, cosb, ALU.mult), [a2, c_], [rp_])
            V(lambda e: e.tensor_tensor(rp_[:, 3], x1, sinb, ALU.mult), [a2, c_], [rp_])
            V(lambda e: e.tensor_tensor(x1, rp_[:, 0], rp_[:, 1], ALU.subtract), [rp_], [a2])
            V(lambda e: e.tensor_tensor(x2, rp_[:, 2], rp_[:, 3], ALU.add), [rp_], [a2])
            A(lambda e: e.copy(qf_[:], a2[:]), [a2], [qf_])
            for pr in range(4):
                PE(lambda e, pr=pr: e.transpose(psB[:, pr * 128:(pr + 1) * 128], qf_[:, pr * 128:(pr + 1) * 128], g.ident_b[:]),
                   [qf_, g.ident_b], [psB])
            V(lambda e: e.tensor_copy(qT_[:].rearrange("p c t -> p (c t)"), psB[:, 0:512]), [psB], [qT_])
            if is_q:
                to = t0 - OWN0 * 128
                kb.dma("sp", g.QT.t[:, :, to:to + 128].rearrange("c p t -> p c t"), qT_[:], [qT_], [g.QT], qT_)
            else:
                kb.dma("sp", g.KT.t[:, :, t0:t0 + 128].rearrange("c p t -> p c t"), qT_[:], [qT_], [g.KT], qT_)
                km_ = kmp[it % 2]
                for pr in range(4):
                    PE(lambda e, pr=pr: e.matmul(psX[:, 256 + pr:257 + pr], a2[:, pr * 128:(pr + 1) * 128], onesc[:],
                                                 start=True, stop=True), [a2, onesc], [psX])
                V(lambda e: e.tensor_copy(km_[:], psX[:, 256:260]), [psX], [km_])
                if it % 2 == 1:
                    V(lambda e: e.tensor_tensor(g.kmT[:, :, it // 2], kmp[0][:], kmp[1][:], ALU.add), [kmp[0], kmp[1]], [g.kmT])

        psl = nextM()
        mm(psl, 256, 3072, xn, prev_c0=1536)
        la_ = la[it % 2]
        laT_ = laT[it % 2]
        A(lambda e: e.activation(la_[:, 0:64], psl[:, 0:64], AF.Tanh), [psl], [la_])
        V(lambda e: e.tensor_copy(la_[:, 64:128], psl[:, 64:128]), [psl], [la_])
        A(lambda e: e.activation(la_[:, 128:256], psl[:, 128:256], AF.Sigmoid), [psl], [la_])
        PE(lambda e: e.transpose(psX[:, 0:128], la_[:, 0:128], g.ident_f[:]), [la_, g.ident_f], [psX])
        PE(lambda e: e.transpose(psX[:, 128:256], la_[:, 128:256], g.ident_f[:]), [la_, g.ident_f], [psX])
        V(lambda e: e.tensor_copy(laT_[:], psX[:, 0:256]), [psX], [laT_])
        pw = nextM()
        PE(lambda e: e.matmul(pw[:], laT_[0:64, 0:128], w2a2[0:64, :], start=True, stop=True), [laT_, w2a2], [pw])
        ld_ = nextS()
        V(lambda e: e.tensor_tensor(ld_[:], pw[:], w0b[:], ALU.add), [pw, w0b], [ld_])
        A(lambda e: e.activation(ld_[:], ld_[:], AF.Sigmoid), [ld_], [ld_])
        V(lambda e: e.tensor_scalar_mul(ld_[:], ld_[:], -0.6065306597126334), [ld_], [ld_])
        stream_store(1, ld_, t0)
        pa = nextM()
        PE(lambda e: e.matmul(pa[:], laT_[64:128, 0:128], w2a2[64:128, :], start=True, stop=True), [laT_, w2a2], [pa])
        a_ = av[it % 2]
        V(lambda e: e.tensor_tensor(a_[:], pa[:], a0b[:], ALU.add), [pa, a0b], [a_])
        A(lambda e: e.activation(a_[:], a_[:], AF.Sigmoid), [a_], [a_])
        if own:
            pg = nextM()
            PE(lambda e: e.matmul(pg[:], laT_[:, 128:256], g2t[:], start=True, stop=True), [laT_, g2t], [pg])
            gg = nextS()
            A(lambda e: e.copy(gg[:], pg[:]), [pg], [gg])
            to = t0 - OWN0 * 128
            kb.dma("sp", g.GR[to:to + 128, :], gg[:], [gg], [g.GR], gg)
        pr_ = nextM()
        mm(pr_, 512, 1536, xn, prev_c0=0)
        r_ = nextS()
        A(lambda e: e.copy(r_[:], pr_[:]), [pr_], [r_])
        stream_store(0, r_, t0)
        pk = nextM()
        mm(pk, 512, 2048, xn, prev_c0=512)
        i = cnt["b"] % NB
        cnt["b"] += 1
        a1, sq_ = t1[i], ssq[i]
        kkn = nextS()
        V(lambda e: e.tensor_tensor(kkn[:], pk[:], kkb[:], ALU.mult), [pk, kkb], [kkn])
        V(lambda e: e.tensor_tensor(a1[:], kkn[:], kkn[:], ALU.mult), [kkn], [a1])
        V(lambda e: e.tensor_reduce(sq_[:, 0:8], a1[:].rearrange("p (h d) -> p h d", h=8), AX.X, ALU.add), [a1], [sq_])
        V(lambda e: e.tensor_scalar_add(sq_[:, 0:8], sq_[:, 0:8], 1e-24), [sq_], [sq_])
        A(lambda e: e.activation(sq_[:, 8:16], sq_[:, 0:8], AF.Sqrt), [sq_], [sq_])
        V(lambda e: e.reciprocal(sq_[:, 8:16], sq_[:, 8:16]), [sq_], [sq_])
        V(lambda e: e.scalar_tensor_tensor(kkn[:].rearrange("p (h d) -> p h d", h=8), kkn[:].rearrange("p (h d) -> p h d", h=8), -1.0,
                                           bc(sq_[:, 8:16].unsqueeze(2), [128, 8, 64]), ALU.mult, ALU.mult), [kkn, sq_], [kkn])
        stream_store(4, kkn, t0)
        b_ = nextS()
        V(lambda e: e.scalar_tensor_tensor(b_[:], kkn[:], -1.0, a_[:], ALU.mult, ALU.mult), [kkn, a_], [b_])
        stream_store(5, b_, t0)
        k_ = nextS()
        V(lambda e: e.scalar_tensor_tensor(a1[:], a_[:], -1.0, kab[:], ALU.add, ALU.mult), [a_, kab], [a1])
        V(lambda e: e.scalar_tensor_tensor(k_[:], a1[:], 1.0, pk[:], ALU.add, ALU.mult), [a1, pk], [k_])
        stream_store(2, k_, t0)
        pv_ = nextM()
        mm(pv_, 512, 2560, xn, prev_c0=1024)
        v_ = nextS()
        if own:
            A(lambda e: e.copy(v_[:], pv_[:]), [pv_], [v_])
        else:
            V(lambda e: e.tensor_scalar_mul(v_[:], pv_[:], g.pv[:, 0:1]), [pv_, g.pv], [v_])
        stream_store(3, v_, t0)
        pmk = nextM()
        mm(pmk, 512, 512, xn)
        qk_post(pmk, kg, False)
        pmv = nextM()
        mm(pmv, 512, 1024, xn)
        vb = gsb[cnt["g"] % 3]
        cnt["g"] += 1
        A(lambda e: e.copy(vb[:], pmv[:]), [pmv], [vb])
        kb.dma("sp", g.VM[t0:t0 + 128, :], vb[:], [vb], [g.VM], vb)
        if own:
            pmq = nextM()
            mm(pmq, 512, 0, xn)
            qk_post(pmq, qg, True)
            to = t0 - OWN0 * 128
            for gi in range(4):
                pg_ = nextM()
                mm(pg_, 512, 3328 + gi * 512, xn)
                gb_ = gsb[cnt["g"] % 3]
                cnt["g"] += 1
                A(lambda e, gb_=gb_, pg_=pg_: e.activation(gb_[:], pg_[:], AF.Sigmoid), [pg_], [gb_])
                kb.dma("sp", g.GATES[to:to + 128, gi * 512:(gi + 1) * 512], gb_[:], [gb_], [g.GATES], gb_)
    for it in range(ntiles):
        tile_body(it)
    kb.end()


def phase_b(kb, g, nqb=16):
    kb.begin()
    V = lambda fn, r, w: kb.op("dve", fn, r, w)
    A = lambda fn, r, w: kb.op("act", fn, r, w)
    PE = lambda fn, r, w: kb.op("pe", fn, r, w)
    PL = lambda fn, r, w: kb.op("pool", fn, r, w)
    NKB = NT // 256
    QB0 = OWN0 // 2
    kmTb = kb.sbuf("kmTb", [128, 4, NKB], BF16)
    V(lambda e: e.tensor_copy(kmTb[:], g.kmT[:]), [g.kmT], [kmTb])
    pastm = kb.sbuf("pastm", [128, 16, NKB], F32)
    pfx = kb.sbuf("pfx", [128, 1], F32)
    PL(lambda e: e.memset(pastm[:], 0.0), [], [pastm])
    for qbl in range(16):
        PL(lambda e, qbl=qbl: e.memset(pastm[:, qbl, QB0 + qbl:NKB], -1e30), [], [pastm])
    V(lambda e: e.tensor_scalar(pfx[:], g.pv[:], -1.0, 1e30, ALU.add, ALU.mult), [g.pv], [pfx])
    V(lambda e: e.tensor_scalar(pastm[:, :, 0:QB0], pastm[:, :, 0:QB0], pfx[:, 0:1], None, ALU.add), [pastm, pfx], [pastm])
    tri = kb.sbuf("tri", [128, 2, 256], BF16)
    PL(lambda e: e.memset(tri[:], 1.0), [], [tri])
    for kc in range(2):
        PL(lambda e, kc=kc: e.affine_select(out=tri[:, kc, :], in_=tri[:, kc, :], pattern=[[1, 256]], compare_op=ALU.is_ge,
                                            fill=0.0, base=-kc * 128, channel_multiplier=-1), [tri], [tri])
    KTs = [kb.sbuf(f"KTs{i}", [128, NT], BF16) for i in range(2)]
    QTs = [kb.sbuf(f"QTs{i}", [128, NOWN], BF16) for i in range(2)]
    Vs = [kb.sbuf(f"Vs{i}", [128, NT // 128, 2, 65], BF16) for i in range(2)]
    for i in range(2):
        PL(lambda e, i=i: e.memset(Vs[i][:, :, :, 64:65], 1.0), [], [Vs[i]])
    sel = [kb.sbuf(f"sel{i}", [128, 32, 2, NKB], F32) for i in range(2)]
    gsm = [kb.sbuf(f"gsm{i}", [128, NKB], F32) for i in range(2)]
    g8 = [kb.sbuf(f"g8{i}", [128, 8], F32) for i in range(2)]
    m1 = [kb.sbuf(f"m1{i}", [128, NKB], F32) for i in range(2)]
    psS = [kb.psum(f"psS{i}", [128, 512], F32) for i in range(3)]
    psO = [kb.psum(f"psO{i}", [128, 2, 65], F32) for i in range(3)]
    psG = [kb.psum(f"psG{i}", [128, 64], F32) for i in range(2)]
    pts = [kb.sbuf(f"pts{i}", [128, 2, 256], BF16) for i in range(4)]
    acc = [kb.sbuf(f"acc{i}", [128, 2, 65], F32) for i in range(2)]
    rc = [kb.sbuf(f"rc{i}", [128, 2], F32) for i in range(2)]
    ob = [kb.sbuf(f"ob{i}", [128, 2, 64], BF16) for i in range(4)]
    cn = {"s": 0, "o": 0, "p": 0, "a": 0, "b": 0, "g": 0}
    vview = g.VM.t.rearrange("(c p) (h d) -> p c h d", p=128, d=64)

    def pair_body(pr):
        KT_, QT_, V_, sel_ = KTs[pr % 2], QTs[pr % 2], Vs[pr % 2], sel[pr % 2]
        kb.dma("sp", KT_[:], g.KT.t[pr], [g.KT], [KT_], KT_)
        kb.dma("sp", QT_[:], g.QT.t[pr], [g.QT], [QT_], QT_)
        for cq in range(4):
            c0 = cq * (NT // 512)
            c1 = c0 + NT // 512
            for h2 in range(2):
                kb.dma("sp", V_[:, c0:c1, h2, 0:64], vview[:, c0:c1, 2 * pr + h2, :], [g.VM], [V_], V_)
        for qt in range(2 * nqb):
            qbl = qt // 2
            for h2 in range(2):
                def selbody(qt=qt, qbl=qbl, h2=h2):
                    i = cn["g"] % 2
                    cn["g"] += 1
                    pg, gs_, g8_, m1_ = psG[i], gsm[i], g8[i], m1[i]
                    rows = slice(h2 * 64, (h2 + 1) * 64)
                    PE(lambda e: e.matmul(pg[:, 0:NKB], QT_[rows, qt * 128:(qt + 1) * 128], kmTb[rows, pr, :], start=True, stop=True),
                       [QT_, kmTb], [pg])
                    V(lambda e: e.tensor_tensor(gs_[:], pg[:, 0:NKB], pastm[:, qbl, :], ALU.add), [pg, pastm], [gs_])
                    V(lambda e: e.max(out=g8_[:], in_=gs_[:]), [gs_], [g8_])
                    V(lambda e: e.tensor_scalar(m1_[:], gs_[:], g8_[:, 2:3], None, ALU.is_ge), [gs_, g8_], [m1_])
                    V(lambda e: e.scalar_tensor_tensor(sel_[:, qt, h2, :], gs_[:], -1e29, m1_[:], ALU.is_gt, ALU.mult), [gs_, m1_], [sel_])
                    V(lambda e: e.memset(sel_[:, qt, h2, QB0 + qbl:QB0 + qbl + 1], 1.0), [], [sel_])
                selbody()
        triples = [(h2, qbl, kblk) for h2 in range(2) for qbl in range(nqb) for kblk in range(QB0 + qbl + 1)]
        state = {}

        def stage1(tr):
            h2, qbl, kblk = tr
            rows = slice(h2 * 64, (h2 + 1) * 64)
            qb = QB0 + qbl
            pS = psS[cn["s"] % 3]
            cn["s"] += 1
            pt = pts[cn["p"] % 4]
            cn["p"] += 1
            for kc in range(2):
                c = kblk * 2 + kc
                PE(lambda e, kc=kc, c=c: e.matmul(pS[:, kc * 256:(kc + 1) * 256], KT_[rows, c * 128:(c + 1) * 128],
                                                  QT_[rows, qbl * 256:(qbl + 1) * 256], start=True, stop=True),
                   [KT_, QT_], [pS])
            A(lambda e: e.activation(pt[:].rearrange("p a b -> p (a b)"), pS[:], AF.Exp, scale=0.125), [pS], [pt])
            if kblk == qb:
                V(lambda e: e.tensor_tensor(pt[:], pt[:], tri[:], ALU.mult), [pt, tri], [pt])
            state[tr] = pt

        def stage2(tr):
            h2, qbl, kblk = tr
            qb = QB0 + qbl
            pt = state.pop(tr)
            if kblk == 0:
                cn["a"] += 1
            acc_ = acc[cn["a"] % 2]
            rc_ = rc[cn["a"] % 2]
            pO = psO[cn["o"] % 3]
            cn["o"] += 1
            for qt in range(2):
                for kc in range(2):
                    c = kblk * 2 + kc
                    PE(lambda e, qt=qt, kc=kc, c=c: e.matmul(pO[:, qt, :], pt[:, kc, qt * 128:(qt + 1) * 128], V_[:, c, h2, :],
                                                             start=(kc == 0), stop=(kc == 1)), [pt, V_], [pO])
            for qt in range(2):
                sc = sel_[:, qbl * 2 + qt, h2, kblk:kblk + 1]
                if kblk == 0:
                    V(lambda e, qt=qt, sc=sc: e.tensor_scalar_mul(acc_[:, qt, :], pO[:, qt, :], sc), [pO, sel_], [acc_])
                else:
                    V(lambda e, qt=qt, sc=sc: e.scalar_tensor_tensor(acc_[:, qt, :], pO[:, qt, :], sc, acc_[:, qt, :],
                                                                     ALU.mult, ALU.add), [pO, sel_, acc_], [acc_])
            if kblk == qb:
                ob_ = ob[cn["b"] % 4]
                cn["b"] += 1
                V(lambda e: e.reciprocal(rc_[:], acc_[:, :, 64]), [acc_], [rc_])
                for qt in range(2):
                    V(lambda e, qt=qt: e.tensor_scalar_mul(ob_[:, qt, :], acc_[:, qt, 0:64], rc_[:, qt:qt + 1]), [acc_, rc_], [ob_])
                hh = pr * 2 + h2
                dst = g.OM.t[qbl * 256:(qbl + 1) * 256, hh * 64:(hh + 1) * 64].rearrange("(a p) d -> p a d", p=128)
                kb.dma("sp", dst, ob_[:], [ob_], [g.OM], ob_)

        stage1(triples[0])
        for i, tr in enumerate(triples):
            if i + 1 < len(triples):
                stage1(triples[i + 1])
            stage2(tr)

    for pr in range(4):
        pair_body(pr)
    kb.end()


def phase_c(kb, g, nchunks=NT // 64):
    kb.begin()
    V = lambda fn, r, w: kb.op("dve", fn, r, w)
    A = lambda fn, r, w: kb.op("act", fn, r, w)
    PE = lambda fn, r, w: kb.op("pe", fn, r, w)
    PL = lambda fn, r, w: kb.op("pool", fn, r, w)
    I_ = g.ident_f
    Lbd = kb.sbuf("Lbd", [128, 128], F32)
    Msu = kb.sbuf("Msu", [128, 128], F32)
    Msl = kb.sbuf("Msl", [128, 128], F32)
    Obd = kb.sbuf("Obd", [128, 128], F32)
    ind2 = kb.sbuf("ind2", [128, 2], F32)
    for t_, op_ in ((Lbd, ALU.is_ge), (Msu, ALU.is_gt)):
        PL(lambda e, t_=t_: e.memset(t_[:], 1.0), [], [t_])
        PL(lambda e, t_=t_, op_=op_: e.affine_select(out=t_[:], in_=t_[:], pattern=[[1, 128]], compare_op=op_, fill=0.0,
                                                     base=0, channel_multiplier=-1), [t_], [t_])
        PL(lambda e, t_=t_: e.memset(t_[0:64, 64:128], 0.0), [], [t_])
    PL(lambda e: e.memset(Msl[:], 1.0), [], [Msl])
    PL(lambda e: e.affine_select(out=Msl[:], in_=Msl[:], pattern=[[-1, 128]], compare_op=ALU.is_gt, fill=0.0,
                                 base=0, channel_multiplier=1), [Msl], [Msl])
    PL(lambda e: e.memset(Msl[64:128, 0:64], 0.0), [], [Msl])
    PL(lambda e: e.memset(Obd[:], 0.0), [], [Obd])
    PL(lambda e: e.memset(Obd[0:64, 0:64], 1.0), [], [Obd])
    PL(lambda e: e.memset(Obd[64:128, 64:128], 1.0), [], [Obd])
    PL(lambda e: e.memset(ind2[:], 0.0), [], [ind2])
    PL(lambda e: e.memset(ind2[0:64, 0:1], 1.0), [], [ind2])
    PL(lambda e: e.memset(ind2[64:128, 1:2], 1.0), [], [ind2])
    cst = kb.sbuf("cst", [128, 3, 4, 64], F32)
    for hp in range(4):
        for h2 in range(2):
            hh = hp * 2 + h2
            for j, src in enumerate((g.rwkv_ln_g, g.rwkv_ln_b, g.rwkv_r_k)):
                kb.dma("sp", cst[h2 * 64:(h2 + 1) * 64, j, hp, :], src.t[:, hh * 64:(hh + 1) * 64].partition_broadcast(64),
                       [src], [cst], cst)
    ldb = [kb.sbuf(f"cld{i}", [128, 4, 6, 64], F32) for i in range(2)]
    gtb = [kb.sbuf(f"cgt{i}", [128, 4, 64], F32) for i in range(2)]
    E = kb.sbuf("cE", [128, 4, 4, 64], F32)
    X = kb.sbuf("cX", [128, 4, 4, 64], F32)
    TA = kb.sbuf("cTA", [128, 4, 4, 64], F32)
    BK = kb.sbuf("cBK", [128, 4, 2, 64], F32)
    Vbd = kb.sbuf("cVbd", [128, 4, 128], F32)
    Ubd = kb.sbuf("cUbd", [128, 4, 128], F32)
    TT = kb.sbuf("cTT", [64, 4, 4, 128], F32)
    AA = [kb.sbuf(f"cAA{i}", [128, 4, 2, 128], BF16) for i in range(2)]
    AXm = kb.sbuf("cAX", [128, 4, 3, 128], F32)
    Y = [kb.sbuf(f"cY{i}", [128, 4, 128], BF16) for i in range(2)]
    Yf = kb.sbuf("cYf", [128, 4, 128], F32)
    WT = kb.sbuf("cWT", [64, 4, 128], F32)
    PC = kb.sbuf("cPC", [64, 4, 2], F32)
    hs = [kb.sbuf(f"ch{i}", [64, 4, 128], F32) for i in range(2)]
    htmp = kb.sbuf("chtmp", [64, 4, 128], F32)
    O = kb.sbuf("cO", [128, 4, 64], F32)
    stt = kb.sbuf("cst2", [128, 8, 4], F32)
    t1 = kb.sbuf("ct1", [128, 4, 64], F32)
    t2 = kb.sbuf("ct2", [128, 4, 64], F32)
    obb = [kb.sbuf(f"cob{i}", [128, 4, 64], BF16) for i in range(2)]
    psA = kb.psum("cps", [128, 4, 2, 512], F32)

    class PB:
        def __init__(self, b):
            self.buf = Buf(None, f"cpsbank{b}")
            self.b = b

        def s(self, hp, sl, rows=slice(0, 128)):
            return psA.t[rows, hp, self.b, sl]

        def all(self, sl, rows=slice(0, 128)):
            return psA.t[rows, :, self.b, sl]
    P0, P1 = PB(0), PB(1)
    PL(lambda e: e.memset(Vbd[:], 0.0), [], [Vbd])
    PL(lambda e: e.memset(Ubd[:], 0.0), [], [Ubd])
    PL(lambda e: e.memset(hs[0][:], 0.0), [], [hs[0]])
    b4 = lambda m: bc(m[:].unsqueeze(1), [128, 4, 128])
    orw4 = g.ORW.t.rearrange("t (hp h2 n) -> t hp h2 n", hp=4, h2=2)
    gr4 = g.GR.t.rearrange("t (hp h2 n) -> t hp h2 n", hp=4, h2=2)

    def chunk(c):
        own = c >= (NT - NOWN) // 64
        ld = ldb[c % 2]
        gt = gtb[c % 2]
        hcur, hnew = hs[c % 2], hs[(c + 1) % 2]
        to = c * 64 - (NT - NOWN)
        def ld_in(j):
            ldj, gtj = ldb[j % 2], gtb[j % 2]
            for hp in range(4):
                for h2 in range(2):
                    src = g.RS.t[:, hp * 2 + h2, j * 64:(j + 1) * 64, :].rearrange("s t n -> t s n")
                    kb.dma("sp", ldj[h2 * 64:(h2 + 1) * 64, hp, :, :], src, [g.RS], [ldj], ldj)
            if j >= (NT - NOWN) // 64:
                tj = j * 64 - (NT - NOWN)
                for h2 in range(2):
                    kb.dma("sp", gtj[h2 * 64:(h2 + 1) * 64, :, :], gr4[tj:tj + 64, :, h2, :], [g.GR], [gtj], gtj)
        if c == 0:
            ld_in(0)
        if c + 1 < nchunks:
            ld_in(c + 1)
        for hp in range(4):
            PE(lambda e, hp=hp: e.matmul(P0.s(hp, slice(0, 64)), Lbd[:], ld[:, hp, 1, :], start=True, stop=True), [Lbd, ld], [P0.buf])
            PE(lambda e, hp=hp: e.matmul(P0.s(hp, slice(64, 128)), Obd[:], ld[:, hp, 1, :], start=True, stop=True), [Obd, ld], [P0.buf])
            PE(lambda e, hp=hp: e.matmul(P0.s(hp, slice(128, 130), slice(0, 64)), ld[:, hp, 1, :], ind2[:], start=True, stop=True),
               [ld, ind2], [P0.buf])
        V(lambda e: e.tensor_copy(E[:, :, 0, :], P0.all(slice(0, 64))), [P0.buf], [E])
        V(lambda e: e.tensor_scalar_mul(E[:, :, 1, :], P0.all(slice(0, 64)), -1.0), [P0.buf], [E])
        V(lambda e: e.tensor_tensor(E[:, :, 2, :], P0.all(slice(0, 64)), ld[:, :, 1, :], ALU.subtract), [P0.buf, ld], [E])
        V(lambda e: e.tensor_tensor(E[:, :, 3, :], P0.all(slice(64, 128)), E[:, :, 0, :], ALU.subtract), [P0.buf, E], [E])
        A(lambda e: e.activation(X[:], E[:], AF.Exp), [E], [X])
        A(lambda e: e.activation(PC[:], P0.all(slice(128, 130), slice(0, 64)), AF.Exp), [P0.buf], [PC])
        V(lambda e: e.tensor_tensor(TA[:, :, 0, :], ld[:, :, 4, :], X[:, :, 2, :], ALU.mult), [ld, X], [TA])
        V(lambda e: e.tensor_tensor(TA[:, :, 1, :], ld[:, :, 0, :], X[:, :, 0, :], ALU.mult), [ld, X], [TA])
        V(lambda e: e.tensor_tensor(TA[:, :, 2, :], ld[:, :, 5, :], X[:, :, 1, :], ALU.mult), [ld, X], [TA])
        V(lambda e: e.tensor_tensor(TA[:, :, 3, :], ld[:, :, 2, :], X[:, :, 1, :], ALU.mult), [ld, X], [TA])
        PL(lambda e: e.tensor_tensor(BK[:, :, 0, :], ld[:, :, 5, :], X[:, :, 3, :], ALU.mult), [ld, X], [BK])
        PL(lambda e: e.tensor_tensor(BK[:, :, 1, :], ld[:, :, 2, :], X[:, :, 3, :], ALU.mult), [ld, X], [BK])
        PL(lambda e: e.tensor_copy(Vbd[0:64, :, 0:64], ld[0:64, :, 3, :]), [ld], [Vbd])
        PL(lambda e: e.tensor_copy(Vbd[64:128, :, 64:128], ld[64:128, :, 3, :]), [ld], [Vbd])
        for hp in range(4):
            for j in range(4):
                PE(lambda e, hp=hp, j=j: e.transpose(P1.s(hp, slice(j * 128, (j + 1) * 128), slice(0, 64)), TA[:, hp, j, :], I_[:]),
                   [TA, I_], [P1.buf])
        A(lambda e: e.copy(TT[:].rearrange("p a j t -> p a (j t)"), P1.all(slice(0, 512), slice(0, 64))), [P1.buf], [TT])
        for hp in range(4):
            AtT, RtT, BtT, KtT = TT[:, hp, 0, :], TT[:, hp, 1, :], TT[:, hp, 2, :], TT[:, hp, 3, :]
            PE(lambda e, hp=hp, a=AtT, b=BtT: e.matmul(P0.s(hp, slice(0, 128)), a, b, start=True, stop=True), [TT], [P0.buf])
            PE(lambda e, hp=hp, a=BtT, b=AtT: e.matmul(P0.s(hp, slice(128, 256)), a, b, start=True, stop=True), [TT], [P0.buf])
            PE(lambda e, hp=hp, a=KtT, b=AtT: e.matmul(P0.s(hp, slice(256, 384)), a, b, start=True, stop=True), [TT], [P0.buf])
            PE(lambda e, hp=hp, a=BtT, b=RtT: e.matmul(P0.s(hp, slice(384, 512)), a, b, start=True, stop=True), [TT], [P0.buf])
            PE(lambda e, hp=hp, a=KtT, b=RtT: e.matmul(P1.s(hp, slice(0, 128)), a, b, start=True, stop=True), [TT], [P1.buf])
        V(lambda e: e.tensor_tensor(AA[0][:, :, 0, :], P0.all(slice(0, 128)), b4(Msl), ALU.mult), [P0.buf, Msl], [AA[0]])
        V(lambda e: e.tensor_tensor(AA[0][:, :, 1, :], P0.all(slice(128, 256)), b4(Msu), ALU.mult), [P0.buf, Msu], [AA[0]])
        V(lambda e: e.tensor_tensor(AXm[:, :, 0, :], P0.all(slice(256, 384)), b4(Msu), ALU.mult), [P0.buf, Msu], [AXm])
        V(lambda e: e.tensor_tensor(AXm[:, :, 1, :], P0.all(slice(384, 512)), b4(Lbd), ALU.mult), [P0.buf, Lbd], [AXm])
        V(lambda e: e.tensor_tensor(AXm[:, :, 2, :], P1.all(slice(0, 128)), b4(Lbd), ALU.mult), [P1.buf, Lbd], [AXm])
        for hp in range(4):
            PE(lambda e, hp=hp: e.matmul(P1.s(hp, slice(128, 192)), AXm[:, hp, 0, :], ld[:, hp, 3, :], start=True, stop=True), [AXm, ld], [P1.buf])
        A(lambda e: e.copy(Y[0][:, :, 0:64], TA[:, :, 0, :]), [TA], [Y[0]])
        A(lambda e: e.copy(Y[0][:, :, 64:128], P1.all(slice(128, 192))), [P1.buf], [Y[0]])
        for lev in range(6):
            a, b = lev % 2, (lev + 1) % 2
            pp = P0 if lev % 2 == 0 else P1
            for hp in range(4):
                PE(lambda e, hp=hp, a=a, pp=pp: e.matmul(pp.s(hp, slice(0, 128)), AA[a][:, hp, 1, :], Y[a][:, hp, :], start=True, stop=True),
                   [AA[a], Y[a]], [pp.buf])
            if lev < 5:
                V(lambda e, a=a, b=b, pp=pp: e.tensor_tensor(Y[b][:], pp.all(slice(0, 128)), Y[a][:], ALU.add), [pp.buf, Y[a]], [Y[b]])
            else:
                V(lambda e, a=a, pp=pp: e.tensor_tensor(Yf[:], pp.all(slice(0, 128)), Y[a][:], ALU.add), [pp.buf, Y[a]], [Yf])
            if lev < 5:
                for hp in range(4):
                    PE(lambda e, hp=hp, a=a, pp=pp: e.matmul(pp.s(hp, slice(128, 256)), AA[a][:, hp, 1, :], AA[a][:, hp, 0, :], start=True, stop=True),
                       [AA[a]], [pp.buf])
                    PE(lambda e, hp=hp, a=a, pp=pp: e.matmul(pp.s(hp, slice(256, 384)), AA[a][:, hp, 0, :], AA[a][:, hp, 1, :], start=True, stop=True),
                       [AA[a]], [pp.buf])
                A(lambda e, b=b, pp=pp: e.copy(AA[b][:].rearrange("p h a t -> p h (a t)"), pp.all(slice(128, 384))), [pp.buf], [AA[b]])
        Xf = Yf
        for hp in range(4):
            PE(lambda e, hp=hp: e.transpose(P0.s(hp, slice(0, 128), slice(0, 64)), Xf[:, hp, 0:64], I_[:]), [Xf, I_], [P0.buf])
        A(lambda e: e.copy(WT[:], P0.all(slice(0, 128), slice(0, 64))), [P0.buf], [WT])
        for hp in range(4):
            PE(lambda e, hp=hp: e.matmul(P0.s(hp, slice(128, 256)), WT[:, hp, :], hcur[:, hp, :], start=True, stop=True), [WT, hcur], [P0.buf])
        V(lambda e: e.tensor_tensor(Ubd[0:64, :, 0:64], P0.all(slice(128, 192), slice(0, 64)), Xf[0:64, :, 64:128], ALU.add), [P0.buf, Xf], [Ubd])
        V(lambda e: e.tensor_tensor(Ubd[64:128, :, 64:128], P0.all(slice(192, 256), slice(64, 128)), Xf[64:128, :, 64:128], ALU.add),
          [P0.buf, Xf], [Ubd])
        if own:
            for hp in range(4):
                PE(lambda e, hp=hp: e.matmul(P1.s(hp, slice(0, 128)), TT[:, hp, 1, :], hcur[:, hp, :], start=True, stop=False), [TT, hcur], [P1.buf])
                PE(lambda e, hp=hp: e.matmul(P1.s(hp, slice(0, 128)), AXm[:, hp, 1, :], Ubd[:, hp, :], start=False, stop=False), [AXm, Ubd], [P1.buf])
                PE(lambda e, hp=hp: e.matmul(P1.s(hp, slice(0, 128)), AXm[:, hp, 2, :], Vbd[:, hp, :], start=False, stop=True), [AXm, Vbd], [P1.buf])
            A(lambda e: e.copy(O[0:64, :, :], P1.all(slice(0, 64), slice(0, 64))), [P1.buf], [O])
            A(lambda e: e.copy(O[64:128, :, :], P1.all(slice(64, 128), slice(64, 128))), [P1.buf], [O])
        for hp in range(4):
            PE(lambda e, hp=hp: e.matmul(P0.s(hp, slice(256, 384), slice(0, 64)), BK[:, hp, 0, :], Ubd[:, hp, :], start=True, stop=False),
               [BK, Ubd], [P0.buf])
            PE(lambda e, hp=hp: e.matmul(P0.s(hp, slice(256, 384), slice(0, 64)), BK[:, hp, 1, :], Vbd[:, hp, :], start=False, stop=True),
               [BK, Vbd], [P0.buf])
        V(lambda e: e.tensor_tensor(htmp[:].rearrange("p h (a v) -> p h a v", a=2), hcur[:].rearrange("p h (a v) -> p h a v", a=2),
                                    bc(PC[:].unsqueeze(3), [64, 4, 2, 64]), ALU.mult), [hcur, PC], [htmp])
        V(lambda e: e.tensor_tensor(hnew[:], htmp[:], P0.all(slice(256, 384), slice(0, 64)), ALU.add), [htmp, P0.buf], [hnew])
        if not own:
            return
        ob = obb[c % 2]
        b64 = lambda ap: bc(ap.unsqueeze(2), [128, 4, 64])
        V(lambda e: e.tensor_reduce(stt[:, 0, :], O[:], AX.X, ALU.add), [O], [stt])
        V(lambda e: e.tensor_scalar_mul(stt[:, 1, :], stt[:, 0, :], 1.0 / 64.0), [stt], [stt])
        V(lambda e: e.tensor_tensor(t1[:], O[:], b64(stt[:, 1, :]), ALU.subtract), [O, stt], [t1])
        A(lambda e: e.activation(t2[:], t1[:], AF.Square), [t1], [t2])
        V(lambda e: e.tensor_reduce(stt[:, 2, :], t2[:], AX.X, ALU.add), [t2], [stt])
        V(lambda e: e.tensor_scalar(stt[:, 3, :], stt[:, 2, :], 1.0 / 64.0, GN_EPS_, ALU.mult, ALU.add), [stt], [stt])
        A(lambda e: e.activation(stt[:, 4, :], stt[:, 3, :], AF.Sqrt), [stt], [stt])
        V(lambda e: e.reciprocal(stt[:, 4, :], stt[:, 4, :]), [stt], [stt])
        V(lambda e: e.tensor_tensor(t1[:], t1[:], b64(stt[:, 4, :]), ALU.mult), [t1, stt], [t1])
        V(lambda e: e.tensor_tensor(t1[:], t1[:], cst[:, 0], ALU.mult), [t1, cst], [t1])
        V(lambda e: e.tensor_tensor(t1[:], t1[:], cst[:, 1], ALU.add), [t1, cst], [t1])
        PL(lambda e: e.tensor_tensor(t2[:], ld[:, :, 0, :], ld[:, :, 2, :], ALU.mult), [ld], [t2])
        PL(lambda e: e.tensor_tensor(t2[:], t2[:], cst[:, 2], ALU.mult), [t2, cst], [t2])
        V(lambda e: e.tensor_reduce(stt[:, 5, :], t2[:], AX.X, ALU.add), [t2], [stt])
        V(lambda e: e.tensor_tensor(t2[:], ld[:, :, 3, :], b64(stt[:, 5, :]), ALU.mult), [ld, stt], [t2])
        V(lambda e: e.tensor_tensor(t1[:], t1[:], t2[:], ALU.add), [t1, t2], [t1])
        V(lambda e: e.tensor_tensor(ob[:], t1[:], gt[:], ALU.mult), [t1, gt], [ob])
        for h2 in range(2):
            kb.dma("sp", orw4[to:to + 64, :, h2, :], ob[h2 * 64:(h2 + 1) * 64, :, :], [ob], [g.ORW], ob)

    for c in range(nchunks):
        chunk(c)
    kb.end()


GN_EPS_ = 64e-5


def phase_p(kb, g, nblk=128):
    kb.begin()
    V = lambda fn, r, w: kb.op("dve", fn, r, w)
    A = lambda fn, r, w: kb.op("act", fn, r, w)
    PE = lambda fn, r, w: kb.op("pe", fn, r, w)
    PL = lambda fn, r, w: kb.op("pool", fn, r, w)
    uf = [kb.sbuf(f"uf{i}", [128, 1024], F32) for i in range(3)]
    ub = [kb.sbuf(f"ub{i}", [128, 1024], BF16) for i in range(3)]
    ut = [kb.sbuf(f"ut{i}", [128, 8, 128], BF16) for i in range(3)]
    vf = [kb.sbuf(f"vf{i}", [128, 1024], F32) for i in range(3)]
    vb = [kb.sbuf(f"vb{i}", [128, 1024], BF16) for i in range(3)]
    pb = [kb.psum(f"ppb{i}", [128, 1024], BF16) for i in range(3)]

    def body(b):
        i = b % 3
        kb.dma("sp", uf[i][:], g.peer_u[b * 128:(b + 1) * 128, :], [g.peer_u], [uf[i]], uf[i])
        kb.dma("sp", vf[i][:], g.peer_v[b * 128:(b + 1) * 128, :], [g.peer_v], [vf[i]], vf[i])
        V(lambda e: e.tensor_copy(ub[i][:], uf[i][:]), [uf[i]], [ub[i]])
        PL(lambda e: e.tensor_copy(vb[i][:], vf[i][:]), [vf[i]], [vb[i]])
        kb.dma("sp", g.VB[b * 128:(b + 1) * 128, :], vb[i][:], [vb[i]], [g.VB], vb[i])
        for k in range(8):
            PE(lambda e, k=k: e.transpose(pb[i][:, k * 128:(k + 1) * 128], ub[i][:, k * 128:(k + 1) * 128], g.ident_b[:]),
               [ub[i], g.ident_b], [pb[i]])
        A(lambda e: e.copy(ut[i][:].rearrange("p k e -> p (k e)"), pb[i][:]), [pb[i]], [ut[i]])
        kb.dma("sp", g.UT.t[:, :, b * 128:(b + 1) * 128].rearrange("k p e -> p k e"), ut[i][:], [ut[i]], [g.UT], ut[i])
    for b in range(nblk):
        body(b)
    kb.end()


def phase_d(kb, g, ntl=NOWN // 128):
    kb.begin()
    V = lambda fn, r, w: kb.op("dve", fn, r, w)
    A = lambda fn, r, w: kb.op("act", fn, r, w)
    PE = lambda fn, r, w: kb.op("pe", fn, r, w)
    PL = lambda fn, r, w: kb.op("pool", fn, r, w)
    Wpm = kb.sbuf("Wpm", [128, 4, 1024], BF16)
    Wpr = kb.sbuf("Wpr", [128, 4, 1024], BF16)
    Wo = kb.sbuf("Wo", [128, 8, 1024], BF16)
    Wq = kb.sbuf("Wq", [128, 8, 2048], BF16)
    skT = kb.sbuf("skT", [128, 16, 128], BF16)
    kb.begin()
    stg = [kb.sbuf(f"dstg{i}", [128, 4, 1024], F32) for i in range(2)]
    psk = kb.psum("psk", [128, 512], F32)
    n = [0]

    def ldw(dst, src, k0, nk, c0, nc_):
        s_ = stg[n[0] % 2]
        n[0] += 1
        kb.dma("sp", s_[:, 0:nk, 0:nc_], src.t.rearrange("(k p) n -> p k n", p=128)[:, k0:k0 + nk, c0:c0 + nc_], [src], [s_], s_)
        if n[0] % 2:
            V(lambda e: e.tensor_copy(dst[:, k0:k0 + nk, c0:c0 + nc_], s_[:, 0:nk, 0:nc_]), [s_], [dst])
        else:
            A(lambda e: e.copy(dst[:, k0:k0 + nk, c0:c0 + nc_], s_[:, 0:nk, 0:nc_]), [s_], [dst])
    ldw(Wpm, g.w_proj_moba, 0, 4, 0, 1024)
    ldw(Wpr, g.w_proj_rwkv, 0, 4, 0, 1024)
    ldw(Wo, g.w_out, 0, 4, 0, 1024)
    ldw(Wo, g.w_out, 4, 4, 0, 1024)
    for k0 in (0, 4):
        for c0 in (0, 1024):
            ldw(Wq, g.peer_wq, k0, 4, c0, 1024)
    for hp in range(16):
        s_ = stg[n[0] % 2]
        n[0] += 1
        kb.dma("sp", s_[:, 0, 0:128], g.peer_sk[hp], [g.peer_sk], [s_], s_)
        PE(lambda e, s_=s_: e.transpose(psk[:, 0:128], s_[:, 0, 0:128], g.ident_f[:]), [s_, g.ident_f], [psk])
        V(lambda e, hp=hp: e.tensor_copy(skT[:, hp, :], psk[:, 0:128]), [psk], [skT])
    kb.end()

    NB = 2
    om = [kb.sbuf(f"om{i}", [128, 2, 512], BF16) for i in range(NB)]
    gts = [kb.sbuf(f"gts{i}", [128, 2048], BF16) for i in range(NB)]
    xo = [kb.sbuf(f"xo{i}", [128, 1024], F32) for i in range(NB)]
    oT = [kb.sbuf(f"oT{i}", [128, 8, 128], BF16) for i in range(NB)]
    m1 = [kb.sbuf(f"dm1{i}", [128, 1024], F32) for i in range(NB)]
    mix = [kb.sbuf(f"mix{i}", [128, 1024], BF16) for i in range(NB)]
    mixT = [kb.sbuf(f"mixT{i}", [128, 8, 128], BF16) for i in range(NB)]
    h1 = [kb.sbuf(f"h1{i}", [128, 1024], F32) for i in range(NB)]
    junk = kb.sbuf("djunk", [128, 1024], BF16)
    stt = [kb.sbuf(f"dstt{i}", [128, 4], F32) for i in range(NB)]
    xn2T = [kb.sbuf(f"xn2T{i}", [128, 8, 128], BF16) for i in range(NB)]
    qT = [kb.sbuf(f"qT{i}", [128, 16, 128], BF16) for i in range(NB)]
    S = [kb.sbuf(f"S{i}", [128, 16, 128], F32) for i in range(NB)]
    S2 = [kb.sbuf(f"S2{i}", [128, 128], F32) for i in range(NB)]
    top = [kb.sbuf(f"top{i}", [128, 16, 16], F32) for i in range(NB)]
    cand = [kb.sbuf(f"cand{i}", [128, 8, 256], F32) for i in range(NB)]
    c2 = [kb.sbuf(f"c2{i}", [128, 256], F32) for i in range(NB)]
    t16 = [kb.sbuf(f"t16{i}", [128, 8, 16], F32) for i in range(NB)]
    thr = [kb.sbuf(f"thr{i}", [128, 4, 8], F32) for i in range(NB)]
    pB = [kb.psum(f"dpB{i}", [128, 1024], BF16) for i in range(2)]
    pM = [kb.psum(f"dpM{i}", [128, 512], F32) for i in range(4)]
    pT = [kb.psum(f"dpT{i}", [128, 512], F32) for i in range(2)]
    cn = {"m": 0}

    def nM():
        cn["m"] += 1
        return pM[cn["m"] % 4]

    def body(it):
        i = it % NB
        t0 = it * 128
        om_, g_, x_, oT_, m1_, mix_, mixT_, h1_, st_, xn_, qT_, S_, S2_, top_, cand_, c2_, t16_, thr_ = (
            om[i], gts[i], xo[i], oT[i], m1[i], mix[i], mixT[i], h1[i], stt[i], xn2T[i], qT[i], S[i], S2[i], top[i], cand[i],
            c2[i], t16[i], thr[i])
        def ld_in(j):
            a0 = j * 128
            kb.dma("sp", om[j % NB][:, 0, :], g.OM[a0:a0 + 128, :], [g.OM], [om[j % NB]], om[j % NB])
            kb.dma("sp", om[j % NB][:, 1, :], g.ORW[a0:a0 + 128, :], [g.ORW], [om[j % NB]], om[j % NB])
            kb.dma("sp", gts[j % NB][:], g.GATES[a0:a0 + 128, :], [g.GATES], [gts[j % NB]], gts[j % NB])
            kb.dma("sp", xo[j % NB][:], g.xs[NT - NOWN + a0:NT - NOWN + a0 + 128, :], [g.xs], [xo[j % NB]], xo[j % NB])
        if it == 0:
            ld_in(0)
        if it + 1 < ntl:
            ld_in(it + 1)
        pb = pB[it % 2]
        for j in range(8):
            PE(lambda e, j=j: e.transpose(pb[:, j * 128:(j + 1) * 128], om_[:, j // 4, (j % 4) * 128:(j % 4 + 1) * 128], g.ident_b[:]),
               [om_, g.ident_b], [pb])
        A(lambda e: e.copy(oT_[:].rearrange("p k t -> p (k t)"), pb[:]), [pb], [oT_])
        for br, W_ in ((0, Wpm), (1, Wpr)):
            for hf in range(2):
                p_ = nM()
                for k in range(4):
                    PE(lambda e, k=k, p_=p_, W_=W_, br=br, hf=hf: e.matmul(p_[:], oT_[:, br * 4 + k, :], W_[:, k, hf * 512:(hf + 1) * 512],
                                                                           start=(k == 0), stop=(k == 3)), [oT_, W_], [p_])
                cs_ = slice(hf * 512, (hf + 1) * 512)
                gs_ = slice(br * 1024 + hf * 512, br * 1024 + (hf + 1) * 512)
                if br == 0:
                    V(lambda e, p_=p_, cs_=cs_, gs_=gs_: e.tensor_tensor(m1_[:, cs_], p_[:], g_[:, gs_], ALU.mult), [p_, g_], [m1_])
                else:
                    V(lambda e, p_=p_, cs_=cs_, gs_=gs_: e.tensor_tensor(h1_[:, cs_], p_[:], g_[:, gs_], ALU.mult), [p_, g_], [h1_])
                    PL(lambda e, cs_=cs_: e.tensor_tensor(mix_[:, cs_], m1_[:, cs_], h1_[:, cs_], ALU.add), [m1_, h1_], [mix_])
        pb2 = pB[(it + 1) % 2]
        for j in range(8):
            PE(lambda e, j=j: e.transpose(pb2[:, j * 128:(j + 1) * 128], mix_[:, j * 128:(j + 1) * 128], g.ident_b[:]),
               [mix_, g.ident_b], [pb2])
        A(lambda e: e.copy(mixT_[:].rearrange("p k t -> p (k t)"), pb2[:]), [pb2], [mixT_])
        for hf in range(2):
            p_ = nM()
            for k in range(8):
                PE(lambda e, k=k, p_=p_, hf=hf: e.matmul(p_[:], mixT_[:, k, :], Wo[:, k, hf * 512:(hf + 1) * 512],
                                                         start=(k == 0), stop=(k == 7)), [mixT_, Wo], [p_])
            cs_ = slice(hf * 512, (hf + 1) * 512)
            V(lambda e, p_=p_, cs_=cs_: e.tensor_tensor(h1_[:, cs_], p_[:], g.gt_bc[:, 0, cs_], ALU.mult), [p_, g.gt_bc], [h1_])
            V(lambda e, cs_=cs_: e.tensor_tensor(h1_[:, cs_], h1_[:, cs_], x_[:, cs_], ALU.add), [h1_, x_], [h1_])
        kb.dma("sp", g.H1[t0:t0 + 128, :], h1_[:], [h1_], [g.H1], h1_)
        A(lambda e: e.activation(junk[:], h1_[:], AF.Square, accum_out=st_[:, 0:1]), [h1_], [junk, st_])
        V(lambda e: e.tensor_scalar(st_[:, 1:2], st_[:, 0:1], 1.0 / 1024.0, 1e-6, ALU.mult, ALU.add), [st_], [st_])
        A(lambda e: e.activation(st_[:, 2:3], st_[:, 1:2], AF.Sqrt), [st_], [st_])
        V(lambda e: e.reciprocal(st_[:, 2:3], st_[:, 2:3]), [st_], [st_])
        V(lambda e: e.tensor_scalar_mul(m1_[:], h1_[:], st_[:, 2:3]), [h1_, st_], [m1_])
        for k in range(8):
            pt = pT[k // 4]
            PE(lambda e, k=k, pt=pt: e.transpose(pt[:, (k % 4) * 128:(k % 4 + 1) * 128], m1_[:, k * 128:(k + 1) * 128], g.ident_f[:]),
               [m1_, g.ident_f], [pt])
            A(lambda e, k=k, pt=pt: e.activation(xn_[:, k, :], pt[:, (k % 4) * 128:(k % 4 + 1) * 128], AF.Identity,
                                                 bias=g.modc[:, 24 + k:25 + k], scale=g.sc2p[:, k:k + 1]), [pt, g.modc, g.sc2p], [xn_])
        kb.dma("sp", g.XN2T.t[:, :, t0:t0 + 128].rearrange("k p t -> p k t"), xn_[:], [xn_], [g.XN2T], xn_)
        for q4 in range(4):
            p_ = nM()
            for jj in range(4):
                hp = q4 * 4 + jj
                for k in range(8):
                    PE(lambda e, k=k, p_=p_, hp=hp, jj=jj: e.matmul(p_[:, jj * 128:(jj + 1) * 128], Wq[:, k, hp * 128:(hp + 1) * 128], xn_[:, k, :],
                                                                  start=(k == 0), stop=(k == 7)), [Wq, xn_], [p_])
            A(lambda e, p_=p_, q4=q4: e.copy(qT_[:, q4 * 4:(q4 + 1) * 4, :].rearrange("p a t -> p (a t)"), p_[:]), [p_], [qT_])
        for q4 in range(4):
            p_ = nM()
            for jj in range(4):
                hp = q4 * 4 + jj
                PE(lambda e, p_=p_, hp=hp, jj=jj: e.matmul(p_[:, jj * 128:(jj + 1) * 128], qT_[:, hp, :], skT[:, hp, :], start=True, stop=True),
                   [qT_, skT], [p_])
            V(lambda e, p_=p_, q4=q4: e.tensor_copy(S_[:, q4 * 4:(q4 + 1) * 4, :].rearrange("p a n -> p (a n)"), p_[:]), [p_], [S_])
        kb.dma("sp", g.SC[t0:t0 + 128, :, :], S_[:], [S_], [g.SC], S_)
        for hp in range(16):
            V(lambda e, hp=hp: e.max(out=top_[:, hp, 0:8], in_=S_[:, hp, :]), [S_], [top_])
            V(lambda e, hp=hp: e.match_replace(out=S2_[:], in_to_replace=top_[:, hp, 0:8], in_values=S_[:, hp, :], imm_value=-1e30),
              [S_, top_], [S2_])
            V(lambda e, hp=hp: e.max(out=top_[:, hp, 8:16], in_=S2_[:]), [S2_], [top_])
        t4 = top_[:].rearrange("p (h two) a -> p h two a", two=2)
        V(lambda e: e.tensor_tensor(cand_[:].rearrange("p h (a b) -> p h a b", b=16), bc(t4[:, :, 0, :].unsqueeze(3), [128, 8, 16, 16]),
                                    bc(t4[:, :, 1, :].unsqueeze(2), [128, 8, 16, 16]), ALU.add), [top_], [cand_])
        for h in range(8):
            V(lambda e, h=h: e.max(out=t16_[:, h, 0:8], in_=cand_[:, h, :]), [cand_], [t16_])
            V(lambda e, h=h: e.match_replace(out=c2_[:], in_to_replace=t16_[:, h, 0:8], in_values=cand_[:, h, :], imm_value=-1e30),
              [cand_, t16_], [c2_])
            V(lambda e, h=h: e.max(out=t16_[:, h, 8:16], in_=c2_[:]), [c2_], [t16_])
        V(lambda e: e.tensor_copy(thr_[:, 0, :], t16_[:, :, 15]), [t16_], [thr_])
        V(lambda e: e.tensor_scalar_mul(thr_[:, 1, :], t16_[:, :, 0], -1.0), [t16_], [thr_])
        V(lambda e: e.tensor_tensor(t16_[:], t16_[:], bc(thr_[:, 1, :].unsqueeze(2), [128, 8, 16]), ALU.add), [t16_, thr_], [t16_])
        A(lambda e: e.activation(t16_[:], t16_[:], AF.Exp), [t16_], [t16_])
        V(lambda e: e.tensor_reduce(thr_[:, 3, :], t16_[:], AX.X, ALU.add), [t16_], [thr_])
        V(lambda e: e.reciprocal(thr_[:, 2, :], thr_[:, 3, :]), [thr_], [thr_])
        kb.dma("sp", g.TH[t0:t0 + 128, :, :], thr_[:], [thr_], [g.TH], thr_)
    for it in range(ntl):
        body(it)
    kb.end()


def phase_e(kb, g, ngroups=NOWN // 256, nec=16):
    kb.begin()
    V = lambda fn, r, w: kb.op("dve", fn, r, w)
    A = lambda fn, r, w: kb.op("act", fn, r, w)
    PE = lambda fn, r, w: kb.op("pe", fn, r, w)
    PL = lambda fn, r, w: kb.op("pool", fn, r, w)
    UTs = [kb.sbuf(f"UTs{i}", [128, 8, 1024], BF16) for i in range(2)]
    VBs = [kb.sbuf(f"VBs{i}", [128, 8, 1024], BF16) for i in range(2)]
    xn = [kb.sbuf(f"exn{i}", [128, 8, 256], BF16) for i in range(2)]
    Ssb = [kb.sbuf(f"eS{i}", [128, 2, 16, 128], F32) for i in range(1)]
    th = [kb.sbuf(f"eth{i}", [128, 2, 4, 8], F32) for i in range(2)]
    Dg = [kb.sbuf(f"eDg{i}", [128, 2, 8, 128], BF16) for i in range(2)]
    Zt = [kb.sbuf(f"eZ{i}", [128, 1024], F32) for i in range(3)]
    Et = [kb.sbuf(f"eE{i}", [128, 1024], BF16) for i in range(3)]
    Mt = [[[kb.sbuf(f"eM{b}_{tt}_{h}", [128, 1024], BF16) for h in range(8)] for tt in range(2)] for b in range(2)]
    gl = [kb.sbuf(f"egl{i}", [128, 256], BF16) for i in range(3)]
    hd = [kb.sbuf(f"ehd{i}", [128, 256], BF16) for i in range(3)]
    h1t = [kb.sbuf(f"eh1{i}", [128, 2, 1024], F32) for i in range(1)]
    fo = [kb.sbuf(f"efo{i}", [128, 1024], F32) for i in range(2)]
    pO = [kb.psum(f"epO{i}", [128, 512], F32) for i in range(4)]
    pS = [kb.psum(f"epS{i}", [128, 512], F32) for i in range(2)]
    pG = [kb.psum(f"epG{i}", [128, 512], F32) for i in range(2)]
    cn = {"z": 0, "s": 0, "g": 0, "h": 0}
    utv = g.UT.t.rearrange("k p e -> p k e")
    vbv = g.VB.t.rearrange("(b p) d -> p b d", p=128)

    class G_:
        pass

    def setup_group(tg):
        c = G_()
        i = tg % 2
        c.t0 = tg * 256
        c.xn, c.S, c.th, c.Dg, c.h1 = xn[i], Ssb[0], th[i], Dg[i], h1t[0]
        t0 = c.t0
        kb.dma("sp", c.xn[:], g.XN2T.t[:, :, t0:t0 + 256].rearrange("k p t -> p k t"), [g.XN2T], [c.xn], c.xn)
        for tt in range(2):
            kb.dma("sp", c.S[:, tt], g.SC[t0 + tt * 128:t0 + (tt + 1) * 128, :, :], [g.SC], [c.S], c.S)
            kb.dma("sp", c.th[:, tt], g.TH[t0 + tt * 128:t0 + (tt + 1) * 128, :, :], [g.TH], [c.th], c.th)
            for h in range(8):
                V(lambda e, tt=tt, h=h: e.tensor_scalar_mul(c.Dg[:, tt, h, :], g.ident_f[:], c.th[:, tt, 2, h:h + 1]), [g.ident_f, c.th], [c.Dg])
        return c

    def gate_tasks(c, k, ec):
        w = k % 2
        U_, Vb_ = UTs[w], VBs[w]
        tasks = []

        def loadw():
            kb.dma("sp", U_[:], utv[:, :, ec * 1024:(ec + 1) * 1024], [g.UT], [U_], U_)
            kb.dma("sp", Vb_[:], vbv[:, ec * 8:(ec + 1) * 8, :], [g.VB], [Vb_], Vb_)
        for tt in range(2):
            for h in range(8):
                def gate(tt=tt, h=h, first=(tt == 0 and h == 0)):
                    if first:
                        loadw()
                    z = cn["z"] % 3
                    cn["z"] += 1
                    Z_, E_, M_ = Zt[z], Et[z], Mt[w][tt][h]
                    (V if h == 7 else PL)(lambda e: e.tensor_tensor(Z_[:].rearrange("p (a b) -> p a b", b=128),
                                                 bc(c.S[:, tt, 2 * h, ec * 8:(ec + 1) * 8].unsqueeze(2), [128, 8, 128]),
                                                 bc(c.S[:, tt, 2 * h + 1, :].unsqueeze(1), [128, 8, 128]), ALU.add), [c.S], [Z_])
                    A(lambda e: e.activation(E_[:], Z_[:], AF.Exp, bias=c.th[:, tt, 1, h:h + 1]), [Z_, c.th], [E_])
                    V(lambda e: e.scalar_tensor_tensor(M_[:], Z_[:], c.th[:, tt, 0, h:h + 1], E_[:], ALU.is_ge, ALU.mult),
                      [Z_, c.th, E_], [M_])
                tasks.append(gate)
        return tasks

    def blk_tasks(c, k, ec):
        w = k % 2
        U_, Vb_ = UTs[w], VBs[w]
        tasks = []
        for ib in range(8):
            def blk(ib=ib):
                eb = ec * 8 + ib
                ps_ = pS[cn["s"] % 2]
                pg_ = pG[cn["g"] % 2]
                cn["s"] += 1
                cn["g"] += 1
                for kk in range(8):
                    PE(lambda e, kk=kk: e.matmul(ps_[:, 0:256], U_[:, kk, ib * 128:(ib + 1) * 128], c.xn[:, kk, :], start=(kk == 0), stop=(kk == 7)),
                       [U_, c.xn], [ps_])
                for tt in range(2):
                    for h in range(8):
                        M_ = Mt[w][tt][h]
                        PE(lambda e, tt=tt, h=h, M_=M_: e.matmul(pg_[:, tt * 128:(tt + 1) * 128], M_[:, ib * 128:(ib + 1) * 128], c.Dg[:, tt, h, :],
                                                               start=(h == 0), stop=(h == 7)), [M_, c.Dg], [pg_])
                j = cn["h"] % 3
                cn["h"] += 1
                A(lambda e: e.activation(gl[j][:], ps_[:, 0:256], AF.Gelu), [ps_], [gl[j]])
                V(lambda e: e.tensor_tensor(hd[j][:], gl[j][:], pg_[:, 0:256], ALU.mult), [gl[j], pg_], [hd[j]])
                for tt in range(2):
                    for hf in range(2):
                        PE(lambda e, tt=tt, hf=hf: e.matmul(pO[tt * 2 + hf][:], hd[j][:, tt * 128:(tt + 1) * 128], Vb_[:, ib, hf * 512:(hf + 1) * 512],
                                                          start=(eb == 0), stop=(eb == nec * 8 - 1)), [hd[j], Vb_], [pO[tt * 2 + hf]])
            tasks.append(blk)
        return tasks

    def finalize(c):
        t0 = c.t0
        for tt in range(2):
            kb.dma("sp", c.h1[:, tt, :], g.H1[t0 + tt * 128:t0 + (tt + 1) * 128, :], [g.H1], [c.h1], c.h1)
        for tt in range(2):
            f_ = fo[tt]
            for hf in range(2):
                cs_ = slice(hf * 512, (hf + 1) * 512)
                V(lambda e, tt=tt, hf=hf, cs_=cs_, f_=f_: e.tensor_tensor(f_[:, cs_], pO[tt * 2 + hf][:], g.gt_bc[:, 1, cs_], ALU.mult),
                  [pO[tt * 2 + hf], g.gt_bc], [f_])
                V(lambda e, tt=tt, cs_=cs_, f_=f_: e.tensor_tensor(f_[:, cs_], f_[:, cs_], c.h1[:, tt, cs_], ALU.add), [f_, c.h1], [f_])
            kb.dma("sp", g.out[t0 + tt * 128:t0 + (tt + 1) * 128, :], f_[:], [f_], [g.out], f_)

    chunks = [(tg, ec) for tg in range(ngroups) for ec in range(nec)]
    ctxs = {}

    def ctx_of(tg):
        if tg not in ctxs:
            ctxs[tg] = setup_group(tg)
        return ctxs[tg]
    tg0, ec0 = chunks[0]
    for t in gate_tasks(ctx_of(tg0), 0, ec0):
        t()
    for k, (tg, ec) in enumerate(chunks):
        c = ctx_of(tg)
        nxt = None
        if k + 1 < len(chunks):
            tgn, ecn = chunks[k + 1]
            nxt = gate_tasks(ctx_of(tgn), k + 1, ecn)
        bt = blk_tasks(c, k, ec)
        for ib in range(8):
            bt[ib]()
            if nxt is not None:
                nxt[2 * ib]()
                nxt[2 * ib + 1]()
        if ec == nec - 1:
            finalize(c)
    kb.end()


def build(dbg=False):
    nc = bass.Bass("TRN2", target_bir_lowering=False)
    gst = ExitStack()
    with gst:
        kb = KB(nc, gst)
        g = declare(kb, dbg)
        phase0(kb, g)
        phase_a(kb, g)
        phase_p(kb, g)
        phase_b(kb, g)
        phase_c(kb, g)
        phase_d(kb, g)
        phase_e(kb, g)
        kb.begin()
        kb.wait_all("sp", [g.out])
        kb.end()
    return nc


def host_inputs(inputs, core, shared):
    b, half = core // 2, core % 2
    x = np.asarray(inputs["x"], dtype=np.float32)
    pos = np.asarray(inputs["positions"]).astype(np.int32)
    xs = np.zeros((NT, 1024), np.float32)
    ps = np.zeros((NT, 1), np.int32)
    if half == 1:
        xs[:] = x[b]
        ps[:, 0] = pos[b]
    else:
        xs[NOWN:] = x[b, :NOWN]
        ps[NOWN:, 0] = pos[b, :NOWN]
    m = dict(shared)
    m["xs"] = xs
    m["pos"] = ps
    m["cc"] = np.ascontiguousarray(np.asarray(inputs["c"], np.float32)[b].reshape(8, 128))
    m["pv"] = np.full((128, 1), float(half), np.float32)
    return m


def shared_inputs(inputs):
    m = {}
    invf = (500000.0 ** (-(np.arange(8, dtype=np.float32) * 2.0) / 16.0)).astype(np.float32)
    m["invf"] = np.ascontiguousarray(np.broadcast_to(invf[None, :], (128, 8)))

    def w(name, shape, key=None):
        m[name] = np.ascontiguousarray(np.asarray(inputs[key or name], np.float32).reshape(shape))
    w("w_ada", (1024, 6144)); w("b_ada", (1, 6144)); w("norm1_g", (1, 1024)); w("w_in", (1024, 5376))
    w("q_norm_g", (1, 64)); w("k_norm_g", (1, 64)); w("rwkv_mu", (1, 1792)); w("rwkv_w0", (1, 512))
    w("rwkv_w2", (64, 512)); w("rwkv_a0", (1, 512)); w("rwkv_a2", (64, 512)); w("rwkv_g2", (128, 512))
    w("rwkv_k_k", (1, 512)); w("rwkv_k_a", (1, 512)); w("rwkv_r_k", (1, 512)); w("rwkv_ln_g", (1, 512))
    w("rwkv_ln_b", (1, 512)); w("w_proj_moba", (512, 1024)); w("w_proj_rwkv", (512, 1024)); w("w_out", (1024, 1024))
    w("norm2_g", (1, 1024)); w("peer_wq", (1024, 2048)); w("peer_sk", (16, 128, 128), "peer_subkeys")
    w("peer_u", (16384, 1024)); w("peer_v", (16384, 1024))
    return m


def kernel(**inputs):
    nc = build(False)
    shared = shared_inputs(inputs)
    in_maps = [host_inputs(inputs, c, shared) for c in range(8)]
    res = run_bass_kernel_spmd(nc, in_maps, core_ids=list(range(8)))
    out = np.zeros((4, 8192, 1024), np.float32)
    for c in range(8):
        b, half = c // 2, c % 2
        out[b, half * NOWN:(half + 1) * NOWN] = np.asarray(res.results[c]["out"], np.float32)
    return out
```
